# Optimizing a Trainium2 kernel written in Bass

```python
import math
import jax, jax.numpy as jnp
from jax import lax
import numpy as np

D_MODEL = 2048
BATCH = 16
SEQ = 2048
DEPTH = 2

N_MEM = 256
GRID_W = 64
HEAD_DIM = 64
EPS = 1e-6
NEG = -1e30
A_HEADS = 12
A_KV_HEADS = 4
A_WINDOW = 128
A_BLOCK = 128
B_HEADS = 8
NA_MAX_KH = 8
NA_KW = 16
NA_QCB = 16
NA_KCB = 32
C_HEADS = 12
C_KV_HEADS = 4
C_BLOCK = 128
ROPE_THETA = 10000.0
REL_BUCKETS = 32
REL_MAX_DIST = 128
MEM_HEADS = 4
MEM_HEAD_DIM = 128
MEM_W = MEM_HEADS * MEM_HEAD_DIM
PEER_HEADS = 8
PEER_NKEYS = 128
PEER_N = PEER_NKEYS * PEER_NKEYS
PEER_DKEY = 256
PEER_TOPK = 16
PEER_CHUNK = 128
A_Q = A_HEADS * HEAD_DIM
A_KV = A_KV_HEADS * HEAD_DIM
B_W = B_HEADS * HEAD_DIM
C_Q = C_HEADS * HEAD_DIM
C_KV = C_KV_HEADS * HEAD_DIM
D_MIX = A_Q + B_W + C_Q
D_IN = A_Q + 2 * A_KV + 3 * B_W + C_Q + 2 * C_KV

kernel_name = "hybrid_parallel_heads_peer_encoder"


def rmsnorm(x, g):
    xf = x.astype(jnp.float32)
    y = xf * lax.rsqrt(jnp.mean(xf * xf, axis=-1, keepdims=True) + EPS)
    return (y * g.astype(jnp.float32)).astype(x.dtype)


def t5_buckets(rel):
    nb = REL_BUCKETS // 2
    max_exact = nb // 2
    n = np.abs(rel)
    large = max_exact + (np.log(np.maximum(n, 1) / max_exact) / np.log(REL_MAX_DIST / max_exact)
                         * (nb - max_exact)).astype(np.int64)
    large = np.minimum(large, nb - 1)
    return np.where(rel > 0, nb, 0) + np.where(n < max_exact, n, large)


def window_attention(q, k, v, sink, rel_bias):
    b, s, _, dh = q.shape
    g = A_HEADS // A_KV_HEADS
    nb = s // A_BLOCK
    span = A_BLOCK + 2 * A_WINDOW
    kp = jnp.pad(k, ((0, 0), (A_WINDOW, A_WINDOW), (0, 0), (0, 0)))
    vp = jnp.pad(v, ((0, 0), (A_WINDOW, A_WINDOW), (0, 0), (0, 0)))
    rel = (np.arange(span)[None, :] - A_WINDOW) - np.arange(A_BLOCK)[:, None]
    in_window = np.abs(rel) <= A_WINDOW
    bias = rel_bias.astype(jnp.float32)[t5_buckets(rel)]
    bias = jnp.transpose(bias, (2, 0, 1)).reshape(A_KV_HEADS, g, A_BLOCK, span)
    sink_f = sink.astype(jnp.float32).reshape(A_KV_HEADS, g)
    qb = q.reshape(b, nb, A_BLOCK, A_KV_HEADS, g, dh).transpose(1, 0, 2, 3, 4, 5)
    scale = dh ** -0.5

    def block(args):
        qblk, bi = args
        start = bi * A_BLOCK
        kb = lax.dynamic_slice_in_dim(kp, start, span, axis=1)
        vb = lax.dynamic_slice_in_dim(vp, start, span, axis=1)
        logits = jnp.einsum('bqhgd,bkhd->bhgqk', qblk, kb).astype(jnp.float32) * scale + bias
        kpos = start - A_WINDOW + jnp.arange(span)
        valid = jnp.asarray(in_window) & (kpos >= 0)[None, :] & (kpos < s)[None, :]
        logits = jnp.where(valid, logits, NEG)
        sk = jnp.broadcast_to(sink_f[None, :, :, None, None], logits.shape[:-1] + (1,))
        p = jax.nn.softmax(jnp.concatenate([logits, sk], axis=-1), axis=-1)[..., :span]
        return jnp.einsum('bhgqk,bkhd->bqhgd', p.astype(v.dtype), vb)

    out = lax.map(block, (qb, jnp.arange(nb)))
    return out.transpose(1, 0, 2, 3, 4, 5).reshape(b, s, A_HEADS * dh)


def neighborhood_attention(q, k, v, rpb):
    b, s, h, dh = q.shape
    rows = s // GRID_W
    kh = min(NA_MAX_KH, rows)
    n_cb = GRID_W // NA_QCB
    slab = np.clip(np.arange(n_cb) * NA_QCB - NA_KW // 2, 0, GRID_W - NA_KCB)
    key_col = slab[:, None] + np.arange(NA_KCB)[None, :]
    q_col = np.arange(n_cb)[:, None] * NA_QCB + np.arange(NA_QCB)[None, :]
    c_start = np.clip(q_col - NA_KW // 2, 0, GRID_W - NA_KW)
    kc = key_col[:, None, :]
    col_ok = (kc >= c_start[:, :, None]) & (kc < c_start[:, :, None] + NA_KW)
    dc_idx = np.clip(kc - q_col[:, :, None] + NA_KW - 1, 0, 2 * NA_KW - 2)
    bias_c = rpb.astype(jnp.float32)[:, :, dc_idx]
    col_ok_b = jnp.asarray(col_ok[:, :, None, :])
    qg = q.reshape(b, rows, GRID_W, h, dh)
    kg = k.reshape(b, rows, GRID_W, h, dh)
    vg = v.reshape(b, rows, GRID_W, h, dh)
    scale = dh ** -0.5

    def one_row(r):
        r0 = jnp.clip(r - kh // 2, 0, rows - kh)
        qr = lax.dynamic_index_in_dim(qg, r, axis=1, keepdims=False).reshape(b, n_cb, NA_QCB, h, dh)
        kr = jnp.take(lax.dynamic_slice_in_dim(kg, r0, kh, axis=1), key_col, axis=2)
        vr = jnp.take(lax.dynamic_slice_in_dim(vg, r0, kh, axis=1), key_col, axis=2)
        logits = jnp.einsum('bcqhd,bickhd->bhcqik', qr, kr).astype(jnp.float32) * scale
        dr = r0 + jnp.arange(kh) - r + NA_MAX_KH - 1
        bias = jnp.take(bias_c, dr, axis=1).transpose(0, 2, 3, 1, 4)
        logits = jnp.where(col_ok_b, logits + bias[None], NEG)
        p = jax.nn.softmax(logits, axis=(-2, -1))
        o = jnp.einsum('bhcqik,bickhd->bcqhd', p.astype(v.dtype), vr)
        return o.reshape(b, GRID_W, h, dh)

    out = lax.map(one_row, jnp.arange(rows))
    return out.transpose(1, 0, 2, 3, 4).reshape(b, s, h * dh)


def rope_half(x, pos):
    n = x.shape[-1]
    nf = n // 2
    inv = jnp.asarray((ROPE_THETA ** (-np.arange(nf) * 2.0 / n)).astype(np.float32))
    ang = pos.astype(jnp.float32)[:, None] * inv[None, :]
    cos = jnp.cos(ang)[None, :, None, :]
    sin = jnp.sin(ang)[None, :, None, :]
    xf = x.astype(jnp.float32)
    x1, x2 = xf[..., :nf], xf[..., nf:]
    return jnp.concatenate([x1 * cos - x2 * sin, x1 * sin + x2 * cos], axis=-1).astype(x.dtype)


def axial_rope(x, row, col):
    half = x.shape[-1] // 2
    return jnp.concatenate([rope_half(x[..., :half], row), rope_half(x[..., half:], col)], axis=-1)


def dense_gqa(q, k, v):
    b, s, h, dh = q.shape
    hk = k.shape[2]
    g = h // hk
    nb = s // C_BLOCK
    qb = q.reshape(b, nb, C_BLOCK, hk, g, dh).transpose(1, 0, 2, 3, 4, 5)
    scale = dh ** -0.5

    def block(qblk):
        logits = jnp.einsum('bqhgd,bkhd->bhgqk', qblk, k).astype(jnp.float32) * scale
        p = jax.nn.softmax(logits, axis=-1)
        return jnp.einsum('bhgqk,bkhd->bqhgd', p.astype(v.dtype), v)

    out = lax.map(block, qb)
    return out.transpose(1, 0, 2, 3, 4, 5).reshape(b, s, h * dh)


def memory_attention(h, mem, w_q, w_kv, qk_g, w_o):
    b, s, _ = h.shape
    q = rmsnorm((h @ w_q).reshape(b, s, MEM_HEADS, MEM_HEAD_DIM), qk_g[0])
    kv = (mem @ w_kv).reshape(b, mem.shape[1], 2, MEM_HEADS, MEM_HEAD_DIM)
    k = rmsnorm(kv[:, :, 0], qk_g[1])
    vv = kv[:, :, 1]
    logits = jnp.einsum('bshd,bmhd->bhsm', q, k).astype(jnp.float32) * MEM_HEAD_DIM ** -0.5
    p = jax.nn.softmax(logits, axis=-1)
    o = jnp.einsum('bhsm,bmhd->bshd', p.astype(vv.dtype), vv)
    return o.reshape(b, s, MEM_W) @ w_o


def peer_ffn(h, w_q, sub_keys, u, v):
    b, s, d = h.shape
    nc = (b * s) // PEER_CHUNK

    def chunk(xc):
        q = (xc @ w_q).reshape(PEER_CHUNK, PEER_HEADS, 2, PEER_DKEY // 2)
        sc = jnp.einsum('chpd,hpnd->chpn', q, sub_keys).astype(jnp.float32)
        top_s, top_i = lax.top_k(sc, PEER_TOPK)
        cand_s = (top_s[:, :, 0, :, None] + top_s[:, :, 1, None, :]).reshape(PEER_CHUNK, PEER_HEADS, PEER_TOPK * PEER_TOPK)
        cand_i = (top_i[:, :, 0, :, None] * PEER_NKEYS + top_i[:, :, 1, None, :]).reshape(PEER_CHUNK, PEER_HEADS, PEER_TOPK * PEER_TOPK)
        best_s, pos = lax.top_k(cand_s, PEER_TOPK)
        expert = jnp.take_along_axis(cand_i, pos, axis=-1)
        gate = jax.nn.softmax(best_s, axis=-1)
        pre = jnp.einsum('cd,chkd->chk', xc, jnp.take(u, expert, axis=0)).astype(jnp.float32)
        w = (gate * jax.nn.gelu(pre, approximate=False)).astype(v.dtype)
        return jnp.einsum('chk,chkd->cd', w, jnp.take(v, expert, axis=0))

    y = lax.map(chunk, h.reshape(nc, PEER_CHUNK, d))
    return y.reshape(b, s, d)


def setup_inputs(seed: int = 0) -> dict:
    key = jax.random.key(seed)
    ks = jax.random.split(key, 24)
    nrm = lambda k, shape, scale: jax.random.normal(k, shape, jnp.float32) * scale
    gain = lambda k, shape: 1.0 + 0.02 * jax.random.normal(k, shape, jnp.float32)
    L = DEPTH
    return {
        "x": nrm(ks[0], (BATCH, SEQ, D_MODEL), 1.0),
        "mem": nrm(ks[1], (BATCH, N_MEM, D_MODEL), 1.0),
        "t5_rel_bias": nrm(ks[2], (REL_BUCKETS, A_HEADS), 0.1),
        "norm_mix": gain(ks[3], (L, D_MODEL)),
        "w_in": nrm(ks[4], (L, D_MODEL, D_IN), D_MODEL ** -0.5),
        "qk_norm_a": gain(ks[5], (L, 2, HEAD_DIM)),
        "sink_a": nrm(ks[6], (L, A_HEADS), 0.5),
        "qk_norm_b": gain(ks[7], (L, 2, HEAD_DIM)),
        "rpb_b": nrm(ks[8], (L, B_HEADS, 2 * NA_MAX_KH - 1, 2 * NA_KW - 1), 0.1),
        "qk_norm_c": gain(ks[9], (L, 2, HEAD_DIM)),
        "out_norm": gain(ks[10], (L, D_MIX)),
        "w_out": nrm(ks[11], (L, D_MIX, D_MODEL), D_MIX ** -0.5),
        "norm_mem": gain(ks[12], (L, D_MODEL)),
        "norm_mem_kv": gain(ks[13], (L, D_MODEL)),
        "w_mem_q": nrm(ks[14], (L, D_MODEL, MEM_W), D_MODEL ** -0.5),
        "w_mem_kv": nrm(ks[15], (L, D_MODEL, 2 * MEM_W), D_MODEL ** -0.5),
        "qk_norm_mem": gain(ks[16], (L, 2, MEM_HEAD_DIM)),
        "w_mem_o": nrm(ks[17], (L, MEM_W, D_MODEL), MEM_W ** -0.5),
        "norm_ffn": gain(ks[18], (L, D_MODEL)),
        "peer_w_q": nrm(ks[19], (L, D_MODEL, PEER_HEADS * PEER_DKEY), D_MODEL ** -0.5),
        "peer_keys": nrm(ks[20], (L, PEER_HEADS, 2, PEER_NKEYS, PEER_DKEY // 2), (PEER_DKEY // 2) ** -0.5),
        "peer_u": nrm(ks[21], (L, PEER_N, D_MODEL), D_MODEL ** -0.5),
        "peer_v": nrm(ks[22], (L, PEER_N, D_MODEL), PEER_HEADS ** -0.5),
    }


def reference(x, mem, t5_rel_bias, norm_mix, w_in, qk_norm_a, sink_a, qk_norm_b, rpb_b, qk_norm_c,
              out_norm, w_out, norm_mem, norm_mem_kv, w_mem_q, w_mem_kv, qk_norm_mem, w_mem_o,
              norm_ffn, peer_w_q, peer_keys, peer_u, peer_v):
    b, s, _ = x.shape
    t = jnp.arange(s)
    row, col = t // GRID_W, t % GRID_W
    splits = list(np.cumsum([A_Q, A_KV, A_KV, B_W, B_W, B_W, C_Q, C_KV]))
    heads = lambda z, n: z.reshape(b, s, n, HEAD_DIM)
    for l in range(DEPTH):
        hn = rmsnorm(x, norm_mix[l])
        qa, ka, va, qb, kb, vb, qc, kc, vc = jnp.split(hn @ w_in[l], splits, axis=-1)
        oa = window_attention(rmsnorm(heads(qa, A_HEADS), qk_norm_a[l, 0]),
                              rmsnorm(heads(ka, A_KV_HEADS), qk_norm_a[l, 1]),
                              heads(va, A_KV_HEADS), sink_a[l], t5_rel_bias)
        ob = neighborhood_attention(rmsnorm(heads(qb, B_HEADS), qk_norm_b[l, 0]),
                                    rmsnorm(heads(kb, B_HEADS), qk_norm_b[l, 1]),
                                    heads(vb, B_HEADS), rpb_b[l])
        oc = dense_gqa(axial_rope(rmsnorm(heads(qc, C_HEADS), qk_norm_c[l, 0]), row, col),
                       axial_rope(rmsnorm(heads(kc, C_KV_HEADS), qk_norm_c[l, 1]), row, col),
                       heads(vc, C_KV_HEADS))
        g_out = out_norm[l]
        mixed = jnp.concatenate([rmsnorm(oa, g_out[:A_Q]),
                                 rmsnorm(ob, g_out[A_Q:A_Q + B_W]),
                                 rmsnorm(oc, g_out[A_Q + B_W:])], axis=-1)
        x = x + mixed @ w_out[l]
        x = x + memory_attention(rmsnorm(x, norm_mem[l]), rmsnorm(mem, norm_mem_kv[l]),
                                 w_mem_q[l], w_mem_kv[l], qk_norm_mem[l], w_mem_o[l])
        x = x + peer_ffn(rmsnorm(x, norm_ffn[l]), peer_w_q[l], peer_keys[l], peer_u[l], peer_v[l])
    return x
```

```python
import math
from contextlib import ExitStack

import ml_dtypes
import numpy as np

import concourse.bass as bass
import concourse.mybir as mybir
from concourse.bass_utils import run_bass_kernel_spmd

F32 = mybir.dt.float32
BF16 = mybir.dt.bfloat16
ALU = mybir.AluOpType
AF = mybir.ActivationFunctionType
AX = mybir.AxisListType

D = 2048
S = 2048
NT = 16
KC = 16
L = 2
NMEM = 256
EPS = 1e-6
N_DMA_SEMS = 24
SAME_ENG_SYNC = True
ARENA = 105000


class Res:
    __slots__ = ("name", "lw", "rs")

    def __init__(self, name=""):
        self.name = name
        self.lw = None
        self.rs = []


class Op:
    __slots__ = ("eng", "fn", "deps", "signal", "sem", "val", "ndma", "clock", "gi")


class Prog:
    ENGS = ["pe", "act", "dve", "pool", "sp"]

    def __init__(self, nc):
        self.nc = nc
        self.ops = []
        self.last = {e: None for e in self.ENGS}
        self.dmas_since_barrier = []

    def op(self, eng, fn, reads=(), writes=(), ndma=0):
        o = Op()
        o.eng = eng
        o.fn = fn
        o.ndma = ndma
        o.signal = ndma > 0
        o.sem = None
        o.val = None
        o.clock = None
        o.gi = len(self.ops)
        deps = {}
        for r in reads:
            if r.lw is not None:
                deps[r.lw.gi] = r.lw
        for w in writes:
            if w.lw is not None:
                deps[w.lw.gi] = w.lw
            for x in w.rs:
                deps[x.gi] = x
        best = {}
        dl = []
        for d in deps.values():
            if d.ndma:
                dl.append(d)
                continue
            if d.eng == o.eng and (o.eng == "pe" or not SAME_ENG_SYNC):
                continue
            b = best.get(d.eng)
            if b is None or b.gi < d.gi:
                best[d.eng] = d
        for d in best.values():
            d.signal = True
            dl.append(d)
        o.deps = dl
        for r in reads:
            r.rs.append(o)
        for w in writes:
            w.lw = o
            w.rs = []
        self.ops.append(o)
        self.last[eng] = o
        if ndma:
            self.dmas_since_barrier.append(o)
        return o

    def barrier(self):
        toks = []
        for e in self.ENGS:
            if self.last[e] is not None:
                r = Res("bar_" + e)
                self.op(e, lambda eng: eng.drain(), writes=[r])
                toks.append(r)
        dm = list(self.dmas_since_barrier)
        self.dmas_since_barrier = []
        for e in self.ENGS:
            o = self.op(e, lambda eng: eng.drain(), reads=toks)
            for d in dm:
                o.deps.append(d)

    def finalize(self, stack):
        nc = self.nc
        engsem = {e: stack.enter_context(nc.semaphore("s_" + e)) for e in self.ENGS}
        dmasems = {
            e: [stack.enter_context(nc.semaphore("d_%s_%d" % (e, i))) for i in range(N_DMA_SEMS)]
            for e in ("sp", "act", "pool")
        }
        prev = {e: [None] * N_DMA_SEMS for e in dmasems}
        sval = {e: [0] * N_DMA_SEMS for e in dmasems}
        dcnt = {e: 0 for e in dmasems}
        cnt = {e: 0 for e in self.ENGS}
        clock = {e: {} for e in self.ENGS}
        lists = {e: [] for e in self.ENGS}
        for o in self.ops:
            ck = clock[o.eng]
            deps = list(o.deps)
            if o.ndma:
                slot = dcnt[o.eng] % N_DMA_SEMS
                dcnt[o.eng] += 1
                p = prev[o.eng][slot]
                if p is not None:
                    deps.append(p)
                o.sem = dmasems[o.eng][slot]
                sval[o.eng][slot] += 16 * o.ndma
                o.val = sval[o.eng][slot]
                prev[o.eng][slot] = o
            waits = {}
            for d in deps:
                key = id(d.sem)
                if ck.get(key, 0) >= d.val:
                    continue
                if key not in waits or waits[key][1] < d.val:
                    waits[key] = (d.sem, d.val)
            for d in deps:
                if d.clock:
                    for k, v in d.clock.items():
                        if ck.get(k, 0) < v:
                            ck[k] = v
            for key, (s, v) in waits.items():
                if ck.get(key, 0) < v:
                    ck[key] = v
            wl = list(waits.values())
            if o.ndma == 0 and o.signal:
                cnt[o.eng] += 1
                o.sem = engsem[o.eng]
                o.val = cnt[o.eng]
            if o.signal:
                c = dict(ck)
                if o.ndma == 0:
                    c[id(o.sem)] = o.val
                o.clock = c
            lists[o.eng].append((wl, o))
        self.lists = lists

    def emit(self):
        nc = self.nc
        lists = self.lists

        def run(eng, items):
            for wl, o in items:
                for s, v in wl:
                    eng.wait_ge(s, v)
                if o.ndma:
                    o.fn(eng, o.sem)
                else:
                    ins = o.fn(eng)
                    if o.signal:
                        ins.then_inc(o.sem, 1)

        with nc.Block() as block:
            @block.tensor
            def _(e):
                run(e, lists["pe"])

            @block.scalar
            def _(e):
                run(e, lists["act"])

            @block.vector
            def _(e):
                run(e, lists["dve"])

            @block.gpsimd
            def _(e):
                run(e, lists["pool"])

            @block.sync
            def _(e):
                run(e, lists["sp"])


def _t5_buckets(rel):
    nb = 16
    max_exact = nb // 2
    n = np.abs(rel)
    large = max_exact + (np.log(np.maximum(n, 1) / max_exact) / np.log(128 / max_exact) * (nb - max_exact)).astype(np.int64)
    large = np.minimum(large, nb - 1)
    return np.where(rel > 0, nb, 0) + np.where(n < max_exact, n, large)


def _static_tables():
    t = {}
    t["c_ident"] = np.eye(128, dtype=np.float32).astype(ml_dtypes.bfloat16)
    t["c_j"] = np.ascontiguousarray(np.eye(128, dtype=np.float32)[::-1])
    delta = np.arange(512) - 255
    bk = _t5_buckets(delta)
    oh = np.zeros((32, 512), np.float32)
    oh[bk, np.arange(512)] = 1.0
    t["c_oh"] = oh
    k = np.arange(128)[:, None, None]
    ri = np.arange(3)[None, :, None]
    q = np.arange(128)[None, None, :]
    dd = k + 128 * (ri - 1) - q
    t["c_maska"] = (np.abs(dd) <= 128).astype(np.float32).astype(ml_dtypes.bfloat16)
    kc = np.arange(64)[:, None]
    qc = np.arange(64)[None, :]
    cs = np.clip(qc - 8, 0, 48)
    cv = ((kc >= cs) & (kc < cs + 16)).astype(np.float32)
    t["c_colv"] = np.concatenate([cv, cv], axis=0).astype(ml_dtypes.bfloat16)
    tok = np.arange(S)
    row = (tok // 64).astype(np.float32)
    col = (tok % 64).astype(np.float32)
    inv = (10000.0 ** (-np.arange(16) * 2.0 / 32)).astype(np.float32)
    ar = row[:, None] * inv[None, :]
    ac = col[:, None] * inv[None, :]
    cc = np.concatenate([np.cos(ar), np.cos(ar), np.cos(ac), np.cos(ac)], axis=1)
    ss = np.concatenate([-np.sin(ar), np.sin(ar), -np.sin(ac), np.sin(ac)], axis=1)
    t["c_ropec"] = cc.astype(np.float32)
    t["c_ropes"] = ss.astype(np.float32)
    t["c_zero"] = np.zeros((8, 1152), np.float32)
    return t


WEIGHT_SPECS = [
    ("t5_rel_bias", (32, 12)), ("norm_mix", (L, D)), ("w_in", (L, D, 4096)), ("qk_norm_a", (L, 2, 64)),
    ("sink_a", (L, 12)), ("qk_norm_b", (L, 2, 64)), ("rpb_b", (L, 8, 15, 31)), ("qk_norm_c", (L, 2, 64)),
    ("out_norm", (L, D)), ("w_out", (L, D, D)), ("norm_mem", (L, D)), ("norm_mem_kv", (L, D)),
    ("w_mem_q", (L, D, 512)), ("w_mem_kv", (L, D, 1024)), ("qk_norm_mem", (L, 2, 128)), ("w_mem_o", (L, 512, D)),
    ("norm_ffn", (L, D)), ("peer_w_q", (L, D, D)), ("peer_keys", (L, 8, 2, 128, 128)),
    ("peer_u", (L, 16384, D)), ("peer_v", (L, 16384, D)),
]


class KB:
    def __init__(self, nc, nseq, layers, phases, dbg, lw=L):
        self.nc = nc
        self.lw = lw
        self.P = Prog(nc)
        self.nseq = nseq
        self.layers = layers
        self.phases = phases
        self.dbg = dbg
        self.st = ExitStack()
        st = self.st
        self.I = {}
        self.I["x"] = nc.dram_tensor("x", [nseq, S, D], F32, kind="ExternalInput").ap()
        self.I["mem"] = nc.dram_tensor("mem", [nseq, NMEM, D], F32, kind="ExternalInput").ap()
        for name, shp in WEIGHT_SPECS:
            if name != "t5_rel_bias":
                shp = (lw,) + tuple(shp[1:])
            if name in ("peer_u", "peer_v") and "E" not in phases:
                shp = (lw, 128, D)
            self.I[name] = nc.dram_tensor(name, list(shp), F32, kind="ExternalInput").ap()
        for name, arr in _static_tables().items():
            dt = BF16 if arr.dtype == ml_dtypes.bfloat16 else F32
            self.I[name] = nc.dram_tensor(name, list(arr.shape), dt, kind="ExternalInput").ap()
        self.out = nc.dram_tensor("out", [nseq, S, D], F32, kind="ExternalOutput").ap()
        kind = "ExternalOutput" if dbg else "Internal"
        self.Sx = {}

        def scr(name, shp, dt):
            self.Sx[name] = nc.dram_tensor(name, list(shp), dt, kind=kind).ap()

        scr("qt_a", [12, 64, S], BF16)
        scr("kt_a", [4, 64, S], BF16)
        scr("v_a", [S, 4, 65], BF16)
        scr("qt_b", [8, 64, S], BF16)
        scr("kt_b", [8, 64, S], BF16)
        scr("v_b", [S, 8, 65], BF16)
        scr("qt_c", [12, 64, S], BF16)
        scr("kt_c", [4, 64, S], BF16)
        scr("v_c", [S, 4, 65], BF16)
        scr("mixed", [S, D], BF16)
        if dbg:
            scr("dbg_sm", [128, 64], F32)
            scr("dbg_s0", [128, 1024], F32)
            scr("dbg_s1", [128, 1024], F32)
            scr("dbg_ge", [128, 512], BF16)
            scr("dbg_wt", [128, 512], BF16)
            scr("dbg_y", [128, 2048], F32)
        scr("tv", [12, 512], F32)
        scr("wb", [8, 1152], F32)
        self.arena = st.enter_context(nc.sbuf_tensor("arena", [128, ARENA], BF16))
        self.ps = [st.enter_context(nc.psum_tensor("ps%d" % i, [128, 512], F32)) for i in range(8)]
        self.rps = [Res("ps%d" % i) for i in range(8)]
        self.top = 0
        self.dma_rr = 0

    def alloc(self, n, dt=BF16):
        nb = n if dt == BF16 else 2 * n
        self.top = (self.top + 31) // 32 * 32
        a = self.top
        self.top += nb
        assert self.top <= ARENA, "arena overflow %d" % self.top
        ap = self.arena[:, a:a + nb]
        if dt == F32:
            ap = ap.bitcast(F32)
        return ap

    def mm(self, out, lhsT, rhs, start, stop, rd, wr):
        return self.P.op("pe", lambda e: e.matmul(out, lhsT=lhsT, rhs=rhs, start=start, stop=stop), rd, wr)

    def tr(self, out, in_, rd, wr):
        ident = self.ident
        return self.P.op("pe", lambda e: e.transpose(out=out, in_=in_, identity=ident), list(rd) + [self.rconst], wr)

    def act(self, out, in_, func, rd, wr, scale=None, accum=None):
        kw = {}
        if scale is not None:
            kw["scale"] = scale
        if accum is not None:
            kw["accum_out"] = accum
        return self.P.op("act", lambda e: e.activation(out=out, in_=in_, func=func, **kw), rd, wr)

    def tt(self, eng, out, in0, in1, op, rd, wr):
        return self.P.op(eng, lambda e: e.tensor_tensor(out=out, in0=in0, in1=in1, op=op), rd, wr)

    def ts(self, eng, out, in0, s1, s2, op0, op1, rd, wr):
        if op1 is None:
            return self.P.op(eng, lambda e: e.tensor_scalar(out=out, in0=in0, scalar1=s1, scalar2=None, op0=op0), rd, wr)
        return self.P.op(eng, lambda e: e.tensor_scalar(out=out, in0=in0, scalar1=s1, scalar2=s2, op0=op0, op1=op1), rd, wr)

    def stt(self, out, in0, scalar, in1, op0, op1, rd, wr):
        return self.P.op("dve", lambda e: e.scalar_tensor_tensor(out=out, in0=in0, scalar=scalar, in1=in1, op0=op0, op1=op1), rd, wr)

    def cp(self, eng, out, in_, rd, wr):
        if eng == "act":
            return self.act(out, in_, AF.Copy, rd, wr)
        return self.P.op(eng, lambda e: e.tensor_copy(out=out, in_=in_), rd, wr)

    def recip(self, out, in_, rd, wr):
        return self.P.op("dve", lambda e: e.reciprocal(out=out, in_=in_), rd, wr)

    def memset(self, eng, ap, val, wr):
        return self.P.op(eng, lambda e: e.memset(ap, val), [], wr)

    def dma(self, out, in_, rd, wr, eng=None, slow=False):
        if eng is None:
            eng = "sp"
        if slow:
            fn = lambda e, s: e.dma_start(out=out, in_=in_, allow_slow_non_contiguous=True).then_inc(s, 16)
        else:
            fn = lambda e, s: e.dma_start(out=out, in_=in_).then_inc(s, 16)
        return self.P.op(eng, fn, rd, wr, ndma=1)

    def rstd_from_ssq(self, st, n, rd_wr):
        self.ts("dve", st[:, 1:2], st[:, 0:1], 1.0 / n, EPS, ALU.mult, ALU.add, [rd_wr], [rd_wr])
        self.act(st[:, 2:3], st[:, 1:2], AF.Sqrt, [rd_wr], [rd_wr])
        self.recip(st[:, 3:4], st[:, 2:3], [rd_wr], [rd_wr])

    def setup_consts(self):
        I = self.I
        self.rconst = Res("const")
        rc = self.rconst
        self.ident = self.alloc(128)
        self.jf = self.alloc(128, F32)
        self.maska = self.alloc(3 * 128)
        self.colv = self.alloc(64)
        self.ropec = self.alloc(NT * 64, F32).rearrange("p (t e) -> p t e", t=NT)
        self.ropes = self.alloc(NT * 64, F32).rearrange("p (t e) -> p t e", t=NT)
        self.eba = self.alloc(3 * 12 * 128).rearrange("p (r h q) -> p r h q", r=3, h=12)
        self.ebb = self.alloc(8 * 14 * 64).rearrange("p (h r q) -> p h r q", h=8, r=14)
        self.gq = {}
        self.dma(self.ident, I["c_ident"], [], [rc])
        self.dma(self.jf, I["c_j"], [], [rc])
        self.dma(self.maska, I["c_maska"].rearrange("k r q -> k (r q)"), [], [rc])
        self.dma(self.colv, I["c_colv"], [], [rc])
        self.dma(self.ropec, I["c_ropec"].rearrange("(t p) e -> p t e", p=128), [], [rc])
        self.dma(self.ropes, I["c_ropes"].rearrange("(t p) e -> p t e", p=128), [], [rc])
        LW = self.lw
        self.gains = self.alloc(LW * (6 * 64 + 2 * 128), F32)
        self.esink = self.alloc(LW * 12, F32)
        self.gcol = self.alloc(LW * 5 * KC, F32).rearrange("p (l g k) -> p l g k", l=LW, g=5)
        self.g = {}
        off = 0
        for l in range(LW):
            for nm, n2 in (("qk_norm_a", 64), ("qk_norm_b", 64), ("qk_norm_c", 64), ("qk_norm_mem", 128)):
                for i in range(2):
                    dst = self.gains[:, off:off + n2]
                    src = I[nm][l, i:i + 1, :]
                    src = bass.AP(src.tensor, src.offset, [[0, 128], [1, n2]])
                    self.dma(dst, src, [], [rc])
                    self.g[(nm, l, i)] = dst
                    off += n2
            src = I["sink_a"][l:l + 1, :]
            src = bass.AP(src.tensor, src.offset, [[0, 128], [1, 12]])
            self.dma(self.esink[:, l * 12:(l + 1) * 12], src, [], [rc])
            for gi, nm in enumerate(("norm_mix", "out_norm", "norm_mem", "norm_mem_kv", "norm_ffn")):
                src = I[nm][l]
                src = bass.AP(src.tensor, src.offset, [[1, 128], [128, KC]])
                self.dma(self.gcol[:, l, gi, :], src, [], [rc], slow=True)
        self.act(self.esink, self.esink, AF.Exp, [rc], [rc])
        self.build_eba()
        self.const_top = self.top

    def build_eba(self):
        I = self.I
        rc = self.rconst
        m = self.top
        relb = self.alloc(12, F32)
        oh = self.alloc(512, F32)
        tvs = self.alloc(512, F32)
        ht = self.alloc(3 * 128, F32).rearrange("p (r k) -> p r k", r=3)
        tmp = self.alloc(128)
        r1, r2, r3, r4 = Res("relb"), Res("tvs"), Res("tvd"), Res("ht")
        self.dma(relb[0:32, :], I["t5_rel_bias"], [], [r1])
        self.dma(oh[0:32, :], I["c_oh"], [], [r1])
        self.mm(self.ps[0][0:12, :], relb[0:32, :], oh[0:32, :], True, True, [r1], [self.rps[0]])
        self.act(tvs[0:12, :], self.ps[0][0:12, :], AF.Copy, [self.rps[0]], [r2])
        self.dma(self.Sx["tv"], tvs[0:12, :], [r2], [r3], eng="pool")
        tvt = self.Sx["tv"].tensor
        rtmp = Res("tmp")
        for h in range(12):
            src = bass.AP(tvt, h * 512, [[1, 128], [128, 3], [1, 128]])
            self.dma(ht, src, [r3], [r4])
            for r in range(3):
                pb = 1 + (h * 3 + r) % 2
                self.mm(self.ps[pb][:, 0:128], ht[:, r, :], self.jf, True, True, [r4, rc], [self.rps[pb]])
                self.act(tmp, self.ps[pb][:, 0:128], AF.Exp, [self.rps[pb]], [rtmp])
                self.tt("dve", self.eba[:, r, h, :], tmp, self.maska[:, r * 128:(r + 1) * 128], ALU.mult, [rtmp, rc], [rc])
        self.P.barrier()
        self.top = m

    def build_ebb(self, l):
        I = self.I
        rc = self.rconst
        m = self.top
        htb = self.alloc(14 * 128, F32).rearrange("p (r k) -> p r k", r=14)
        tmp = self.alloc(7 * 64)
        r1, r2, r3 = Res("wbz"), Res("htb"), Res("tmpb")
        wbt = self.Sx["wb"].tensor
        self.dma(self.Sx["wb"], I["c_zero"], [], [r1])
        for h in range(8):
            dst = bass.AP(wbt, h * 1152 + 48, [[64, 15], [1, 31]])
            self.dma(dst, I["rpb_b"][l, h], [r1], [r1])
        j64 = self.jf[0:64, 64:128]
        for h in range(8):
            src = bass.AP(wbt, h * 1152, [[1, 64], [64, 14], [1, 128]])
            self.dma(htb[0:64], src, [r1], [r2])
            for half in range(2):
                pb = 1 + half
                for r in range(7):
                    self.mm(self.ps[pb][:, r * 64:(r + 1) * 64], htb[0:64, half * 7 + r, :], j64, True, True,
                            [r2, rc], [self.rps[pb]])
                self.act(tmp, self.ps[pb][:, 0:448], AF.Exp, [self.rps[pb]], [r3])
                colb = bass.AP(self.colv.tensor, self.colv.offset, [list(self.colv.ap[0]), [0, 7], [1, 64]])
                self.tt("dve", self.ebb[:, h, half * 7:(half + 1) * 7, :], tmp.rearrange("p (r q) -> p r q", r=7), colb,
                        ALU.mult, [r3, rc], [rc])
        self.P.barrier()
        self.top = m

    def norm_transpose(self, src_rows, ntiles, dstT, rdst, rsrc=None, pbase=0):
        m = self.top
        xt = [self.alloc(D, F32) for _ in range(2)]
        xn = [self.alloc(D) for _ in range(2)]
        junk = self.alloc(D)
        stt_ = [self.alloc(8, F32) for _ in range(2)]
        rx = [Res("xt%d" % i) for i in range(2)]
        rn = [Res("xn%d" % i) for i in range(2)]
        rs = [Res("st%d" % i) for i in range(2)]
        rj = Res("junk")
        for t in range(ntiles):
            b = t % 2
            self.dma(xt[b], src_rows(t), [rsrc[t]] if rsrc else [], [rx[b]])
            self.act(junk, xt[b], AF.Square, [rx[b]], [rj, rs[b]], accum=stt_[b][:, 0:1])
            self.rstd_from_ssq(stt_[b], D, rs[b])
            self.act(xn[b], xt[b], AF.Copy, [rx[b], rs[b]], [rn[b]], scale=stt_[b][:, 3:4])
            for g in range(4):
                pb = pbase + g % 2
                pv = self.ps[pb][:, 0:256].bitcast(BF16)
                for j in range(4):
                    kc = g * 4 + j
                    self.tr(pv[:, j * 128:(j + 1) * 128], xn[b][:, kc * 128:(kc + 1) * 128], [rn[b]], [self.rps[pb]])
                eng = "dve" if g % 2 == 0 else "act"
                self.cp(eng, dstT[:, g * 4:(g + 1) * 4, t * 128:(t + 1) * 128], pv.rearrange("p (j q) -> p j q", j=4),
                        [self.rps[pb]], [rdst[t]])
        self.P.barrier()
        self.top = m

    def linear(self, actT, ract, ntiles, w_dram, kcn, ncols, nblk, gcol, epilogue, pbase=2):
        m = self.top
        wst = [self.alloc(kcn * nblk, F32).rearrange("p (k n) -> p k n", k=kcn) for _ in range(2)]
        wbf = [self.alloc(kcn * nblk).rearrange("p (k n) -> p k n", k=kcn) for _ in range(2)]
        rst = [Res("wst%d" % i) for i in range(2)]
        rwb = [[Res("wbf%d_%d" % (i, k)) for k in range(kcn)] for i in range(2)]
        wv = w_dram.rearrange("(k p) n -> p k n", p=128)
        engs = ["act", "dve", "pool"]
        for nb in range(ncols // nblk):
            b = nb % 2
            self.dma(wst[b], wv[:, :, nb * nblk:(nb + 1) * nblk], [], [rst[b]])
            for k in range(kcn):
                eng = engs[k % 3]
                if gcol is None:
                    self.cp(eng, wbf[b][:, k, :], wst[b][:, k, :], [rst[b]], [rwb[b][k]])
                elif eng == "act":
                    self.act(wbf[b][:, k, :], wst[b][:, k, :], AF.Copy, [rst[b], self.rconst], [rwb[b][k]], scale=gcol[:, k:k + 1])
                else:
                    self.ts(eng, wbf[b][:, k, :], wst[b][:, k, :], gcol[:, k:k + 1], None, ALU.mult, None,
                            [rst[b], self.rconst], [rwb[b][k]])
            for t in range(ntiles):
                pb = pbase + t % 2
                ps = self.ps[pb][:, 0:nblk]
                for k in range(kcn):
                    self.mm(ps, actT[:, k, t * 128:(t + 1) * 128], wbf[b][:, k, :], k == 0, k == kcn - 1,
                            [ract[t], rwb[b][k]], [self.rps[pb]])
                epilogue(nb, t, ps, self.rps[pb])
        self.P.barrier()
        self.top = m

    def headnorm(self, ps, rps, nh, dh, gain, sc):
        sq, ss, qn, qg = sc["sq"], sc["ss"], sc["qn"], sc["qg"]
        r = sc["r"]
        n = nh * dh
        self.act(sq[:, 0:n], ps, AF.Square, [rps], [r["sq"]])
        self.P.op("dve", lambda e: e.tensor_reduce(out=ss[:, 0:nh], in_=sq[:, 0:n].rearrange("p (h d) -> p h d", h=nh), axis=AX.X, op=ALU.add),
                  [r["sq"]], [r["ss"]])
        self.ts("dve", ss[:, 8:8 + nh], ss[:, 0:nh], 1.0 / dh, EPS, ALU.mult, ALU.add, [r["ss"]], [r["ss"]])
        self.act(ss[:, 16:16 + nh], ss[:, 8:8 + nh], AF.Sqrt, [r["ss"]], [r["ss"]])
        self.recip(ss[:, 24:24 + nh], ss[:, 16:16 + nh], [r["ss"]], [r["ss"]])
        rsb = ss[:, 24:24 + nh].unsqueeze(2).to_broadcast([128, nh, dh])
        self.tt("dve", qn[:, 0:n].rearrange("p (h d) -> p h d", h=nh), ps.rearrange("p (h d) -> p h d", h=nh), rsb, ALU.mult,
                [rps, r["ss"]], [r["qn"]])
        gb = gain.unsqueeze(1).to_broadcast([128, nh, dh])
        return gb

    def phase_A(self, l, xsrc):
        I, Sx = self.I, self.Sx
        m = self.top
        xnT = self.alloc(KC * S).rearrange("p (k t) -> p k t", k=KC)
        rxn = [Res("xnT%d" % t) for t in range(NT)]
        self.norm_transpose(lambda t: xsrc[t * 128:(t + 1) * 128, :], NT, xnT, rxn)
        sc = {
            "sq": self.alloc(256, F32), "ss": self.alloc(32, F32), "qn": self.alloc(256, F32), "qg": self.alloc(256, F32),
            "t1": self.alloc(256, F32), "t2": self.alloc(256, F32),
            "r": {k: Res(k) for k in ("sq", "ss", "qn", "qg", "t1", "t2")},
        }
        qb = [self.alloc(256) for _ in range(2)]
        rqb = [Res("qb%d" % i) for i in range(2)]
        hT = [self.alloc(4 * S).rearrange("p (h t) -> p h t", h=4) for _ in range(2)]
        rhT = [Res("hT%d" % i) for i in range(2)]
        vst = [self.alloc(NT * 4 * 65).rearrange("p (t h e) -> p t h e", t=NT, h=4) for _ in range(1)]
        rvs = [Res("vst%d" % i) for i in range(1)]
        for i in range(1):
            self.memset("pool", vst[i][:, :, :, 64:65], 1.0, [rvs[i]])
        blocks = [("q", "qt_a", 0, ("qk_norm_a", 0), False), ("q", "qt_a", 4, ("qk_norm_a", 0), False), ("q", "qt_a", 8, ("qk_norm_a", 0), False),
                  ("q", "kt_a", 0, ("qk_norm_a", 1), False), ("v", "v_a", 0, None, False),
                  ("q", "qt_b", 0, ("qk_norm_b", 0), False), ("q", "qt_b", 4, ("qk_norm_b", 0), False),
                  ("q", "kt_b", 0, ("qk_norm_b", 1), False), ("q", "kt_b", 4, ("qk_norm_b", 1), False),
                  ("v", "v_b", 0, None, False), ("v", "v_b", 4, None, False),
                  ("q", "qt_c", 0, ("qk_norm_c", 0), True), ("q", "qt_c", 4, ("qk_norm_c", 0), True), ("q", "qt_c", 8, ("qk_norm_c", 0), True),
                  ("q", "kt_c", 0, ("qk_norm_c", 1), True), ("v", "v_c", 0, None, False)]
        cnt = {"q": 0, "v": 0, "e": 0}

        def epi(nb, t, ps, rps):
            kind, dst, h0, gk, rope = blocks[nb]
            if kind == "v":
                vb = 0
                cnt["v"] += 1
                self.cp("act", vst[vb][:, t, :, 0:64], ps.rearrange("p (h d) -> p h d", h=4), [rps], [rvs[vb]])
                if t == NT - 1:
                    dv = Sx[dst].rearrange("(t p) h e -> p t h e", p=128)[:, :, h0:h0 + 4, :]
                    self.dma(dv, vst[vb], [rvs[vb]], [], eng="pool")
                return
            hb = (cnt["q"] // NT) % 2
            cnt["q"] += 1
            e = cnt["e"] % 2
            cnt["e"] += 1
            gain = self.g[(gk[0], l, gk[1])]
            gb = self.headnorm(ps, rps, 4, 64, gain, sc)
            r = sc["r"]
            qn3 = sc["qn"].rearrange("p (h d) -> p h d", h=4)
            if not rope:
                self.tt("dve", qb[e].rearrange("p (h d) -> p h d", h=4), qn3, gb, ALU.mult, [r["qn"], self.rconst], [rqb[e]])
            else:
                qg = sc["qg"]
                self.tt("dve", qg.rearrange("p (h d) -> p h d", h=4), qn3, gb, ALU.mult, [r["qn"], self.rconst], [r["qg"]])
                cc = self.ropec[:, t, :].unsqueeze(1).to_broadcast([128, 4, 64])
                self.tt("dve", sc["t1"].rearrange("p (h d) -> p h d", h=4), qg.rearrange("p (h d) -> p h d", h=4), cc, ALU.mult,
                        [r["qg"], self.rconst], [r["t1"]])
                qg5 = qg.rearrange("p (a x f) -> p a x f", x=2, f=16)
                t25 = sc["t2"].rearrange("p (a x f) -> p a x f", x=2, f=16)
                ss5 = self.ropes[:, t, :].rearrange("p (a x f) -> p a x f", x=2, f=16)
                for x in range(2):
                    src = qg5[:, :, 1 - x, :].rearrange("p (h a) f -> p h a f", h=4)
                    dst_ = t25[:, :, x, :].rearrange("p (h a) f -> p h a f", h=4)
                    sb = ss5[:, :, x, :].unsqueeze(1).to_broadcast([128, 4, 2, 16])
                    self.tt("dve", dst_, src, sb, ALU.mult, [r["qg"], self.rconst], [r["t2"]])
                self.tt("dve", qb[e], sc["t1"], sc["t2"], ALU.add, [r["t1"], r["t2"]], [rqb[e]])
            pb = 4 + e
            pv = self.ps[pb][:, 0:256].bitcast(BF16)
            for j in range(4):
                self.tr(pv[0:64, j * 128:(j + 1) * 128], qb[e][:, j * 64:(j + 1) * 64], [rqb[e]], [self.rps[pb]])
            self.cp("act", hT[hb][0:64, :, t * 128:(t + 1) * 128], pv[0:64, :].rearrange("p (j q) -> p j q", j=4),
                    [self.rps[pb]], [rhT[hb]])
            if t == NT - 1:
                dv = Sx[dst][h0:h0 + 4].rearrange("h d t -> d h t")
                self.dma(dv, hT[hb][0:64], [rhT[hb]], [], eng="pool")

        self.linear(xnT, rxn, NT, I["w_in"][l], KC, 4096, 256, self.gcol[:, l, 0, :], epi)
        self.top = m
        self.P.barrier()

    def outnorm_store(self, o_f32, n, parts, dst, sc):
        r = sc["r"]
        self.act(sc["junk"][0:parts, 0:n], o_f32, AF.Square, [r["o"]], [r["junk"], r["st"]], accum=sc["st"][0:parts, 0:1])
        self.rstd_from_ssq(sc["st"][0:parts], n, r["st"])
        self.act(sc["ob"][0:parts, 0:n], o_f32, AF.Copy, [r["o"], r["st"]], [r["ob"]], scale=sc["st"][0:parts, 3:4])
        self.dma(dst, sc["ob"][0:parts, 0:n], [r["ob"]], [], eng="pool")

    def mixer_AC(self, l, which):
        I, Sx = self.I, self.Sx
        m = self.top
        qn_, kn_, vn_ = ("qt_a", "kt_a", "v_a") if which == "a" else ("qt_c", "kt_c", "v_c")
        QT = self.alloc(12 * S).rearrange("p (h t) -> p h t", h=12)
        KT = self.alloc(4 * S).rearrange("p (h t) -> p h t", h=4)
        V = self.alloc(NT * 4 * 65).rearrange("p (t h e) -> p t h e", t=NT, h=4)
        rq, rk, rv = Res("QT"), Res("KT"), Res("V")
        self.dma(QT[0:64], Sx[qn_].rearrange("h d t -> d h t"), [], [rq])
        self.dma(KT[0:64], Sx[kn_].rearrange("h d t -> d h t"), [], [rk])
        self.dma(V, Sx[vn_].rearrange("(t p) h e -> p t h e", p=128), [], [rv])
        NR = 2
        Pe = [self.alloc(384) for _ in range(NR)]
        Pm = [self.alloc(384) for _ in range(NR)]
        rpe = [Res("pe%d" % i) for i in range(NR)]
        rpm = [Res("pm%d" % i) for i in range(NR)]
        scs = []
        for i in range(2):
            scs.append({"den": self.alloc(16, F32), "rden": self.alloc(16, F32), "o": self.alloc(768, F32), "junk": self.alloc(768),
                        "st": self.alloc(8, F32), "ob": self.alloc(768),
                        "r": {k: Res(k + str(i)) for k in ("den", "o", "junk", "st", "ob")}})
        rot = 0
        col0 = 0 if which == "a" else 1280
        for qb in range(NT):
            ob_ = qb % 2
            pso = [self.ps[2 + 3 * ob_ + g] for g in range(3)]
            rpo = [self.rps[2 + 3 * ob_ + g] for g in range(3)]
            kts = [kt for kt in ((qb - 1, qb, qb + 1) if which == "a" else range(NT)) if 0 <= kt < NT]
            for hk in range(4):
                for idx, kt in enumerate(kts):
                    s_ = rot % NR
                    rot += 1
                    ps = self.ps[s_][:, 0:384]
                    self.mm(ps, KT[0:64, hk, kt * 128:(kt + 1) * 128], QT[0:64, 3 * hk:3 * hk + 3, qb * 128:(qb + 1) * 128],
                            True, True, [rk, rq], [self.rps[s_]])
                    self.act(Pe[s_], ps, AF.Exp, [self.rps[s_]], [rpe[s_]], scale=0.125)
                    if which == "a":
                        ri = kt - qb + 1
                        self.tt("dve", Pm[s_].rearrange("p (g q) -> p g q", g=3), Pe[s_].rearrange("p (g q) -> p g q", g=3),
                                self.eba[:, ri, 3 * hk:3 * hk + 3, :], ALU.mult, [rpe[s_], self.rconst], [rpm[s_]])
                        pp, rpp = Pm[s_], rpm[s_]
                    else:
                        pp, rpp = Pe[s_], rpe[s_]
                    for g in range(3):
                        self.mm(pso[g][:, hk * 65:hk * 65 + 65], pp[:, g * 128:(g + 1) * 128], V[:, kt, hk, :],
                                idx == 0, idx == len(kts) - 1, [rpp, rv], [rpo[g]])
            sc = scs[ob_]
            r = sc["r"]
            den3 = sc["den"][:, 0:12].rearrange("p (k g) -> p k g", g=3)
            rden3 = sc["rden"][:, 0:12].rearrange("p (k g) -> p k g", g=3)
            es3 = self.esink[:, l * 12:(l + 1) * 12].rearrange("p (k g) -> p k g", g=3)
            o4 = sc["o"].rearrange("p (k g d) -> p k g d", k=4, g=3)
            for g in range(3):
                o3 = pso[g][:, 0:260].rearrange("p (k e) -> p k e", k=4)
                if which == "a":
                    self.tt("dve", den3[:, :, g], o3[:, :, 64], es3[:, :, g], ALU.add, [rpo[g], self.rconst], [r["den"]])
                else:
                    self.cp("dve", den3[:, :, g], o3[:, :, 64], [rpo[g]], [r["den"]])
            self.recip(sc["rden"][:, 0:12], sc["den"][:, 0:12], [r["den"]], [r["den"]])
            for g in range(3):
                o3 = pso[g][:, 0:260].rearrange("p (k e) -> p k e", k=4)
                rb = rden3[:, :, g].unsqueeze(2).to_broadcast([128, 4, 64])
                self.tt("dve", o4[:, :, g, :], o3[:, :, 0:64], rb, ALU.mult, [rpo[g], r["den"]], [r["o"]])
            self.outnorm_store(sc["o"], 768, 128, Sx["mixed"][qb * 128:(qb + 1) * 128, col0:col0 + 768], sc)
        self.top = m
        self.P.barrier()

    def mixer_B(self, l):
        I, Sx = self.I, self.Sx
        m = self.top
        QT = self.alloc(8 * S).rearrange("p (h t) -> p h t", h=8)
        KT = self.alloc(8 * S).rearrange("p (h t) -> p h t", h=8)
        rq, rk = Res("QTb"), Res("KTb")
        self.dma(QT[0:64], Sx["qt_b"].rearrange("h d t -> d h t"), [], [rq])
        self.dma(KT[0:64], Sx["kt_b"].rearrange("h d t -> d h t"), [], [rk])
        Vr = [self.alloc(4 * 8 * 65).rearrange("p (i h e) -> p i h e", i=4, h=8) for _ in range(2)]
        rvr = [Res("vr%d" % i) for i in range(2)]
        NR = 3
        Pe = [self.alloc(512) for _ in range(NR)]
        Pm = [self.alloc(512) for _ in range(NR)]
        rpe = [Res("peb%d" % i) for i in range(NR)]
        rpm = [Res("pmb%d" % i) for i in range(NR)]
        scs = []
        for i in range(2):
            scs.append({"rden": self.alloc(16, F32), "o": self.alloc(512, F32), "junk": self.alloc(512), "st": self.alloc(8, F32),
                        "ob": self.alloc(512), "r": {k: Res(k + "b" + str(i)) for k in ("den", "o", "junk", "st", "ob")}})
        vbt = Sx["v_b"].tensor
        rot = 0
        for r_ in range(32):
            r0 = min(max(r_ - 4, 0), 24)
            s_ = r_ - r0
            vb = r_ % 2
            src = bass.AP(vbt, 64 * r0 * 520, [[520, 128], [128 * 520, 4], [1, 520]])
            self.dma(Vr[vb].rearrange("p i h e -> p i (h e)"), src, [], [rvr[vb]])
            ob_ = r_ % 2
            pso = [self.ps[3 + 2 * ob_], self.ps[4 + 2 * ob_]]
            rpo = [self.rps[3 + 2 * ob_], self.rps[4 + 2 * ob_]]
            for hp in range(4):
                sl = rot % NR
                rot += 1
                ps = self.ps[sl][:, 0:512].rearrange("p (a i q) -> p a i q", a=2, i=4)
                for hh in range(2):
                    h = 2 * hp + hh
                    for i in range(4):
                        k0 = 64 * r0 + 128 * i
                        self.mm(ps[:, hh, i, :], KT[0:64, h, k0:k0 + 128], QT[0:64, h, 64 * r_:64 * r_ + 64], True, True,
                                [rk, rq], [self.rps[sl]])
                self.act(Pe[sl], self.ps[sl][:, 0:512], AF.Exp, [self.rps[sl]], [rpe[sl]], scale=0.125)
                eb = self.ebb[:, 2 * hp:2 * hp + 2, 7 - s_:7 - s_ + 7:2, :]
                self.tt("dve", Pm[sl].rearrange("p (a i q) -> p a i q", a=2, i=4), Pe[sl].rearrange("p (a i q) -> p a i q", a=2, i=4), eb,
                        ALU.mult, [rpe[sl], self.rconst], [rpm[sl]])
                pm4 = Pm[sl].rearrange("p (a i q) -> p a i q", a=2, i=4)
                for hh in range(2):
                    h = 2 * hp + hh
                    for i in range(4):
                        self.mm(pso[h // 4][0:64, (h % 4) * 65:(h % 4) * 65 + 65], pm4[:, hh, i, :], Vr[vb][:, i, h, :], i == 0, i == 3,
                                [rpm[sl], rvr[vb]], [rpo[h // 4]])
            sc = scs[ob_]
            r = sc["r"]
            for b in range(2):
                o3 = pso[b][0:64, 0:260].rearrange("p (h e) -> p h e", h=4)
                self.recip(sc["rden"][0:64, 4 * b:4 * b + 4], o3[:, :, 64], [rpo[b]], [r["den"]])
            for b in range(2):
                o3 = pso[b][0:64, 0:260].rearrange("p (h e) -> p h e", h=4)
                rb = sc["rden"][0:64, 4 * b:4 * b + 4].unsqueeze(2).to_broadcast([64, 4, 64])
                self.tt("dve", sc["o"][0:64, 256 * b:256 * b + 256].rearrange("p (h d) -> p h d", h=4), o3[:, :, 0:64], rb, ALU.mult,
                        [rpo[b], r["den"]], [r["o"]])
            self.outnorm_store(sc["o"][0:64], 512, 64, Sx["mixed"][64 * r_:64 * r_ + 64, 768:1280], sc)
        self.top = m
        self.P.barrier()

    def residual_epilogue(self, xsrc, xdst, nblk, rrow):
        m_ = {}
        xt = [self.alloc(nblk, F32) for _ in range(3)]
        rxt = [Res("rxt%d" % i) for i in range(3)]
        cnt = [0]

        def epi(nb, t, ps, rps):
            b = cnt[0] % 3
            cnt[0] += 1
            self.dma(xt[b], xsrc[t * 128:(t + 1) * 128, nb * nblk:(nb + 1) * nblk], [rrow[t]] if rrow else [], [rxt[b]])
            self.tt("dve", xt[b], ps, xt[b], ALU.add, [rps, rxt[b]], [rxt[b]])
            self.dma(xdst[t * 128:(t + 1) * 128, nb * nblk:(nb + 1) * nblk], xt[b], [rxt[b]], [rrow[t]] if rrow else [], eng="pool")

        return epi

    def phase_C(self, l, xsrc, xdst, inplace):
        I, Sx = self.I, self.Sx
        m = self.top
        mixT = self.alloc(KC * S).rearrange("p (k t) -> p k t", k=KC)
        rmx = [Res("mixT%d" % t) for t in range(NT)]
        mt = [self.alloc(D) for _ in range(2)]
        rmt = [Res("mt%d" % i) for i in range(2)]
        for t in range(NT):
            b = t % 2
            self.dma(mt[b], Sx["mixed"][t * 128:(t + 1) * 128, :], [], [rmt[b]])
            for g in range(4):
                pb = g % 2
                pv = self.ps[pb][:, 0:256].bitcast(BF16)
                for j in range(4):
                    kc = g * 4 + j
                    self.tr(pv[:, j * 128:(j + 1) * 128], mt[b][:, kc * 128:(kc + 1) * 128], [rmt[b]], [self.rps[pb]])
                self.cp("dve" if g % 2 == 0 else "act", mixT[:, g * 4:(g + 1) * 4, t * 128:(t + 1) * 128],
                        pv.rearrange("p (j q) -> p j q", j=4), [self.rps[pb]], [rmx[t]])
        rrow = [Res("row%d" % t) for t in range(NT)] if inplace else None
        epi = self.residual_epilogue(xsrc, xdst, 256, rrow)
        self.linear(mixT, rmx, NT, I["w_out"][l], KC, D, 256, self.gcol[:, l, 1, :], epi)
        self.top = m
        self.P.barrier()

    def phase_D(self, l, s, xio):
        I, Sx = self.I, self.Sx
        m = self.top
        memT = self.alloc(KC * NMEM).rearrange("p (k t) -> p k t", k=KC)
        rmemT = [Res("memT%d" % t) for t in range(2)]
        msrc = I["mem"][s]
        self.norm_transpose(lambda t: msrc[t * 128:(t + 1) * 128, :], 2, memT, rmemT)
        KmT = self.alloc(4 * NMEM).rearrange("p (h t) -> p h t", h=4)
        Vm = self.alloc(2 * 4 * 129).rearrange("p (t h e) -> p t h e", t=2, h=4)
        rkm, rvm = Res("KmT"), Res("Vm")
        self.memset("pool", Vm[:, :, :, 128:129], 1.0, [rvm])
        sc = {"sq": self.alloc(512, F32), "ss": self.alloc(32, F32), "qn": self.alloc(512, F32), "qg": None,
              "r": {k: Res(k + "d") for k in ("sq", "ss", "qn")}}
        qb = [self.alloc(512) for _ in range(2)]
        rqb = [Res("qbd%d" % i) for i in range(2)]
        cnt = [0]

        def epi_kv(nb, t, ps, rps):
            if nb == 1:
                self.cp("act", Vm[:, t, :, 0:128], ps.rearrange("p (h d) -> p h d", h=4), [rps], [rvm])
                return
            e = cnt[0] % 2
            cnt[0] += 1
            gb = self.headnorm(ps, rps, 4, 128, self.g[("qk_norm_mem", l, 1)], sc)
            self.tt("dve", qb[e].rearrange("p (h d) -> p h d", h=4), sc["qn"].rearrange("p (h d) -> p h d", h=4), gb, ALU.mult,
                    [sc["r"]["qn"], self.rconst], [rqb[e]])
            pb = 4 + e
            pv = self.ps[pb][:, 0:256].bitcast(BF16)
            for j in range(4):
                self.tr(pv[:, j * 128:(j + 1) * 128], qb[e][:, j * 128:(j + 1) * 128], [rqb[e]], [self.rps[pb]])
            self.cp("act", KmT[:, :, t * 128:(t + 1) * 128], pv.rearrange("p (j q) -> p j q", j=4), [self.rps[pb]], [rkm])

        self.linear(memT, rmemT, 2, I["w_mem_kv"][l], KC, 1024, 512, self.gcol[:, l, 3, :], epi_kv)
        QmT = self.alloc(4 * S).rearrange("p (h t) -> p h t", h=4)
        rqm = [Res("QmT%d" % t) for t in range(NT)]
        mD2 = self.top
        xnT = self.alloc(KC * S).rearrange("p (k t) -> p k t", k=KC)
        rxn = [Res("xnTd%d" % t) for t in range(NT)]
        rrow = [Res("rowd%d" % t) for t in range(NT)]
        self.norm_transpose(lambda t: xio[t * 128:(t + 1) * 128, :], NT, xnT, rxn, rsrc=rrow)

        def epi_q(nb, t, ps, rps):
            e = cnt[0] % 2
            cnt[0] += 1
            gb = self.headnorm(ps, rps, 2, 128, self.g[("qk_norm_mem", l, 0)], sc)
            self.tt("dve", qb[e][:, 0:256].rearrange("p (h d) -> p h d", h=2), sc["qn"][:, 0:256].rearrange("p (h d) -> p h d", h=2), gb, ALU.mult,
                    [sc["r"]["qn"], self.rconst], [rqb[e]])
            pb = 4 + e
            pv = self.ps[pb][:, 0:256].bitcast(BF16)
            for j in range(2):
                self.tr(pv[:, j * 128:(j + 1) * 128], qb[e][:, j * 128:(j + 1) * 128], [rqb[e]], [self.rps[pb]])
            self.cp("act", QmT[:, 2 * nb:2 * nb + 2, t * 128:(t + 1) * 128], pv[:, 0:256].rearrange("p (j q) -> p j q", j=2), [self.rps[pb]], [rqm[t]])

        self.linear(xnT, rxn, NT, I["w_mem_q"][l], KC, 512, 256, self.gcol[:, l, 2, :], epi_q)
        self.top = mD2
        wo_st = self.alloc(4 * 512, F32).rearrange("p (k n) -> p k n", k=4)
        wo = self.alloc(4 * D).rearrange("p (k n) -> p k n", k=4)
        rwst, rwo = Res("wost"), Res("wo")
        wov = I["w_mem_o"][l].rearrange("(k p) n -> p k n", p=128)
        for nb in range(4):
            self.dma(wo_st, wov[:, :, nb * 512:(nb + 1) * 512], [], [rwst])
            self.cp("pool" if nb % 2 else "act", wo[:, :, nb * 512:(nb + 1) * 512], wo_st, [rwst], [rwo])
        Pe = [self.alloc(1024) for _ in range(2)]
        rpe = [Res("ped%d" % i) for i in range(2)]
        rden = [self.alloc(8, F32) for _ in range(2)]
        om = [self.alloc(512) for _ in range(2)]
        omT = [self.alloc(512).rearrange("p (h t) -> p h t", h=4) for _ in range(2)]
        xt = [self.alloc(D, F32) for _ in range(2)]
        rrd = [Res("rdend%d" % i) for i in range(2)]
        rom = [Res("om%d" % i) for i in range(2)]
        romT = [Res("omT%d" % i) for i in range(2)]
        rxt = [Res("xtd%d" % i) for i in range(2)]
        scale = 128.0 ** -0.5
        for t in range(NT):
            b = t % 2
            self.dma(xt[b], xio[t * 128:(t + 1) * 128, :], [rrow[t]], [rxt[b]])
            for mt_ in range(2):
                for h in range(4):
                    self.mm(self.ps[mt_][:, h * 128:(h + 1) * 128], KmT[:, h, mt_ * 128:(mt_ + 1) * 128], QmT[:, h, t * 128:(t + 1) * 128],
                            True, True, [rkm, rqm[t]], [self.rps[mt_]])
                self.act(Pe[b][:, mt_ * 512:(mt_ + 1) * 512], self.ps[mt_][:, 0:512], AF.Exp, [self.rps[mt_]], [rpe[b]], scale=scale)
            for h in range(4):
                pb, off = (2, h * 129) if h < 3 else (3, 0)
                for mt_ in range(2):
                    self.mm(self.ps[pb][:, off:off + 129], Pe[b][:, mt_ * 512 + h * 128:mt_ * 512 + (h + 1) * 128], Vm[:, mt_, h, :],
                            mt_ == 0, mt_ == 1, [rpe[b], rvm], [self.rps[pb]])
            o3 = self.ps[2][:, 0:387].rearrange("p (h e) -> p h e", h=3)
            self.recip(rden[b][:, 0:3], o3[:, :, 128], [self.rps[2]], [rrd[b]])
            self.recip(rden[b][:, 3:4], self.ps[3][:, 128:129], [self.rps[3]], [rrd[b]])
            self.tt("dve", om[b][:, 0:384].rearrange("p (h d) -> p h d", h=3), o3[:, :, 0:128],
                    rden[b][:, 0:3].unsqueeze(2).to_broadcast([128, 3, 128]), ALU.mult, [self.rps[2], rrd[b]], [rom[b]])
            self.ts("dve", om[b][:, 384:512], self.ps[3][:, 0:128], rden[b][:, 3:4], None, ALU.mult, None, [self.rps[3], rrd[b]], [rom[b]])
            pv = self.ps[4][:, 0:256].bitcast(BF16)
            for j in range(4):
                self.tr(pv[:, j * 128:(j + 1) * 128], om[b][:, j * 128:(j + 1) * 128], [rom[b]], [self.rps[4]])
            self.cp("act", omT[b], pv.rearrange("p (j q) -> p j q", j=4), [self.rps[4]], [romT[b]])
            for nb in range(4):
                pb = 5 + nb % 2
                for k in range(4):
                    self.mm(self.ps[pb][:, 0:512], omT[b][:, k, :], wo[:, k, nb * 512:(nb + 1) * 512], k == 0, k == 3,
                            [romT[b], rwo], [self.rps[pb]])
                self.tt("dve", xt[b][:, nb * 512:(nb + 1) * 512], self.ps[pb][:, 0:512], xt[b][:, nb * 512:(nb + 1) * 512], ALU.add,
                        [self.rps[pb], rxt[b]], [rxt[b]])
            self.dma(xio[t * 128:(t + 1) * 128, :], xt[b], [rxt[b]], [rrow[t]], eng="pool")
        self.top = m
        self.P.barrier()

    def phase_E(self, l, xio):
        I, Sx = self.I, self.Sx
        m = self.top
        TP = 512
        NTP = 4
        keysT = self.alloc(16 * 128).rearrange("p (a n) -> p a n", a=16)
        rkt = Res("keysT")
        m1 = self.top
        kst = self.alloc(16 * 128, F32).rearrange("p (a n) -> p a n", a=16)
        kbf = self.alloc(16 * 128).rearrange("p (a n) -> p a n", a=16)
        rks, rkb = Res("kst"), Res("kbf")
        self.dma(kst, I["peer_keys"][l].rearrange("h c n d -> n (h c) d"), [], [rks])
        self.cp("dve", kbf, kst, [rks], [rkb])
        for g in range(4):
            pv = self.ps[g % 2][:, 0:256].bitcast(BF16)
            for j in range(4):
                self.tr(pv[:, j * 128:(j + 1) * 128], kbf[:, g * 4 + j, :], [rkb], [self.rps[g % 2]])
            self.cp("act", keysT[:, g * 4:(g + 1) * 4, :], pv.rearrange("p (j q) -> p j q", j=4), [self.rps[g % 2]], [rkt])
        self.P.barrier()
        gcol = self.gcol[:, l, 4, :]
        for p_ in range(S // TP):
            self.top = m1
            rows = lambda t: xio[p_ * TP + t * 128:p_ * TP + (t + 1) * 128, :]
            hnT = self.alloc(KC * TP).rearrange("p (k t) -> p k t", k=KC)
            rhn = [Res("hnT%d" % t) for t in range(NTP)]
            s1b = self.alloc(NTP * 8 * 128, F32).rearrange("p (t h n) -> p t h n", t=NTP, h=8)
            phi = self.alloc(NTP * 128 * 8, F32).rearrange("p (t c h) -> p t c h", t=NTP, c=128)
            Dm = self.alloc(NTP * 8 * 128).rearrange("p (t h q) -> p t h q", t=NTP, h=8)
            ytok = self.alloc(NTP * D, F32).rearrange("p (t d) -> p t d", t=NTP)
            rs1 = [Res("s1b%d" % t) for t in range(NTP)]
            rphi = [Res("phi%d" % t) for t in range(NTP)]
            rdm = [Res("Dm%d" % t) for t in range(NTP)]
            ryt = [[Res("yt%d_%d" % (t, d)) for d in range(4)] for t in range(NTP)]
            m2 = self.top
            self.norm_transpose(rows, NTP, hnT, rhn)
            qT = self.alloc(16 * TP).rearrange("p (a t) -> p a t", a=16)
            rqT = [Res("qT%d" % a) for a in range(16)]
            wst = [self.alloc(KC * 128, F32).rearrange("p (k n) -> p k n", k=KC) for _ in range(2)]
            wbf = [self.alloc(KC * 128).rearrange("p (k n) -> p k n", k=KC) for _ in range(2)]
            rst = [Res("pwst%d" % i) for i in range(2)]
            rwb = [[Res("pwbf%d_%d" % (i, k)) for k in range(KC)] for i in range(2)]
            wv = I["peer_w_q"][l].rearrange("(k p) n -> p k n", p=128)
            engs = ["act", "dve", "pool"]
            for nb in range(16):
                b = nb % 2
                self.dma(wst[b], wv[:, :, nb * 128:(nb + 1) * 128], [], [rst[b]])
                for k in range(KC):
                    eng = engs[k % 3]
                    if eng == "act":
                        self.act(wbf[b][:, k, :], wst[b][:, k, :], AF.Copy, [rst[b], self.rconst], [rwb[b][k]], scale=gcol[:, k:k + 1])
                    else:
                        self.ts(eng, wbf[b][:, k, :], wst[b][:, k, :], gcol[:, k:k + 1], None, ALU.mult, None, [rst[b], self.rconst], [rwb[b][k]])
                for ch in range(1):
                    a = nb
                    pb = 2 + a % 2
                    for k in range(KC):
                        self.mm(self.ps[pb][:, 0:TP], wbf[b][:, k, ch * 128:(ch + 1) * 128], hnT[:, k, :], k == 0, k == KC - 1,
                                [rwb[b][k]] + rhn, [self.rps[pb]])
                    self.cp("act" if a % 2 else "dve", qT[:, a, :], self.ps[pb][:, 0:TP], [self.rps[pb]], [rqT[a]])
            s0t = self.alloc(8 * 128, F32).rearrange("p (h n) -> p h n", h=8)
            top = self.alloc(16 * 16, F32).rearrange("p (a k) -> p a k", a=16)
            wk = self.alloc(256, F32)
            cand = self.alloc(8 * 256, F32).rearrange("p (h a b) -> p h a b", h=8, a=16)
            best = self.alloc(8 * 16, F32).rearrange("p (h k) -> p h k", h=8)
            eb_ = self.alloc(8 * 16, F32).rearrange("p (h k) -> p h k", h=8)
            sm = self.alloc(64, F32)
            rs0, rtop, rwk, rcand, rbest, rsm = Res("s0t"), Res("top"), Res("wk"), Res("cand"), Res("best"), Res("sm")
            for t in range(NTP):
                for g in range(4):
                    pb = 4 + g % 2
                    for j in range(4):
                        a = g * 4 + j
                        self.mm(self.ps[pb][:, j * 128:(j + 1) * 128], qT[:, a, t * 128:(t + 1) * 128], keysT[:, a, :], True, True,
                                [rqT[a], rkt], [self.rps[pb]])
                    p4 = self.ps[pb][:, 0:512].rearrange("p (h c n) -> p h c n", h=2, c=2)
                    self.cp("act", s0t[:, 2 * g:2 * g + 2, :], p4[:, :, 0, :], [self.rps[pb]], [rs0])
                    self.cp("act", s1b[:, t, 2 * g:2 * g + 2, :], p4[:, :, 1, :], [self.rps[pb]], [rs1[t]])
                for a in range(16):
                    src = s0t[:, a // 2, :] if a % 2 == 0 else s1b[:, t, a // 2, :]
                    rsrc_ = rs0 if a % 2 == 0 else rs1[t]
                    self.P.op("dve", lambda e, src=src, a=a: e.max(out=top[:, a, 0:8], in_=src), [rsrc_], [rtop])
                    self.P.op("dve", lambda e, src=src, a=a: e.match_replace(out=wk[:, 0:128], in_to_replace=top[:, a, 0:8], in_values=src, imm_value=-1e30),
                              [rsrc_, rtop], [rwk])
                    self.P.op("dve", lambda e, a=a: e.max(out=top[:, a, 8:16], in_=wk[:, 0:128]), [rwk], [rtop])
                top4 = top.rearrange("p (h c) k -> p h c k", c=2)
                in0 = top4[:, :, 0, :].unsqueeze(3).to_broadcast([128, 8, 16, 16])
                in1 = top4[:, :, 1, :].unsqueeze(2).to_broadcast([128, 8, 16, 16])
                self.tt("dve", cand, in0, in1, ALU.add, [rtop], [rcand])
                for h in range(8):
                    cf = cand[:, h].rearrange("p a b -> p (a b)")
                    self.P.op("dve", lambda e, cf=cf, h=h: e.max(out=best[:, h, 0:8], in_=cf), [rcand], [rbest])
                    self.P.op("dve", lambda e, cf=cf, h=h: e.match_replace(out=wk, in_to_replace=best[:, h, 0:8], in_values=cf, imm_value=-1e30),
                              [rcand, rbest], [rwk])
                    self.P.op("dve", lambda e, h=h: e.max(out=best[:, h, 8:16], in_=wk), [rwk], [rbest])
                self.cp("dve", sm[:, 0:8], best[:, :, 0], [rbest], [rsm])
                self.ts("dve", sm[:, 8:16], best[:, :, 15], -1e-5, None, ALU.add, None, [rbest], [rsm])
                self.tt("dve", eb_, best, sm[:, 0:8].unsqueeze(2).to_broadcast([128, 8, 16]), ALU.subtract, [rbest, rsm], [rbest])
                self.act(eb_, eb_, AF.Exp, [rbest], [rbest])
                self.P.op("dve", lambda e: e.tensor_reduce(out=sm[:, 16:24], in_=eb_, axis=AX.X, op=ALU.add), [rbest], [rsm])
                self.recip(sm[:, 24:32], sm[:, 16:24], [rsm], [rsm])
                self.tt("dve", sm[:, 32:40], sm[:, 8:16], sm[:, 0:8], ALU.subtract, [rsm], [rsm])
                self.act(sm[:, 32:40], sm[:, 32:40], AF.Exp, [rsm], [rsm])
                self.tt("dve", sm[:, 40:48], sm[:, 32:40], sm[:, 24:32], ALU.mult, [rsm], [rsm])
                self.tt("dve", phi[:, t].rearrange("p c h -> p h c"), s0t, sm[:, 8:16].unsqueeze(2).to_broadcast([128, 8, 128]), ALU.subtract,
                        [rs0, rsm], [rphi[t]])
                for h in range(8):
                    eng = "pool" if h % 2 else "dve"
                    self.ts(eng, Dm[:, t, h, :], self.ident, sm[:, 40 + h:41 + h], None, ALU.mult, None, [rsm, self.rconst], [rdm[t]])
                if self.dbg and p_ == 0 and t == 0:
                    self.dma(Sx["dbg_sm"], sm, [rsm], [], eng="pool")
                    self.dma(Sx["dbg_s0"], s0t.rearrange("p h n -> p (h n)"), [rs0], [], eng="pool")
                    self.dma(Sx["dbg_s1"], s1b[:, 0].rearrange("p h n -> p (h n)"), [rs1[0]], [], eng="pool")
            self.P.barrier()
            self.top = m2
            ust = self.alloc(D, F32)
            ubf = self.alloc(D)
            uT = [self.alloc(KC * 128).rearrange("p (k e) -> p k e", k=KC) for _ in range(2)]
            vst_ = self.alloc(D, F32)
            YG = 2
            vbf = [self.alloc(YG * D).rearrange("p (c d) -> p c d", c=YG) for _ in range(2)]
            wT = [self.alloc(YG * TP).rearrange("p (c t) -> p c t", c=YG) for _ in range(2)]
            zz = [self.alloc(1024, F32) for _ in range(2)]
            E_ = [self.alloc(1024) for _ in range(2)]
            Gm = [self.alloc(1024) for _ in range(2)]
            ge = [self.alloc(TP) for _ in range(2)]
            rust, rubf, rvst = Res("ust"), Res("ubf"), Res("vst")
            ruT = [[Res("uT%d_%d" % (i, k)) for k in range(KC)] for i in range(2)]
            rvbf = [[Res("vbf%d_%d" % (i, c)) for c in range(YG)] for i in range(2)]
            rwT = [[Res("wT%d_%d" % (i, c)) for c in range(YG)] for i in range(2)]
            rzz = [Res("zz%d" % i) for i in range(2)]
            rE = [Res("E%d" % i) for i in range(2)]
            rGm = [Res("Gm%d" % i) for i in range(2)]
            rge = [Res("ge%d" % i) for i in range(2)]
            uv = I["peer_u"][l]
            vv = I["peer_v"][l]
            zi = 0
            for c in range(128):
                ub = c % 2
                yg, yc = (c // YG) % 2, c % YG
                self.dma(ust, uv[c * 128:(c + 1) * 128, :], [], [rust])
                self.cp("pool", ubf, ust, [rust], [rubf])
                for g in range(4):
                    pv = self.ps[0][:, 0:256].bitcast(BF16) if g % 2 == 0 else self.ps[0][:, 256:512].bitcast(BF16)
                    for j in range(4):
                        k = g * 4 + j
                        self.tr(pv[:, j * 128:(j + 1) * 128], ubf[:, k * 128:(k + 1) * 128], [rubf], [self.rps[0]])
                    for j in range(4):
                        k = g * 4 + j
                        self.act(uT[ub][:, k, :], pv[:, j * 128:(j + 1) * 128], AF.Copy, [self.rps[0], self.rconst], [ruT[ub][k]],
                                 scale=gcol[:, k:k + 1])
                self.dma(vst_, vv[c * 128:(c + 1) * 128, :], [], [rvst])
                self.cp("pool", vbf[yg][:, yc, :], vst_, [rvst], [rvbf[yg][yc]])
                pb = 1 + c % 2
                for k in range(KC):
                    self.mm(self.ps[pb][:, 0:TP], uT[ub][:, k, :], hnT[:, k, :], k == 0, k == KC - 1, [ruT[ub][k]] + rhn, [self.rps[pb]])
                self.act(ge[c % 2], self.ps[pb][:, 0:TP], AF.Gelu, [self.rps[pb]], [rge[c % 2]])
                gb_ = 3 + c % 2
                for t in range(NTP):
                    z = zi % 2
                    zi += 1
                    in0 = s1b[:, t]
                    in1 = phi[:, t, c, :].unsqueeze(2).to_broadcast([128, 8, 128])
                    self.tt("dve", zz[z].rearrange("p (h n) -> p h n", h=8), in0, in1, ALU.add, [rs1[t], rphi[t]], [rzz[z]])
                    self.act(E_[z], zz[z], AF.Exp, [rzz[z]], [rE[z]])
                    self.stt(Gm[z], zz[z], 0.0, E_[z], ALU.is_ge, ALU.mult, [rzz[z], rE[z]], [rGm[z]])
                    for h in range(8):
                        self.mm(self.ps[gb_][:, t * 128:(t + 1) * 128], Gm[z][:, h * 128:(h + 1) * 128], Dm[:, t, h, :], h == 0, h == 7,
                                [rGm[z], rdm[t]], [self.rps[gb_]])
                self.tt("dve", wT[yg][:, yc, :], ge[c % 2], self.ps[gb_][:, 0:TP], ALU.mult, [rge[c % 2], self.rps[gb_]], [rwT[yg][yc]])
                if self.dbg and p_ == 0 and c == 0:
                    self.dma(Sx["dbg_ge"], ge[0], [rge[0]], [], eng="pool")
                    self.dma(Sx["dbg_wt"], wT[yg][:, yc, :], [rwT[yg][yc]], [], eng="pool")
                if yc == YG - 1:
                    for t in range(NTP):
                        for d4 in range(4):
                            pb2 = 5 + (t * 4 + d4) % 3
                            for cc in range(YG):
                                self.mm(self.ps[pb2][:, 0:512], wT[yg][:, cc, t * 128:(t + 1) * 128], vbf[yg][:, cc, d4 * 512:(d4 + 1) * 512],
                                        cc == 0, cc == YG - 1, [rwT[yg][cc], rvbf[yg][cc]], [self.rps[pb2]])
                            dst = ytok[:, t, d4 * 512:(d4 + 1) * 512]
                            if c == YG - 1:
                                self.cp("act", dst, self.ps[pb2][:, 0:512], [self.rps[pb2]], [ryt[t][d4]])
                            else:
                                self.tt("dve", dst, self.ps[pb2][:, 0:512], dst, ALU.add, [self.rps[pb2], ryt[t][d4]], [ryt[t][d4]])
            if self.dbg and p_ == 0:
                self.dma(Sx["dbg_y"], ytok[:, 0, :], ryt[0], [], eng="pool")
            xt = ust
            rxt = rust
            for t in range(NTP):
                self.dma(xt, rows(t), [], [rxt])
                self.tt("pool", xt, xt, ytok[:, t, :], ALU.add, [rxt] + ryt[t], [rxt])
                self.dma(rows(t), xt, [rxt], [], eng="pool")
            self.P.barrier()
        self.top = m
        self.P.barrier()

    def build(self):
        self.setup_consts()
        ph = self.phases
        for l in self.layers:
            self.build_ebb(l)
            for s in range(self.nseq):
                xsrc = self.I["x"][s] if l == self.layers[0] else self.out[s]
                xo = self.out[s]
                if "A" in ph:
                    self.phase_A(l, xsrc)
                if "B" in ph:
                    self.mixer_AC(l, "a")
                    self.mixer_B(l)
                    self.mixer_AC(l, "c")
                if "C" in ph:
                    self.phase_C(l, xsrc, xo, l != self.layers[0])
                if "D" in ph:
                    self.phase_D(l, s, xo)
                if "E" in ph:
                    self.phase_E(l, xo)
        self.P.barrier()
        self.P.finalize(self.st)
        self.P.emit()
        self.st.close()
        return self.nc


def build_program(nseq=2, layers=(0, 1), phases="ABCDE", dbg=False, lw=L):
    nc = bass.Bass("TRN2", target_bir_lowering=False)
    kb = KB(nc, nseq, list(layers), phases, dbg, lw)
    kb.build()
    return nc, kb


def make_in_maps(inputs, nseq=2, ncores=8, small_peer=False):
    tabs = _static_tables()
    maps = []
    w = {name: np.ascontiguousarray(np.asarray(inputs[name], dtype=np.float32)) for name, _ in WEIGHT_SPECS}
    if small_peer:
        w["peer_u"] = np.ascontiguousarray(w["peer_u"][:, :128])
        w["peer_v"] = np.ascontiguousarray(w["peer_v"][:, :128])
    x = np.asarray(inputs["x"], dtype=np.float32)
    mem = np.asarray(inputs["mem"], dtype=np.float32)
    for c in range(ncores):
        m = dict(w)
        m.update(tabs)
        m["x"] = np.ascontiguousarray(x[2 * c:2 * c + nseq])
        m["mem"] = np.ascontiguousarray(mem[2 * c:2 * c + nseq])
        maps.append(m)
    return maps


def kernel(**inputs):
    nc, _ = build_program(nseq=1, layers=(0,), lw=1)
    tabs = _static_tables()
    w = {name: np.asarray(inputs[name], dtype=np.float32) for name, _ in WEIGHT_SPECS}
    cur = np.asarray(inputs["x"], dtype=np.float32)
    mem = np.asarray(inputs["mem"], dtype=np.float32)
    for l in range(L):
        wl = {name: (w[name] if name == "t5_rel_bias" else np.ascontiguousarray(w[name][l:l + 1])) for name in w}
        nxt = np.empty_like(cur)
        for sq in range(2):
            maps = []
            for c in range(8):
                m = dict(wl)
                m.update(tabs)
                m["x"] = np.ascontiguousarray(cur[2 * c + sq:2 * c + sq + 1])
                m["mem"] = np.ascontiguousarray(mem[2 * c + sq:2 * c + sq + 1])
                maps.append(m)
            res = run_bass_kernel_spmd(nc, maps, core_ids=list(range(8)))
            for c in range(8):
                nxt[2 * c + sq] = np.asarray(res.results[c]["out"], dtype=np.float32)[0]
        cur = nxt
    return cur
```

```python
import math
from contextlib import ExitStack

import ml_dtypes
import numpy as np

import concourse.bass as bass
import concourse.mybir as mybir
from concourse.bass_utils import run_bass_kernel_spmd

F32 = mybir.dt.float32
BF16 = mybir.dt.bfloat16
ALU = mybir.AluOpType
AF = mybir.ActivationFunctionType
AX = mybir.AxisListType

D = 2048
S = 2048
NT = 16
KC = 16
L = 2
NMEM = 256
EPS = 1e-6
N_DMA_SEMS = 24
SAME_ENG_SYNC = True
ARENA = 105000


class Res:
    __slots__ = ("name", "lw", "rs")

    def __init__(self, name=""):
        self.name = name
        self.lw = None
        self.rs = []


class Op:
    __slots__ = ("eng", "fn", "deps", "signal", "sem", "val", "ndma", "clock", "gi")


class Prog:
    ENGS = ["pe", "act", "dve", "pool", "sp"]

    def __init__(self, nc):
        self.nc = nc
        self.ops = []
        self.last = {e: None for e in self.ENGS}
        self.dmas_since_barrier = []

    def op(self, eng, fn, reads=(), writes=(), ndma=0):
        o = Op()
        o.eng = eng
        o.fn = fn
        o.ndma = ndma
        o.signal = ndma > 0
        o.sem = None
        o.val = None
        o.clock = None
        o.gi = len(self.ops)
        deps = {}
        for r in reads:
            if r.lw is not None:
                deps[r.lw.gi] = r.lw
        for w in writes:
            if w.lw is not None:
                deps[w.lw.gi] = w.lw
            for x in w.rs:
                deps[x.gi] = x
        best = {}
        dl = []
        for d in deps.values():
            if d.ndma:
                dl.append(d)
                continue
            if d.eng == o.eng and (o.eng == "pe" or not SAME_ENG_SYNC):
                continue
            b = best.get(d.eng)
            if b is None or b.gi < d.gi:
                best[d.eng] = d
        for d in best.values():
            d.signal = True
            dl.append(d)
        o.deps = dl
        for r in reads:
            r.rs.append(o)
        for w in writes:
            w.lw = o
            w.rs = []
        self.ops.append(o)
        self.last[eng] = o
        if ndma:
            self.dmas_since_barrier.append(o)
        return o

    def barrier(self):
        toks = []
        for e in self.ENGS:
            if self.last[e] is not None:
                r = Res("bar_" + e)
                self.op(e, lambda eng: eng.drain(), writes=[r])
                toks.append(r)
        dm = list(self.dmas_since_barrier)
        self.dmas_since_barrier = []
        for e in self.ENGS:
            o = self.op(e, lambda eng: eng.drain(), reads=toks)
            for d in dm:
                o.deps.append(d)

    def finalize(self, stack):
        nc = self.nc
        engsem = {e: stack.enter_context(nc.semaphore("s_" + e)) for e in self.ENGS}
        dmasems = {
            e: [stack.enter_context(nc.semaphore("d_%s_%d" % (e, i))) for i in range(N_DMA_SEMS)]
            for e in ("sp", "act", "pool")
        }
        prev = {e: [None] * N_DMA_SEMS for e in dmasems}
        sval = {e: [0] * N_DMA_SEMS for e in dmasems}
        dcnt = {e: 0 for e in dmasems}
        cnt = {e: 0 for e in self.ENGS}
        clock = {e: {} for e in self.ENGS}
        lists = {e: [] for e in self.ENGS}
        for o in self.ops:
            ck = clock[o.eng]
            deps = list(o.deps)
            if o.ndma:
                slot = dcnt[o.eng] % N_DMA_SEMS
                dcnt[o.eng] += 1
                p = prev[o.eng][slot]
                if p is not None:
                    deps.append(p)
                o.sem = dmasems[o.eng][slot]
                sval[o.eng][slot] += 16 * o.ndma
                o.val = sval[o.eng][slot]
                prev[o.eng][slot] = o
            waits = {}
            for d in deps:
                key = id(d.sem)
                if ck.get(key, 0) >= d.val:
                    continue
                if key not in waits or waits[key][1] < d.val:
                    waits[key] = (d.sem, d.val)
            for d in deps:
                if d.clock:
                    for k, v in d.clock.items():
                        if ck.get(k, 0) < v:
                            ck[k] = v
            for key, (s, v) in waits.items():
                if ck.get(key, 0) < v:
                    ck[key] = v
            wl = list(waits.values())
            if o.ndma == 0 and o.signal:
                cnt[o.eng] += 1
                o.sem = engsem[o.eng]
                o.val = cnt[o.eng]
            if o.signal:
                c = dict(ck)
                if o.ndma == 0:
                    c[id(o.sem)] = o.val
                o.clock = c
            lists[o.eng].append((wl, o))
        self.lists = lists

    def emit(self):
        nc = self.nc
        lists = self.lists

        def run(eng, items):
            for wl, o in items:
                for s, v in wl:
                    eng.wait_ge(s, v)
                if o.ndma:
                    o.fn(eng, o.sem)
                else:
                    ins = o.fn(eng)
                    if o.signal:
                        ins.then_inc(o.sem, 1)

        with nc.Block() as block:
            @block.tensor
            def _(e):
                run(e, lists["pe"])

            @block.scalar
            def _(e):
                run(e, lists["act"])

            @block.vector
            def _(e):
                run(e, lists["dve"])

            @block.gpsimd
            def _(e):
                run(e, lists["pool"])

            @block.sync
            def _(e):
                run(e, lists["sp"])


def _t5_buckets(rel):
    nb = 16
    max_exact = nb // 2
    n = np.abs(rel)
    large = max_exact + (np.log(np.maximum(n, 1) / max_exact) / np.log(128 / max_exact) * (nb - max_exact)).astype(np.int64)
    large = np.minimum(large, nb - 1)
    return np.where(rel > 0, nb, 0) + np.where(n < max_exact, n, large)


def _static_tables():
    t = {}
    t["c_ident"] = np.eye(128, dtype=np.float32).astype(ml_dtypes.bfloat16)
    t["c_j"] = np.ascontiguousarray(np.eye(128, dtype=np.float32)[::-1])
    delta = np.arange(512) - 255
    bk = _t5_buckets(delta)
    oh = np.zeros((32, 512), np.float32)
    oh[bk, np.arange(512)] = 1.0
    t["c_oh"] = oh
    k = np.arange(128)[:, None, None]
    ri = np.arange(3)[None, :, None]
    q = np.arange(128)[None, None, :]
    dd = k + 128 * (ri - 1) - q
    t["c_maska"] = (np.abs(dd) <= 128).astype(np.float32).astype(ml_dtypes.bfloat16)
    kc = np.arange(64)[:, None]
    qc = np.arange(64)[None, :]
    cs = np.clip(qc - 8, 0, 48)
    cv = ((kc >= cs) & (kc < cs + 16)).astype(np.float32)
    t["c_colv"] = np.concatenate([cv, cv], axis=0).astype(ml_dtypes.bfloat16)
    tok = np.arange(S)
    row = (tok // 64).astype(np.float32)
    col = (tok % 64).astype(np.float32)
    inv = (10000.0 ** (-np.arange(16) * 2.0 / 32)).astype(np.float32)
    ar = row[:, None] * inv[None, :]
    ac = col[:, None] * inv[None, :]
    cc = np.concatenate([np.cos(ar), np.cos(ar), np.cos(ac), np.cos(ac)], axis=1)
    ss = np.concatenate([-np.sin(ar), np.sin(ar), -np.sin(ac), np.sin(ac)], axis=1)
    t["c_ropec"] = cc.astype(np.float32)
    t["c_ropes"] = ss.astype(np.float32)
    t["c_zero"] = np.zeros((8, 1152), np.float32)
    return t


WEIGHT_SPECS = [
    ("t5_rel_bias", (32, 12)), ("norm_mix", (L, D)), ("w_in", (L, D, 4096)), ("qk_norm_a", (L, 2, 64)),
    ("sink_a", (L, 12)), ("qk_norm_b", (L, 2, 64)), ("rpb_b", (L, 8, 15, 31)), ("qk_norm_c", (L, 2, 64)),
    ("out_norm", (L, D)), ("w_out", (L, D, D)), ("norm_mem", (L, D)), ("norm_mem_kv", (L, D)),
    ("w_mem_q", (L, D, 512)), ("w_mem_kv", (L, D, 1024)), ("qk_norm_mem", (L, 2, 128)), ("w_mem_o", (L, 512, D)),
    ("norm_ffn", (L, D)), ("peer_w_q", (L, D, D)), ("peer_keys", (L, 8, 2, 128, 128)),
    ("peer_u", (L, 16384, D)), ("peer_v", (L, 16384, D)),
]


class KB:
    def __init__(self, nc, nseq, layers, phases, dbg, lw=L):
        self.nc = nc
        self.lw = lw
        self.P = Prog(nc)
        self.nseq = nseq
        self.layers = layers
        self.phases = phases
        self.dbg = dbg
        self.st = ExitStack()
        st = self.st
        self.I = {}
        self.I["x"] = nc.dram_tensor("x", [nseq, S, D], F32, kind="ExternalInput").ap()
        self.I["mem"] = nc.dram_tensor("mem", [nseq, NMEM, D], F32, kind="ExternalInput").ap()
        for name, shp in WEIGHT_SPECS:
            if name != "t5_rel_bias":
                shp = (lw,) + tuple(shp[1:])
            if name in ("peer_u", "peer_v") and "E" not in phases:
                shp = (lw, 128, D)
            self.I[name] = nc.dram_tensor(name, list(shp), F32, kind="ExternalInput").ap()
        for name, arr in _static_tables().items():
            dt = BF16 if arr.dtype == ml_dtypes.bfloat16 else F32
            self.I[name] = nc.dram_tensor(name, list(arr.shape), dt, kind="ExternalInput").ap()
        self.out = nc.dram_tensor("out", [nseq, S, D], F32, kind="ExternalOutput").ap()
        kind = "ExternalOutput" if dbg else "Internal"
        self.Sx = {}

        def scr(name, shp, dt):
            self.Sx[name] = nc.dram_tensor(name, list(shp), dt, kind=kind).ap()

        scr("qt_a", [12, 64, S], BF16)
        scr("kt_a", [4, 64, S], BF16)
        scr("v_a", [S, 4, 65], BF16)
        scr("qt_b", [8, 64, S], BF16)
        scr("kt_b", [8, 64, S], BF16)
        scr("v_b", [S, 8, 65], BF16)
        scr("qt_c", [12, 64, S], BF16)
        scr("kt_c", [4, 64, S], BF16)
        scr("v_c", [S, 4, 65], BF16)
        scr("mixed", [S, D], BF16)
        if dbg:
            scr("dbg_sm", [128, 64], F32)
            scr("dbg_s0", [128, 1024], F32)
            scr("dbg_s1", [128, 1024], F32)
            scr("dbg_ge", [128, 512], BF16)
            scr("dbg_wt", [128, 512], BF16)
            scr("dbg_y", [128, 2048], F32)
        scr("tv", [12, 512], F32)
        self.ut_s = nc.dram_tensor("ut_s", [128, 128, KC * 128], BF16, kind="Internal").ap()
        self.v_s = nc.dram_tensor("v_s", [16384, D], BF16, kind="Internal").ap()
        scr("wb", [8, 1152], F32)
        self.arena = st.enter_context(nc.sbuf_tensor("arena", [128, ARENA], BF16))
        self.ps = [st.enter_context(nc.psum_tensor("ps%d" % i, [128, 512], F32)) for i in range(8)]
        self.rps = [Res("ps%d" % i) for i in range(8)]
        self.top = 0
        self.dma_rr = 0

    def alloc(self, n, dt=BF16):
        nb = n if dt == BF16 else 2 * n
        self.top = (self.top + 31) // 32 * 32
        a = self.top
        self.top += nb
        assert self.top <= ARENA, "arena overflow %d" % self.top
        ap = self.arena[:, a:a + nb]
        if dt == F32:
            ap = ap.bitcast(F32)
        return ap

    def mm(self, out, lhsT, rhs, start, stop, rd, wr):
        return self.P.op("pe", lambda e: e.matmul(out, lhsT=lhsT, rhs=rhs, start=start, stop=stop), rd, wr)

    def tr(self, out, in_, rd, wr):
        ident = self.ident
        return self.P.op("pe", lambda e: e.transpose(out=out, in_=in_, identity=ident), list(rd) + [self.rconst], wr)

    def act(self, out, in_, func, rd, wr, scale=None, accum=None):
        kw = {}
        if scale is not None:
            kw["scale"] = scale
        if accum is not None:
            kw["accum_out"] = accum
        return self.P.op("act", lambda e: e.activation(out=out, in_=in_, func=func, **kw), rd, wr)

    def tt(self, eng, out, in0, in1, op, rd, wr):
        return self.P.op(eng, lambda e: e.tensor_tensor(out=out, in0=in0, in1=in1, op=op), rd, wr)

    def ts(self, eng, out, in0, s1, s2, op0, op1, rd, wr):
        if op1 is None:
            return self.P.op(eng, lambda e: e.tensor_scalar(out=out, in0=in0, scalar1=s1, scalar2=None, op0=op0), rd, wr)
        return self.P.op(eng, lambda e: e.tensor_scalar(out=out, in0=in0, scalar1=s1, scalar2=s2, op0=op0, op1=op1), rd, wr)

    def stt(self, out, in0, scalar, in1, op0, op1, rd, wr):
        return self.P.op("dve", lambda e: e.scalar_tensor_tensor(out=out, in0=in0, scalar=scalar, in1=in1, op0=op0, op1=op1), rd, wr)

    def cp(self, eng, out, in_, rd, wr):
        if eng == "act":
            return self.act(out, in_, AF.Copy, rd, wr)
        return self.P.op(eng, lambda e: e.tensor_copy(out=out, in_=in_), rd, wr)

    def recip(self, out, in_, rd, wr):
        return self.P.op("dve", lambda e: e.reciprocal(out=out, in_=in_), rd, wr)

    def memset(self, eng, ap, val, wr):
        return self.P.op(eng, lambda e: e.memset(ap, val), [], wr)

    def dma(self, out, in_, rd, wr, eng=None, slow=False):
        if eng is None:
            eng = "sp"
        if slow:
            fn = lambda e, s: e.dma_start(out=out, in_=in_, allow_slow_non_contiguous=True).then_inc(s, 16)
        else:
            fn = lambda e, s: e.dma_start(out=out, in_=in_).then_inc(s, 16)
        return self.P.op(eng, fn, rd, wr, ndma=1)

    def rstd_from_ssq(self, st, n, rd_wr):
        self.ts("dve", st[:, 1:2], st[:, 0:1], 1.0 / n, EPS, ALU.mult, ALU.add, [rd_wr], [rd_wr])
        self.act(st[:, 2:3], st[:, 1:2], AF.Sqrt, [rd_wr], [rd_wr])
        self.recip(st[:, 3:4], st[:, 2:3], [rd_wr], [rd_wr])

    def setup_consts(self):
        I = self.I
        self.rconst = Res("const")
        rc = self.rconst
        self.ident = self.alloc(128)
        self.jf = self.alloc(128, F32)
        self.maska = self.alloc(3 * 128)
        self.colv = self.alloc(64)
        self.ropec = self.alloc(NT * 64, F32).rearrange("p (t e) -> p t e", t=NT)
        self.ropes = self.alloc(NT * 64, F32).rearrange("p (t e) -> p t e", t=NT)
        self.eba = self.alloc(3 * 12 * 128).rearrange("p (r h q) -> p r h q", r=3, h=12)
        self.ebb = self.alloc(8 * 14 * 64).rearrange("p (h r q) -> p h r q", h=8, r=14)
        self.gq = {}
        self.dma(self.ident, I["c_ident"], [], [rc])
        self.dma(self.jf, I["c_j"], [], [rc])
        self.dma(self.maska, I["c_maska"].rearrange("k r q -> k (r q)"), [], [rc])
        self.dma(self.colv, I["c_colv"], [], [rc])
        self.dma(self.ropec, I["c_ropec"].rearrange("(t p) e -> p t e", p=128), [], [rc])
        self.dma(self.ropes, I["c_ropes"].rearrange("(t p) e -> p t e", p=128), [], [rc])
        LW = self.lw
        self.gains = self.alloc(LW * (6 * 64 + 2 * 128), F32)
        self.esink = self.alloc(LW * 12, F32)
        self.gcol = self.alloc(LW * 5 * KC, F32).rearrange("p (l g k) -> p l g k", l=LW, g=5)
        self.g = {}
        off = 0
        for l in range(LW):
            for nm, n2 in (("qk_norm_a", 64), ("qk_norm_b", 64), ("qk_norm_c", 64), ("qk_norm_mem", 128)):
                for i in range(2):
                    dst = self.gains[:, off:off + n2]
                    src = I[nm][l, i:i + 1, :]
                    src = bass.AP(src.tensor, src.offset, [[0, 128], [1, n2]])
                    self.dma(dst, src, [], [rc])
                    self.g[(nm, l, i)] = dst
                    off += n2
            src = I["sink_a"][l:l + 1, :]
            src = bass.AP(src.tensor, src.offset, [[0, 128], [1, 12]])
            self.dma(self.esink[:, l * 12:(l + 1) * 12], src, [], [rc])
            for gi, nm in enumerate(("norm_mix", "out_norm", "norm_mem", "norm_mem_kv", "norm_ffn")):
                src = I[nm][l]
                src = bass.AP(src.tensor, src.offset, [[1, 128], [128, KC]])
                self.dma(self.gcol[:, l, gi, :], src, [], [rc], slow=True)
        self.act(self.esink, self.esink, AF.Exp, [rc], [rc])
        self.build_eba()
        self.const_top = self.top

    def build_eba(self):
        I = self.I
        rc = self.rconst
        m = self.top
        relb = self.alloc(12, F32)
        oh = self.alloc(512, F32)
        tvs = self.alloc(512, F32)
        ht = self.alloc(3 * 128, F32).rearrange("p (r k) -> p r k", r=3)
        tmp = self.alloc(128)
        r1, r2, r3, r4 = Res("relb"), Res("tvs"), Res("tvd"), Res("ht")
        self.dma(relb[0:32, :], I["t5_rel_bias"], [], [r1])
        self.dma(oh[0:32, :], I["c_oh"], [], [r1])
        self.mm(self.ps[0][0:12, :], relb[0:32, :], oh[0:32, :], True, True, [r1], [self.rps[0]])
        self.act(tvs[0:12, :], self.ps[0][0:12, :], AF.Copy, [self.rps[0]], [r2])
        self.dma(self.Sx["tv"], tvs[0:12, :], [r2], [r3], eng="pool")
        tvt = self.Sx["tv"].tensor
        rtmp = Res("tmp")
        for h in range(12):
            src = bass.AP(tvt, h * 512, [[1, 128], [128, 3], [1, 128]])
            self.dma(ht, src, [r3], [r4])
            for r in range(3):
                pb = 1 + (h * 3 + r) % 2
                self.mm(self.ps[pb][:, 0:128], ht[:, r, :], self.jf, True, True, [r4, rc], [self.rps[pb]])
                self.act(tmp, self.ps[pb][:, 0:128], AF.Exp, [self.rps[pb]], [rtmp])
                self.tt("dve", self.eba[:, r, h, :], tmp, self.maska[:, r * 128:(r + 1) * 128], ALU.mult, [rtmp, rc], [rc])
        self.P.barrier()
        self.top = m

    def build_ebb(self, l):
        I = self.I
        rc = self.rconst
        m = self.top
        htb = self.alloc(14 * 128, F32).rearrange("p (r k) -> p r k", r=14)
        tmp = self.alloc(7 * 64)
        r1, r2, r3 = Res("wbz"), Res("htb"), Res("tmpb")
        wbt = self.Sx["wb"].tensor
        self.dma(self.Sx["wb"], I["c_zero"], [], [r1])
        for h in range(8):
            dst = bass.AP(wbt, h * 1152 + 48, [[64, 15], [1, 31]])
            self.dma(dst, I["rpb_b"][l, h], [r1], [r1])
        j64 = self.jf[0:64, 64:128]
        for h in range(8):
            src = bass.AP(wbt, h * 1152, [[1, 64], [64, 14], [1, 128]])
            self.dma(htb[0:64], src, [r1], [r2])
            for half in range(2):
                pb = 1 + half
                for r in range(7):
                    self.mm(self.ps[pb][:, r * 64:(r + 1) * 64], htb[0:64, half * 7 + r, :], j64, True, True,
                            [r2, rc], [self.rps[pb]])
                self.act(tmp, self.ps[pb][:, 0:448], AF.Exp, [self.rps[pb]], [r3])
                colb = bass.AP(self.colv.tensor, self.colv.offset, [list(self.colv.ap[0]), [0, 7], [1, 64]])
                self.tt("dve", self.ebb[:, h, half * 7:(half + 1) * 7, :], tmp.rearrange("p (r q) -> p r q", r=7), colb,
                        ALU.mult, [r3, rc], [rc])
        self.P.barrier()
        self.top = m

    def norm_transpose(self, src_rows, ntiles, dstT, rdst, rsrc=None, pbase=0, gcol=None):
        m = self.top
        xt = [self.alloc(D, F32) for _ in range(2)]
        xn = [self.alloc(D) for _ in range(2)]
        junk = self.alloc(D)
        stt_ = [self.alloc(8, F32) for _ in range(2)]
        rx = [Res("xt%d" % i) for i in range(2)]
        rn = [Res("xn%d" % i) for i in range(2)]
        rs = [Res("st%d" % i) for i in range(2)]
        rj = Res("junk")
        for t in range(ntiles):
            b = t % 2
            self.dma(xt[b], src_rows(t), [rsrc[t]] if rsrc else [], [rx[b]])
            self.act(junk, xt[b], AF.Square, [rx[b]], [rj, rs[b]], accum=stt_[b][:, 0:1])
            self.rstd_from_ssq(stt_[b], D, rs[b])
            self.act(xn[b], xt[b], AF.Copy, [rx[b], rs[b]], [rn[b]], scale=stt_[b][:, 3:4])
            for g in range(4):
                pb = pbase + g % 2
                pv = self.ps[pb][:, 0:256].bitcast(BF16)
                for j in range(4):
                    kc = g * 4 + j
                    self.tr(pv[:, j * 128:(j + 1) * 128], xn[b][:, kc * 128:(kc + 1) * 128], [rn[b]], [self.rps[pb]])
                eng = "dve" if g % 2 == 0 else "act"
                if gcol is None:
                    self.cp(eng, dstT[:, g * 4:(g + 1) * 4, t * 128:(t + 1) * 128], pv.rearrange("p (j q) -> p j q", j=4),
                            [self.rps[pb]], [rdst[t]])
                else:
                    for j in range(4):
                        kc = g * 4 + j
                        if (kc % 2) == 0:
                            self.act(dstT[:, kc, t * 128:(t + 1) * 128], pv[:, j * 128:(j + 1) * 128], AF.Copy, [self.rps[pb], self.rconst],
                                     [rdst[t]], scale=gcol[:, kc:kc + 1])
                        else:
                            self.ts("dve", dstT[:, kc, t * 128:(t + 1) * 128], pv[:, j * 128:(j + 1) * 128], gcol[:, kc:kc + 1], None,
                                    ALU.mult, None, [self.rps[pb], self.rconst], [rdst[t]])
        self.P.barrier()
        self.top = m

    def linear(self, actT, ract, ntiles, w_dram, kcn, ncols, nblk, gcol, epilogue, pbase=2):
        m = self.top
        wst = [self.alloc(kcn * nblk, F32).rearrange("p (k n) -> p k n", k=kcn) for _ in range(2)]
        wbf = [self.alloc(kcn * nblk).rearrange("p (k n) -> p k n", k=kcn) for _ in range(2)]
        rst = [Res("wst%d" % i) for i in range(2)]
        rwb = [[Res("wbf%d_%d" % (i, k)) for k in range(kcn)] for i in range(2)]
        wv = w_dram.rearrange("(k p) n -> p k n", p=128)
        engs = ["act", "dve", "pool"]
        for nb in range(ncols // nblk):
            b = nb % 2
            self.dma(wst[b], wv[:, :, nb * nblk:(nb + 1) * nblk], [], [rst[b]])
            for k in range(kcn):
                eng = engs[k % 3]
                if gcol is None:
                    self.cp(eng, wbf[b][:, k, :], wst[b][:, k, :], [rst[b]], [rwb[b][k]])
                elif eng == "act":
                    self.act(wbf[b][:, k, :], wst[b][:, k, :], AF.Copy, [rst[b], self.rconst], [rwb[b][k]], scale=gcol[:, k:k + 1])
                else:
                    self.ts(eng, wbf[b][:, k, :], wst[b][:, k, :], gcol[:, k:k + 1], None, ALU.mult, None,
                            [rst[b], self.rconst], [rwb[b][k]])
            for t in range(ntiles):
                pb = pbase + t % 2
                ps = self.ps[pb][:, 0:nblk]
                for k in range(kcn):
                    self.mm(ps, actT[:, k, t * 128:(t + 1) * 128], wbf[b][:, k, :], k == 0, k == kcn - 1,
                            [ract[t], rwb[b][k]], [self.rps[pb]])
                epilogue(nb, t, ps, self.rps[pb])
        self.P.barrier()
        self.top = m

    def headnorm(self, ps, rps, nh, dh, gain, sc):
        sq, ss, qn, qg = sc["sq"], sc["ss"], sc["qn"], sc["qg"]
        r = sc["r"]
        n = nh * dh
        self.act(sq[:, 0:n], ps, AF.Square, [rps], [r["sq"]])
        self.P.op("dve", lambda e: e.tensor_reduce(out=ss[:, 0:nh], in_=sq[:, 0:n].rearrange("p (h d) -> p h d", h=nh), axis=AX.X, op=ALU.add),
                  [r["sq"]], [r["ss"]])
        self.ts("dve", ss[:, 8:8 + nh], ss[:, 0:nh], 1.0 / dh, EPS, ALU.mult, ALU.add, [r["ss"]], [r["ss"]])
        self.act(ss[:, 16:16 + nh], ss[:, 8:8 + nh], AF.Sqrt, [r["ss"]], [r["ss"]])
        self.recip(ss[:, 24:24 + nh], ss[:, 16:16 + nh], [r["ss"]], [r["ss"]])
        rsb = ss[:, 24:24 + nh].unsqueeze(2).to_broadcast([128, nh, dh])
        self.tt("dve", qn[:, 0:n].rearrange("p (h d) -> p h d", h=nh), ps.rearrange("p (h d) -> p h d", h=nh), rsb, ALU.mult,
                [rps, r["ss"]], [r["qn"]])
        gb = gain.unsqueeze(1).to_broadcast([128, nh, dh])
        return gb

    def phase_A(self, l, xsrc):
        I, Sx = self.I, self.Sx
        m = self.top
        xnT = self.alloc(KC * S).rearrange("p (k t) -> p k t", k=KC)
        rxn = [Res("xnT%d" % t) for t in range(NT)]
        self.norm_transpose(lambda t: xsrc[t * 128:(t + 1) * 128, :], NT, xnT, rxn)
        sc = {
            "sq": self.alloc(256, F32), "ss": self.alloc(32, F32), "qn": self.alloc(256, F32), "qg": self.alloc(256, F32),
            "t1": self.alloc(256, F32), "t2": self.alloc(256, F32),
            "r": {k: Res(k) for k in ("sq", "ss", "qn", "qg", "t1", "t2")},
        }
        qb = [self.alloc(256) for _ in range(2)]
        rqb = [Res("qb%d" % i) for i in range(2)]
        hT = [self.alloc(4 * S).rearrange("p (h t) -> p h t", h=4) for _ in range(2)]
        rhT = [Res("hT%d" % i) for i in range(2)]
        vst = [self.alloc(NT * 4 * 65).rearrange("p (t h e) -> p t h e", t=NT, h=4) for _ in range(1)]
        rvs = [Res("vst%d" % i) for i in range(1)]
        for i in range(1):
            self.memset("pool", vst[i][:, :, :, 64:65], 1.0, [rvs[i]])
        blocks = [("q", "qt_a", 0, ("qk_norm_a", 0), False), ("q", "qt_a", 4, ("qk_norm_a", 0), False), ("q", "qt_a", 8, ("qk_norm_a", 0), False),
                  ("q", "kt_a", 0, ("qk_norm_a", 1), False), ("v", "v_a", 0, None, False),
                  ("q", "qt_b", 0, ("qk_norm_b", 0), False), ("q", "qt_b", 4, ("qk_norm_b", 0), False),
                  ("q", "kt_b", 0, ("qk_norm_b", 1), False), ("q", "kt_b", 4, ("qk_norm_b", 1), False),
                  ("v", "v_b", 0, None, False), ("v", "v_b", 4, None, False),
                  ("q", "qt_c", 0, ("qk_norm_c", 0), True), ("q", "qt_c", 4, ("qk_norm_c", 0), True), ("q", "qt_c", 8, ("qk_norm_c", 0), True),
                  ("q", "kt_c", 0, ("qk_norm_c", 1), True), ("v", "v_c", 0, None, False)]
        cnt = {"q": 0, "v": 0, "e": 0}

        def epi(nb, t, ps, rps):
            kind, dst, h0, gk, rope = blocks[nb]
            if kind == "v":
                vb = 0
                cnt["v"] += 1
                self.cp("act", vst[vb][:, t, :, 0:64], ps.rearrange("p (h d) -> p h d", h=4), [rps], [rvs[vb]])
                if t == NT - 1:
                    dv = Sx[dst].rearrange("(t p) h e -> p t h e", p=128)[:, :, h0:h0 + 4, :]
                    self.dma(dv, vst[vb], [rvs[vb]], [], eng="pool")
                return
            hb = (cnt["q"] // NT) % 2
            cnt["q"] += 1
            e = cnt["e"] % 2
            cnt["e"] += 1
            gain = self.g[(gk[0], l, gk[1])]
            gb = self.headnorm(ps, rps, 4, 64, gain, sc)
            r = sc["r"]
            qn3 = sc["qn"].rearrange("p (h d) -> p h d", h=4)
            if not rope:
                self.tt("dve", qb[e].rearrange("p (h d) -> p h d", h=4), qn3, gb, ALU.mult, [r["qn"], self.rconst], [rqb[e]])
            else:
                qg = sc["qg"]
                self.tt("dve", qg.rearrange("p (h d) -> p h d", h=4), qn3, gb, ALU.mult, [r["qn"], self.rconst], [r["qg"]])
                cc = self.ropec[:, t, :].unsqueeze(1).to_broadcast([128, 4, 64])
                self.tt("dve", sc["t1"].rearrange("p (h d) -> p h d", h=4), qg.rearrange("p (h d) -> p h d", h=4), cc, ALU.mult,
                        [r["qg"], self.rconst], [r["t1"]])
                qg5 = qg.rearrange("p (a x f) -> p a x f", x=2, f=16)
                t25 = sc["t2"].rearrange("p (a x f) -> p a x f", x=2, f=16)
                ss5 = self.ropes[:, t, :].rearrange("p (a x f) -> p a x f", x=2, f=16)
                for x in range(2):
                    src = qg5[:, :, 1 - x, :].rearrange("p (h a) f -> p h a f", h=4)
                    dst_ = t25[:, :, x, :].rearrange("p (h a) f -> p h a f", h=4)
                    sb = ss5[:, :, x, :].unsqueeze(1).to_broadcast([128, 4, 2, 16])
                    self.tt("dve", dst_, src, sb, ALU.mult, [r["qg"], self.rconst], [r["t2"]])
                self.tt("dve", qb[e], sc["t1"], sc["t2"], ALU.add, [r["t1"], r["t2"]], [rqb[e]])
            pb = 4 + e
            pv = self.ps[pb][:, 0:256].bitcast(BF16)
            for j in range(4):
                self.tr(pv[0:64, j * 128:(j + 1) * 128], qb[e][:, j * 64:(j + 1) * 64], [rqb[e]], [self.rps[pb]])
            self.cp("act", hT[hb][0:64, :, t * 128:(t + 1) * 128], pv[0:64, :].rearrange("p (j q) -> p j q", j=4),
                    [self.rps[pb]], [rhT[hb]])
            if t == NT - 1:
                dv = Sx[dst][h0:h0 + 4].rearrange("h d t -> d h t")
                self.dma(dv, hT[hb][0:64], [rhT[hb]], [], eng="pool")

        self.linear(xnT, rxn, NT, I["w_in"][l], KC, 4096, 256, self.gcol[:, l, 0, :], epi)
        self.top = m
        self.P.barrier()

    def outnorm_store(self, o_f32, n, parts, dst, sc):
        r = sc["r"]
        self.act(sc["junk"][0:parts, 0:n], o_f32, AF.Square, [r["o"]], [r["junk"], r["st"]], accum=sc["st"][0:parts, 0:1])
        self.rstd_from_ssq(sc["st"][0:parts], n, r["st"])
        self.act(sc["ob"][0:parts, 0:n], o_f32, AF.Copy, [r["o"], r["st"]], [r["ob"]], scale=sc["st"][0:parts, 3:4])
        self.dma(dst, sc["ob"][0:parts, 0:n], [r["ob"]], [], eng="pool")

    def mixer_AC(self, l, which):
        I, Sx = self.I, self.Sx
        m = self.top
        qn_, kn_, vn_ = ("qt_a", "kt_a", "v_a") if which == "a" else ("qt_c", "kt_c", "v_c")
        QT = self.alloc(12 * S).rearrange("p (h t) -> p h t", h=12)
        KT = self.alloc(4 * S).rearrange("p (h t) -> p h t", h=4)
        V = self.alloc(NT * 4 * 65).rearrange("p (t h e) -> p t h e", t=NT, h=4)
        rq, rk, rv = Res("QT"), Res("KT"), Res("V")
        self.dma(QT[0:64], Sx[qn_].rearrange("h d t -> d h t"), [], [rq])
        self.dma(KT[0:64], Sx[kn_].rearrange("h d t -> d h t"), [], [rk])
        self.dma(V, Sx[vn_].rearrange("(t p) h e -> p t h e", p=128), [], [rv])
        NR = 2
        Pe = [self.alloc(384) for _ in range(NR)]
        Pm = [self.alloc(384) for _ in range(NR)]
        rpe = [Res("pe%d" % i) for i in range(NR)]
        rpm = [Res("pm%d" % i) for i in range(NR)]
        scs = []
        for i in range(2):
            scs.append({"den": self.alloc(16, F32), "rden": self.alloc(16, F32), "o": self.alloc(768, F32), "junk": self.alloc(768),
                        "st": self.alloc(8, F32), "ob": self.alloc(768),
                        "r": {k: Res(k + str(i)) for k in ("den", "o", "junk", "st", "ob")}})
        rot = 0
        col0 = 0 if which == "a" else 1280
        for qb in range(NT):
            ob_ = qb % 2
            pso = [self.ps[2 + 3 * ob_ + g] for g in range(3)]
            rpo = [self.rps[2 + 3 * ob_ + g] for g in range(3)]
            kts = [kt for kt in ((qb - 1, qb, qb + 1) if which == "a" else range(NT)) if 0 <= kt < NT]
            for hk in range(4):
                for idx, kt in enumerate(kts):
                    s_ = rot % NR
                    rot += 1
                    ps = self.ps[s_][:, 0:384]
                    self.mm(ps, KT[0:64, hk, kt * 128:(kt + 1) * 128], QT[0:64, 3 * hk:3 * hk + 3, qb * 128:(qb + 1) * 128],
                            True, True, [rk, rq], [self.rps[s_]])
                    self.act(Pe[s_], ps, AF.Exp, [self.rps[s_]], [rpe[s_]], scale=0.125)
                    if which == "a":
                        ri = kt - qb + 1
                        self.tt("dve", Pm[s_].rearrange("p (g q) -> p g q", g=3), Pe[s_].rearrange("p (g q) -> p g q", g=3),
                                self.eba[:, ri, 3 * hk:3 * hk + 3, :], ALU.mult, [rpe[s_], self.rconst], [rpm[s_]])
                        pp, rpp = Pm[s_], rpm[s_]
                    else:
                        pp, rpp = Pe[s_], rpe[s_]
                    for g in range(3):
                        self.mm(pso[g][:, hk * 65:hk * 65 + 65], pp[:, g * 128:(g + 1) * 128], V[:, kt, hk, :],
                                idx == 0, idx == len(kts) - 1, [rpp, rv], [rpo[g]])
            sc = scs[ob_]
            r = sc["r"]
            den3 = sc["den"][:, 0:12].rearrange("p (k g) -> p k g", g=3)
            rden3 = sc["rden"][:, 0:12].rearrange("p (k g) -> p k g", g=3)
            es3 = self.esink[:, l * 12:(l + 1) * 12].rearrange("p (k g) -> p k g", g=3)
            o4 = sc["o"].rearrange("p (k g d) -> p k g d", k=4, g=3)
            for g in range(3):
                o3 = pso[g][:, 0:260].rearrange("p (k e) -> p k e", k=4)
                if which == "a":
                    self.tt("dve", den3[:, :, g], o3[:, :, 64], es3[:, :, g], ALU.add, [rpo[g], self.rconst], [r["den"]])
                else:
                    self.cp("dve", den3[:, :, g], o3[:, :, 64], [rpo[g]], [r["den"]])
            self.recip(sc["rden"][:, 0:12], sc["den"][:, 0:12], [r["den"]], [r["den"]])
            for g in range(3):
                o3 = pso[g][:, 0:260].rearrange("p (k e) -> p k e", k=4)
                rb = rden3[:, :, g].unsqueeze(2).to_broadcast([128, 4, 64])
                self.tt("dve", o4[:, :, g, :], o3[:, :, 0:64], rb, ALU.mult, [rpo[g], r["den"]], [r["o"]])
            self.outnorm_store(sc["o"], 768, 128, Sx["mixed"][qb * 128:(qb + 1) * 128, col0:col0 + 768], sc)
        self.top = m
        self.P.barrier()

    def mixer_B(self, l):
        I, Sx = self.I, self.Sx
        m = self.top
        QT = self.alloc(8 * S).rearrange("p (h t) -> p h t", h=8)
        KT = self.alloc(8 * S).rearrange("p (h t) -> p h t", h=8)
        rq, rk = Res("QTb"), Res("KTb")
        self.dma(QT[0:64], Sx["qt_b"].rearrange("h d t -> d h t"), [], [rq])
        self.dma(KT[0:64], Sx["kt_b"].rearrange("h d t -> d h t"), [], [rk])
        Vr = [self.alloc(4 * 8 * 65).rearrange("p (i h e) -> p i h e", i=4, h=8) for _ in range(2)]
        rvr = [Res("vr%d" % i) for i in range(2)]
        NR = 3
        Pe = [self.alloc(512) for _ in range(NR)]
        Pm = [self.alloc(512) for _ in range(NR)]
        rpe = [Res("peb%d" % i) for i in range(NR)]
        rpm = [Res("pmb%d" % i) for i in range(NR)]
        scs = []
        for i in range(2):
            scs.append({"rden": self.alloc(16, F32), "o": self.alloc(512, F32), "junk": self.alloc(512), "st": self.alloc(8, F32),
                        "ob": self.alloc(512), "r": {k: Res(k + "b" + str(i)) for k in ("den", "o", "junk", "st", "ob")}})
        vbt = Sx["v_b"].tensor
        rot = 0
        for r_ in range(32):
            r0 = min(max(r_ - 4, 0), 24)
            s_ = r_ - r0
            vb = r_ % 2
            src = bass.AP(vbt, 64 * r0 * 520, [[520, 128], [128 * 520, 4], [1, 520]])
            self.dma(Vr[vb].rearrange("p i h e -> p i (h e)"), src, [], [rvr[vb]])
            ob_ = r_ % 2
            pso = [self.ps[3 + 2 * ob_], self.ps[4 + 2 * ob_]]
            rpo = [self.rps[3 + 2 * ob_], self.rps[4 + 2 * ob_]]
            for hp in range(4):
                sl = rot % NR
                rot += 1
                ps = self.ps[sl][:, 0:512].rearrange("p (a i q) -> p a i q", a=2, i=4)
                for hh in range(2):
                    h = 2 * hp + hh
                    for i in range(4):
                        k0 = 64 * r0 + 128 * i
                        self.mm(ps[:, hh, i, :], KT[0:64, h, k0:k0 + 128], QT[0:64, h, 64 * r_:64 * r_ + 64], True, True,
                                [rk, rq], [self.rps[sl]])
                self.act(Pe[sl], self.ps[sl][:, 0:512], AF.Exp, [self.rps[sl]], [rpe[sl]], scale=0.125)
                eb = self.ebb[:, 2 * hp:2 * hp + 2, 7 - s_:7 - s_ + 7:2, :]
                self.tt("dve", Pm[sl].rearrange("p (a i q) -> p a i q", a=2, i=4), Pe[sl].rearrange("p (a i q) -> p a i q", a=2, i=4), eb,
                        ALU.mult, [rpe[sl], self.rconst], [rpm[sl]])
                pm4 = Pm[sl].rearrange("p (a i q) -> p a i q", a=2, i=4)
                for hh in range(2):
                    h = 2 * hp + hh
                    for i in range(4):
                        self.mm(pso[h // 4][0:64, (h % 4) * 65:(h % 4) * 65 + 65], pm4[:, hh, i, :], Vr[vb][:, i, h, :], i == 0, i == 3,
                                [rpm[sl], rvr[vb]], [rpo[h // 4]])
            sc = scs[ob_]
            r = sc["r"]
            for b in range(2):
                o3 = pso[b][0:64, 0:260].rearrange("p (h e) -> p h e", h=4)
                self.recip(sc["rden"][0:64, 4 * b:4 * b + 4], o3[:, :, 64], [rpo[b]], [r["den"]])
            for b in range(2):
                o3 = pso[b][0:64, 0:260].rearrange("p (h e) -> p h e", h=4)
                rb = sc["rden"][0:64, 4 * b:4 * b + 4].unsqueeze(2).to_broadcast([64, 4, 64])
                self.tt("dve", sc["o"][0:64, 256 * b:256 * b + 256].rearrange("p (h d) -> p h d", h=4), o3[:, :, 0:64], rb, ALU.mult,
                        [rpo[b], r["den"]], [r["o"]])
            self.outnorm_store(sc["o"][0:64], 512, 64, Sx["mixed"][64 * r_:64 * r_ + 64, 768:1280], sc)
        self.top = m
        self.P.barrier()

    def residual_epilogue(self, xsrc, xdst, nblk, rrow):
        m_ = {}
        xt = [self.alloc(nblk, F32) for _ in range(3)]
        rxt = [Res("rxt%d" % i) for i in range(3)]
        cnt = [0]

        def epi(nb, t, ps, rps):
            b = cnt[0] % 3
            cnt[0] += 1
            self.dma(xt[b], xsrc[t * 128:(t + 1) * 128, nb * nblk:(nb + 1) * nblk], [rrow[t]] if rrow else [], [rxt[b]])
            self.tt("dve", xt[b], ps, xt[b], ALU.add, [rps, rxt[b]], [rxt[b]])
            self.dma(xdst[t * 128:(t + 1) * 128, nb * nblk:(nb + 1) * nblk], xt[b], [rxt[b]], [rrow[t]] if rrow else [], eng="pool")

        return epi

    def phase_C(self, l, xsrc, xdst, inplace):
        I, Sx = self.I, self.Sx
        m = self.top
        mixT = self.alloc(KC * S).rearrange("p (k t) -> p k t", k=KC)
        rmx = [Res("mixT%d" % t) for t in range(NT)]
        mt = [self.alloc(D) for _ in range(2)]
        rmt = [Res("mt%d" % i) for i in range(2)]
        for t in range(NT):
            b = t % 2
            self.dma(mt[b], Sx["mixed"][t * 128:(t + 1) * 128, :], [], [rmt[b]])
            for g in range(4):
                pb = g % 2
                pv = self.ps[pb][:, 0:256].bitcast(BF16)
                for j in range(4):
                    kc = g * 4 + j
                    self.tr(pv[:, j * 128:(j + 1) * 128], mt[b][:, kc * 128:(kc + 1) * 128], [rmt[b]], [self.rps[pb]])
                self.cp("dve" if g % 2 == 0 else "act", mixT[:, g * 4:(g + 1) * 4, t * 128:(t + 1) * 128],
                        pv.rearrange("p (j q) -> p j q", j=4), [self.rps[pb]], [rmx[t]])
        rrow = [Res("row%d" % t) for t in range(NT)] if inplace else None
        epi = self.residual_epilogue(xsrc, xdst, 256, rrow)
        self.linear(mixT, rmx, NT, I["w_out"][l], KC, D, 256, self.gcol[:, l, 1, :], epi)
        self.top = m
        self.P.barrier()

    def phase_D(self, l, s, xio):
        I, Sx = self.I, self.Sx
        m = self.top
        memT = self.alloc(KC * NMEM).rearrange("p (k t) -> p k t", k=KC)
        rmemT = [Res("memT%d" % t) for t in range(2)]
        msrc = I["mem"][s]
        self.norm_transpose(lambda t: msrc[t * 128:(t + 1) * 128, :], 2, memT, rmemT)
        KmT = self.alloc(4 * NMEM).rearrange("p (h t) -> p h t", h=4)
        Vm = self.alloc(2 * 4 * 129).rearrange("p (t h e) -> p t h e", t=2, h=4)
        rkm, rvm = Res("KmT"), Res("Vm")
        self.memset("pool", Vm[:, :, :, 128:129], 1.0, [rvm])
        sc = {"sq": self.alloc(512, F32), "ss": self.alloc(32, F32), "qn": self.alloc(512, F32), "qg": None,
              "r": {k: Res(k + "d") for k in ("sq", "ss", "qn")}}
        qb = [self.alloc(512) for _ in range(2)]
        rqb = [Res("qbd%d" % i) for i in range(2)]
        cnt = [0]

        def epi_kv(nb, t, ps, rps):
            if nb == 1:
                self.cp("act", Vm[:, t, :, 0:128], ps.rearrange("p (h d) -> p h d", h=4), [rps], [rvm])
                return
            e = cnt[0] % 2
            cnt[0] += 1
            gb = self.headnorm(ps, rps, 4, 128, self.g[("qk_norm_mem", l, 1)], sc)
            self.tt("dve", qb[e].rearrange("p (h d) -> p h d", h=4), sc["qn"].rearrange("p (h d) -> p h d", h=4), gb, ALU.mult,
                    [sc["r"]["qn"], self.rconst], [rqb[e]])
            pb = 4 + e
            pv = self.ps[pb][:, 0:256].bitcast(BF16)
            for j in range(4):
                self.tr(pv[:, j * 128:(j + 1) * 128], qb[e][:, j * 128:(j + 1) * 128], [rqb[e]], [self.rps[pb]])
            self.cp("act", KmT[:, :, t * 128:(t + 1) * 128], pv.rearrange("p (j q) -> p j q", j=4), [self.rps[pb]], [rkm])

        self.linear(memT, rmemT, 2, I["w_mem_kv"][l], KC, 1024, 512, self.gcol[:, l, 3, :], epi_kv)
        QmT = self.alloc(4 * S).rearrange("p (h t) -> p h t", h=4)
        rqm = [Res("QmT%d" % t) for t in range(NT)]
        mD2 = self.top
        xnT = self.alloc(KC * S).rearrange("p (k t) -> p k t", k=KC)
        rxn = [Res("xnTd%d" % t) for t in range(NT)]
        rrow = [Res("rowd%d" % t) for t in range(NT)]
        self.norm_transpose(lambda t: xio[t * 128:(t + 1) * 128, :], NT, xnT, rxn, rsrc=rrow)

        def epi_q(nb, t, ps, rps):
            e = cnt[0] % 2
            cnt[0] += 1
            gb = self.headnorm(ps, rps, 2, 128, self.g[("qk_norm_mem", l, 0)], sc)
            self.tt("dve", qb[e][:, 0:256].rearrange("p (h d) -> p h d", h=2), sc["qn"][:, 0:256].rearrange("p (h d) -> p h d", h=2), gb, ALU.mult,
                    [sc["r"]["qn"], self.rconst], [rqb[e]])
            pb = 4 + e
            pv = self.ps[pb][:, 0:256].bitcast(BF16)
            for j in range(2):
                self.tr(pv[:, j * 128:(j + 1) * 128], qb[e][:, j * 128:(j + 1) * 128], [rqb[e]], [self.rps[pb]])
            self.cp("act", QmT[:, 2 * nb:2 * nb + 2, t * 128:(t + 1) * 128], pv[:, 0:256].rearrange("p (j q) -> p j q", j=2), [self.rps[pb]], [rqm[t]])

        self.linear(xnT, rxn, NT, I["w_mem_q"][l], KC, 512, 256, self.gcol[:, l, 2, :], epi_q)
        self.top = mD2
        wo_st = self.alloc(4 * 512, F32).rearrange("p (k n) -> p k n", k=4)
        wo = self.alloc(4 * D).rearrange("p (k n) -> p k n", k=4)
        rwst, rwo = Res("wost"), Res("wo")
        wov = I["w_mem_o"][l].rearrange("(k p) n -> p k n", p=128)
        for nb in range(4):
            self.dma(wo_st, wov[:, :, nb * 512:(nb + 1) * 512], [], [rwst])
            self.cp("pool" if nb % 2 else "act", wo[:, :, nb * 512:(nb + 1) * 512], wo_st, [rwst], [rwo])
        Pe = [self.alloc(1024) for _ in range(2)]
        rpe = [Res("ped%d" % i) for i in range(2)]
        rden = [self.alloc(8, F32) for _ in range(2)]
        om = [self.alloc(512) for _ in range(2)]
        omT = [self.alloc(512).rearrange("p (h t) -> p h t", h=4) for _ in range(2)]
        xt = [self.alloc(D, F32) for _ in range(2)]
        rrd = [Res("rdend%d" % i) for i in range(2)]
        rom = [Res("om%d" % i) for i in range(2)]
        romT = [Res("omT%d" % i) for i in range(2)]
        rxt = [Res("xtd%d" % i) for i in range(2)]
        scale = 128.0 ** -0.5
        for t in range(NT):
            b = t % 2
            self.dma(xt[b], xio[t * 128:(t + 1) * 128, :], [rrow[t]], [rxt[b]])
            for mt_ in range(2):
                for h in range(4):
                    self.mm(self.ps[mt_][:, h * 128:(h + 1) * 128], KmT[:, h, mt_ * 128:(mt_ + 1) * 128], QmT[:, h, t * 128:(t + 1) * 128],
                            True, True, [rkm, rqm[t]], [self.rps[mt_]])
                self.act(Pe[b][:, mt_ * 512:(mt_ + 1) * 512], self.ps[mt_][:, 0:512], AF.Exp, [self.rps[mt_]], [rpe[b]], scale=scale)
            for h in range(4):
                pb, off = (2, h * 129) if h < 3 else (3, 0)
                for mt_ in range(2):
                    self.mm(self.ps[pb][:, off:off + 129], Pe[b][:, mt_ * 512 + h * 128:mt_ * 512 + (h + 1) * 128], Vm[:, mt_, h, :],
                            mt_ == 0, mt_ == 1, [rpe[b], rvm], [self.rps[pb]])
            o3 = self.ps[2][:, 0:387].rearrange("p (h e) -> p h e", h=3)
            self.recip(rden[b][:, 0:3], o3[:, :, 128], [self.rps[2]], [rrd[b]])
            self.recip(rden[b][:, 3:4], self.ps[3][:, 128:129], [self.rps[3]], [rrd[b]])
            self.tt("dve", om[b][:, 0:384].rearrange("p (h d) -> p h d", h=3), o3[:, :, 0:128],
                    rden[b][:, 0:3].unsqueeze(2).to_broadcast([128, 3, 128]), ALU.mult, [self.rps[2], rrd[b]], [rom[b]])
            self.ts("dve", om[b][:, 384:512], self.ps[3][:, 0:128], rden[b][:, 3:4], None, ALU.mult, None, [self.rps[3], rrd[b]], [rom[b]])
            pv = self.ps[4][:, 0:256].bitcast(BF16)
            for j in range(4):
                self.tr(pv[:, j * 128:(j + 1) * 128], om[b][:, j * 128:(j + 1) * 128], [rom[b]], [self.rps[4]])
            self.cp("act", omT[b], pv.rearrange("p (j q) -> p j q", j=4), [self.rps[4]], [romT[b]])
            for nb in range(4):
                pb = 5 + nb % 2
                for k in range(4):
                    self.mm(self.ps[pb][:, 0:512], omT[b][:, k, :], wo[:, k, nb * 512:(nb + 1) * 512], k == 0, k == 3,
                            [romT[b], rwo], [self.rps[pb]])
                self.tt("dve", xt[b][:, nb * 512:(nb + 1) * 512], self.ps[pb][:, 0:512], xt[b][:, nb * 512:(nb + 1) * 512], ALU.add,
                        [self.rps[pb], rxt[b]], [rxt[b]])
            self.dma(xio[t * 128:(t + 1) * 128, :], xt[b], [rxt[b]], [rrow[t]], eng="pool")
        self.top = m
        self.P.barrier()

    def phase_P(self, l):
        I = self.I
        m = self.top
        ust = [self.alloc(D, F32) for _ in range(2)]
        ubf = [self.alloc(D) for _ in range(2)]
        uTb = [self.alloc(KC * 128).rearrange("p (k e) -> p k e", k=KC) for _ in range(2)]
        vst = [self.alloc(D, F32) for _ in range(2)]
        vb = [self.alloc(D) for _ in range(2)]
        r = {k: [Res(k + str(i)) for i in range(2)] for k in ("ust", "ubf", "uTb", "vst", "vb")}
        uv = I["peer_u"][l]
        vv = I["peer_v"][l]
        for c in range(128):
            b = c % 2
            self.dma(ust[b], uv[c * 128:(c + 1) * 128, :], [], [r["ust"][b]])
            self.cp("pool", ubf[b], ust[b], [r["ust"][b]], [r["ubf"][b]])
            for g in range(4):
                pb = g % 2
                pv = self.ps[pb][:, 0:256].bitcast(BF16)
                for j in range(4):
                    k = g * 4 + j
                    self.tr(pv[:, j * 128:(j + 1) * 128], ubf[b][:, k * 128:(k + 1) * 128], [r["ubf"][b]], [self.rps[pb]])
                self.cp("act" if g % 2 else "dve", uTb[b][:, g * 4:(g + 1) * 4, :], pv.rearrange("p (j q) -> p j q", j=4),
                        [self.rps[pb]], [r["uTb"][b]])
            self.dma(self.ut_s[c], uTb[b].rearrange("p k e -> p (k e)"), [r["uTb"][b]], [], eng="pool")
            self.dma(vst[b], vv[c * 128:(c + 1) * 128, :], [], [r["vst"][b]])
            self.cp("act" if c % 2 else "dve", vb[b], vst[b], [r["vst"][b]], [r["vb"][b]])
            self.dma(self.v_s[c * 128:(c + 1) * 128, :], vb[b], [r["vb"][b]], [], eng="pool")
        self.P.barrier()
        self.top = m

    def phase_E(self, l, xio):
        I, Sx = self.I, self.Sx
        m = self.top
        TP = 512
        NTP = 4
        keysT = self.alloc(16 * 128).rearrange("p (a n) -> p a n", a=16)
        rkt = Res("keysT")
        m1 = self.top
        kst = self.alloc(16 * 128, F32).rearrange("p (a n) -> p a n", a=16)
        kbf = self.alloc(16 * 128).rearrange("p (a n) -> p a n", a=16)
        rks, rkb = Res("kst"), Res("kbf")
        self.dma(kst, I["peer_keys"][l].rearrange("h c n d -> n (h c) d"), [], [rks])
        self.cp("dve", kbf, kst, [rks], [rkb])
        for g in range(4):
            pv = self.ps[g % 2][:, 0:256].bitcast(BF16)
            for j in range(4):
                self.tr(pv[:, j * 128:(j + 1) * 128], kbf[:, g * 4 + j, :], [rkb], [self.rps[g % 2]])
            self.cp("act", keysT[:, g * 4:(g + 1) * 4, :], pv.rearrange("p (j q) -> p j q", j=4), [self.rps[g % 2]], [rkt])
        self.P.barrier()
        gcol = self.gcol[:, l, 4, :]
        for p_ in range(S // TP):
            self.top = m1
            rows = lambda t: xio[p_ * TP + t * 128:p_ * TP + (t + 1) * 128, :]
            hnT = self.alloc(KC * TP).rearrange("p (k t) -> p k t", k=KC)
            rhn = [Res("hnT%d" % t) for t in range(NTP)]
            s1b = self.alloc(NTP * 8 * 128, F32).rearrange("p (t h n) -> p t h n", t=NTP, h=8)
            phi = self.alloc(NTP * 128 * 8, F32).rearrange("p (t c h) -> p t c h", t=NTP, c=128)
            Dm = self.alloc(NTP * 8 * 128).rearrange("p (t h q) -> p t h q", t=NTP, h=8)
            ytok = self.alloc(NTP * D, F32).rearrange("p (t d) -> p t d", t=NTP)
            rs1 = [Res("s1b%d" % t) for t in range(NTP)]
            rphi = [Res("phi%d" % t) for t in range(NTP)]
            rdm = [Res("Dm%d" % t) for t in range(NTP)]
            ryt = [[Res("yt%d_%d" % (t, d)) for d in range(4)] for t in range(NTP)]
            m2 = self.top
            self.norm_transpose(rows, NTP, hnT, rhn, gcol=gcol)
            qT = self.alloc(16 * TP).rearrange("p (a t) -> p a t", a=16)
            rqT = [Res("qT%d" % a) for a in range(16)]
            wst = [self.alloc(KC * 128, F32).rearrange("p (k n) -> p k n", k=KC) for _ in range(2)]
            wbf = [self.alloc(KC * 128).rearrange("p (k n) -> p k n", k=KC) for _ in range(2)]
            rst = [Res("pwst%d" % i) for i in range(2)]
            rwb = [[Res("pwbf%d_%d" % (i, k)) for k in range(KC)] for i in range(2)]
            wv = I["peer_w_q"][l].rearrange("(k p) n -> p k n", p=128)
            engs = ["act", "dve", "pool"]
            for nb in range(16):
                b = nb % 2
                self.dma(wst[b], wv[:, :, nb * 128:(nb + 1) * 128], [], [rst[b]])
                self.P.op("act" if nb % 2 else "pool", (lambda e, o_=wbf[b], i_=wst[b]: e.activation(out=o_, in_=i_, func=AF.Copy)) if nb % 2 else
                          (lambda e, o_=wbf[b], i_=wst[b]: e.tensor_copy(out=o_, in_=i_)), [rst[b]], rwb[b])
                for ch in range(1):
                    a = nb
                    pb = 2 + a % 2
                    for k in range(KC):
                        self.mm(self.ps[pb][:, 0:TP], wbf[b][:, k, ch * 128:(ch + 1) * 128], hnT[:, k, :], k == 0, k == KC - 1,
                                [rwb[b][k]] + rhn, [self.rps[pb]])
                    self.cp("act" if a % 2 else "dve", qT[:, a, :], self.ps[pb][:, 0:TP], [self.rps[pb]], [rqT[a]])
            s0t = self.alloc(8 * 128, F32).rearrange("p (h n) -> p h n", h=8)
            top = self.alloc(16 * 16, F32).rearrange("p (a k) -> p a k", a=16)
            wk = self.alloc(256, F32)
            cand = self.alloc(8 * 256, F32).rearrange("p (h a b) -> p h a b", h=8, a=16)
            best = self.alloc(8 * 16, F32).rearrange("p (h k) -> p h k", h=8)
            eb_ = self.alloc(8 * 16, F32).rearrange("p (h k) -> p h k", h=8)
            sm = self.alloc(64, F32)
            rs0, rtop, rwk, rcand, rbest, rsm = Res("s0t"), Res("top"), Res("wk"), Res("cand"), Res("best"), Res("sm")
            for t in range(NTP):
                for g in range(4):
                    pb = 4 + g % 2
                    for j in range(4):
                        a = g * 4 + j
                        self.mm(self.ps[pb][:, j * 128:(j + 1) * 128], qT[:, a, t * 128:(t + 1) * 128], keysT[:, a, :], True, True,
                                [rqT[a], rkt], [self.rps[pb]])
                    p4 = self.ps[pb][:, 0:512].rearrange("p (h c n) -> p h c n", h=2, c=2)
                    self.cp("act", s0t[:, 2 * g:2 * g + 2, :], p4[:, :, 0, :], [self.rps[pb]], [rs0])
                    self.cp("act", s1b[:, t, 2 * g:2 * g + 2, :], p4[:, :, 1, :], [self.rps[pb]], [rs1[t]])
                for a in range(16):
                    src = s0t[:, a // 2, :] if a % 2 == 0 else s1b[:, t, a // 2, :]
                    rsrc_ = rs0 if a % 2 == 0 else rs1[t]
                    self.P.op("dve", lambda e, src=src, a=a: e.max(out=top[:, a, 0:8], in_=src), [rsrc_], [rtop])
                    self.P.op("dve", lambda e, src=src, a=a: e.match_replace(out=wk[:, 0:128], in_to_replace=top[:, a, 0:8], in_values=src, imm_value=-1e30),
                              [rsrc_, rtop], [rwk])
                    self.P.op("dve", lambda e, a=a: e.max(out=top[:, a, 8:16], in_=wk[:, 0:128]), [rwk], [rtop])
                top4 = top.rearrange("p (h c) k -> p h c k", c=2)
                in0 = top4[:, :, 0, :].unsqueeze(3).to_broadcast([128, 8, 16, 16])
                in1 = top4[:, :, 1, :].unsqueeze(2).to_broadcast([128, 8, 16, 16])
                self.tt("dve", cand, in0, in1, ALU.add, [rtop], [rcand])
                for h in range(8):
                    cf = cand[:, h].rearrange("p a b -> p (a b)")
                    self.P.op("dve", lambda e, cf=cf, h=h: e.max(out=best[:, h, 0:8], in_=cf), [rcand], [rbest])
                    self.P.op("dve", lambda e, cf=cf, h=h: e.match_replace(out=wk, in_to_replace=best[:, h, 0:8], in_values=cf, imm_value=-1e30),
                              [rcand, rbest], [rwk])
                    self.P.op("dve", lambda e, h=h: e.max(out=best[:, h, 8:16], in_=wk), [rwk], [rbest])
                self.cp("dve", sm[:, 0:8], best[:, :, 0], [rbest], [rsm])
                self.ts("dve", sm[:, 8:16], best[:, :, 15], -1e-5, None, ALU.add, None, [rbest], [rsm])
                self.tt("dve", eb_, best, sm[:, 0:8].unsqueeze(2).to_broadcast([128, 8, 16]), ALU.subtract, [rbest, rsm], [rbest])
                self.act(eb_, eb_, AF.Exp, [rbest], [rbest])
                self.P.op("dve", lambda e: e.tensor_reduce(out=sm[:, 16:24], in_=eb_, axis=AX.X, op=ALU.add), [rbest], [rsm])
                self.recip(sm[:, 24:32], sm[:, 16:24], [rsm], [rsm])
                self.tt("dve", sm[:, 32:40], sm[:, 8:16], sm[:, 0:8], ALU.subtract, [rsm], [rsm])
                self.act(sm[:, 32:40], sm[:, 32:40], AF.Exp, [rsm], [rsm])
                self.tt("dve", sm[:, 40:48], sm[:, 32:40], sm[:, 24:32], ALU.mult, [rsm], [rsm])
                self.tt("dve", phi[:, t].rearrange("p c h -> p h c"), s0t, sm[:, 8:16].unsqueeze(2).to_broadcast([128, 8, 128]), ALU.subtract,
                        [rs0, rsm], [rphi[t]])
                for h in range(8):
                    eng = "pool" if h % 2 else "dve"
                    self.ts(eng, Dm[:, t, h, :], self.ident, sm[:, 40 + h:41 + h], None, ALU.mult, None, [rsm, self.rconst], [rdm[t]])
                if self.dbg and p_ == 0 and t == 0:
                    self.dma(Sx["dbg_sm"], sm, [rsm], [], eng="pool")
                    self.dma(Sx["dbg_s0"], s0t.rearrange("p h n -> p (h n)"), [rs0], [], eng="pool")
                    self.dma(Sx["dbg_s1"], s1b[:, 0].rearrange("p h n -> p (h n)"), [rs1[0]], [], eng="pool")
            self.P.barrier()
            self.top = m2
            uT = [self.alloc(KC * 128).rearrange("p (k e) -> p k e", k=KC) for _ in range(2)]
            vbf = [self.alloc(2 * D).rearrange("p (c d) -> p c d", c=2) for _ in range(2)]
            wT = [self.alloc(2 * TP).rearrange("p (c t) -> p c t", c=2) for _ in range(2)]
            zz = [self.alloc(2048, F32) for _ in range(2)]
            E_ = [self.alloc(2048) for _ in range(2)]
            Gm = [self.alloc(2048) for _ in range(2)]
            ge = [self.alloc(TP) for _ in range(2)]
            ruT = [Res("uT%d" % i) for i in range(2)]
            rvbf = [[Res("vbf%d_%d" % (i, c)) for c in range(2)] for i in range(2)]
            rwT = [[Res("wT%d_%d" % (i, c)) for c in range(2)] for i in range(2)]
            rzz = [Res("zz%d" % i) for i in range(2)]
            rE = [Res("E%d" % i) for i in range(2)]
            rGm = [Res("Gm%d" % i) for i in range(2)]
            rge = [Res("ge%d" % i) for i in range(2)]
            zi = 0
            for cp_ in range(64):
                yg = cp_ % 2
                for cc in range(2):
                    c = 2 * cp_ + cc
                    self.dma(uT[cc].rearrange("p k e -> p (k e)"), self.ut_s[c], [], [ruT[cc]])
                    self.dma(vbf[yg][:, cc, :], self.v_s[c * 128:(c + 1) * 128, :], [], [rvbf[yg][cc]])
                    for k in range(KC):
                        self.mm(self.ps[cc][:, 0:TP], uT[cc][:, k, :], hnT[:, k, :], k == 0, k == KC - 1, [ruT[cc]] + rhn, [self.rps[cc]])
                    self.act(ge[cc], self.ps[cc][:, 0:TP], AF.Gelu, [self.rps[cc]], [rge[cc]])
                for t in range(NTP):
                    z = zi % 2
                    zi += 1
                    in0 = s1b[:, t].unsqueeze(1).to_broadcast([128, 2, 8, 128])
                    in1 = phi[:, t, 2 * cp_:2 * cp_ + 2, :].unsqueeze(3).to_broadcast([128, 2, 8, 128])
                    self.tt("dve", zz[z].rearrange("p (c h n) -> p c h n", c=2, h=8), in0, in1, ALU.add, [rs1[t], rphi[t]], [rzz[z]])
                    self.act(E_[z], zz[z], AF.Exp, [rzz[z]], [rE[z]])
                    self.stt(Gm[z], zz[z], 0.0, E_[z], ALU.is_ge, ALU.mult, [rzz[z], rE[z]], [rGm[z]])
                    for cc in range(2):
                        gb_ = 2 + cc
                        for h in range(8):
                            o_ = (cc * 8 + h) * 128
                            self.mm(self.ps[gb_][:, t * 128:(t + 1) * 128], Gm[z][:, o_:o_ + 128], Dm[:, t, h, :], h == 0, h == 7,
                                    [rGm[z], rdm[t]], [self.rps[gb_]])
                for cc in range(2):
                    self.tt("dve", wT[yg][:, cc, :], ge[cc], self.ps[2 + cc][:, 0:TP], ALU.mult, [rge[cc], self.rps[2 + cc]], [rwT[yg][cc]])
                if self.dbg and p_ == 0 and cp_ == 0:
                    self.dma(Sx["dbg_ge"], ge[0], [rge[0]], [], eng="pool")
                    self.dma(Sx["dbg_wt"], wT[yg][:, 0, :], [rwT[yg][0]], [], eng="pool")
                for t in range(NTP):
                    for d4 in range(4):
                        pb2 = 4 + (t * 4 + d4) % 4
                        for cc in range(2):
                            self.mm(self.ps[pb2][:, 0:512], wT[yg][:, cc, t * 128:(t + 1) * 128], vbf[yg][:, cc, d4 * 512:(d4 + 1) * 512],
                                    cc == 0, cc == 1, [rwT[yg][cc], rvbf[yg][cc]], [self.rps[pb2]])
                        dst = ytok[:, t, d4 * 512:(d4 + 1) * 512]
                        if cp_ == 0:
                            self.cp("act", dst, self.ps[pb2][:, 0:512], [self.rps[pb2]], [ryt[t][d4]])
                        else:
                            self.tt("dve", dst, self.ps[pb2][:, 0:512], dst, ALU.add, [self.rps[pb2], ryt[t][d4]], [ryt[t][d4]])
            if self.dbg and p_ == 0:
                self.dma(Sx["dbg_y"], ytok[:, 0, :], ryt[0], [], eng="pool")
            xt = zz[0]
            rxt = rzz[0]
            for t in range(NTP):
                self.dma(xt, rows(t), [], [rxt])
                self.tt("pool", xt, xt, ytok[:, t, :], ALU.add, [rxt] + ryt[t], [rxt])
                self.dma(rows(t), xt, [rxt], [], eng="pool")
            self.P.barrier()
        self.top = m
        self.P.barrier()

    def build(self):
        self.setup_consts()
        ph = self.phases
        for l in self.layers:
            self.build_ebb(l)
            if "E" in ph:
                self.phase_P(l)
            for s in range(self.nseq):
                xsrc = self.I["x"][s] if l == self.layers[0] else self.out[s]
                xo = self.out[s]
                if "A" in ph:
                    self.phase_A(l, xsrc)
                if "B" in ph:
                    self.mixer_AC(l, "a")
                    self.mixer_B(l)
                    self.mixer_AC(l, "c")
                if "C" in ph:
                    self.phase_C(l, xsrc, xo, l != self.layers[0])
                if "D" in ph:
                    self.phase_D(l, s, xo)
                if "E" in ph:
                    self.phase_E(l, xo)
        self.P.barrier()
        self.P.finalize(self.st)
        self.P.emit()
        self.st.close()
        return self.nc


def build_program(nseq=2, layers=(0, 1), phases="ABCDE", dbg=False, lw=L):
    nc = bass.Bass("TRN2", target_bir_lowering=False)
    kb = KB(nc, nseq, list(layers), phases, dbg, lw)
    kb.build()
    return nc, kb


def make_in_maps(inputs, nseq=2, ncores=8, small_peer=False):
    tabs = _static_tables()
    maps = []
    w = {name: np.ascontiguousarray(np.asarray(inputs[name], dtype=np.float32)) for name, _ in WEIGHT_SPECS}
    if small_peer:
        w["peer_u"] = np.ascontiguousarray(w["peer_u"][:, :128])
        w["peer_v"] = np.ascontiguousarray(w["peer_v"][:, :128])
    x = np.asarray(inputs["x"], dtype=np.float32)
    mem = np.asarray(inputs["mem"], dtype=np.float32)
    for c in range(ncores):
        m = dict(w)
        m.update(tabs)
        m["x"] = np.ascontiguousarray(x[2 * c:2 * c + nseq])
        m["mem"] = np.ascontiguousarray(mem[2 * c:2 * c + nseq])
        maps.append(m)
    return maps


def kernel(**inputs):
    nc, _ = build_program(nseq=2, layers=(0, 1), lw=L)
    maps = make_in_maps(inputs)
    res = run_bass_kernel_spmd(nc, maps, core_ids=list(range(8)))
    return np.concatenate([np.asarray(r["out"], dtype=np.float32) for r in res.results], axis=0)
```

```python
import math
from contextlib import ExitStack

import ml_dtypes
import numpy as np

import concourse.bass as bass
import concourse.mybir as mybir
from concourse.bass_utils import run_bass_kernel_spmd

F32 = mybir.dt.float32
BF16 = mybir.dt.bfloat16
ALU = mybir.AluOpType
AF = mybir.ActivationFunctionType
AX = mybir.AxisListType

D = 2048
S = 2048
NT = 16
KC = 16
L = 2
NMEM = 256
EPS = 1e-6
N_DMA_SEMS = 24
SAME_ENG_SYNC = True
ARENA = 105000


class Res:
    __slots__ = ("name", "lw", "rs")

    def __init__(self, name=""):
        self.name = name
        self.lw = None
        self.rs = []


class Op:
    __slots__ = ("eng", "fn", "deps", "signal", "sem", "val", "ndma", "clock", "gi")


class Prog:
    ENGS = ["pe", "act", "dve", "pool", "sp"]

    def __init__(self, nc):
        self.nc = nc
        self.ops = []
        self.last = {e: None for e in self.ENGS}
        self.dmas_since_barrier = []

    def op(self, eng, fn, reads=(), writes=(), ndma=0):
        o = Op()
        o.eng = eng
        o.fn = fn
        o.ndma = ndma
        o.signal = ndma > 0
        o.sem = None
        o.val = None
        o.clock = None
        o.gi = len(self.ops)
        deps = {}
        for r in reads:
            if r.lw is not None:
                deps[r.lw.gi] = r.lw
        for w in writes:
            if w.lw is not None:
                deps[w.lw.gi] = w.lw
            for x in w.rs:
                deps[x.gi] = x
        best = {}
        dl = []
        for d in deps.values():
            if d.ndma:
                dl.append(d)
                continue
            if d.eng == o.eng and (o.eng == "pe" or not SAME_ENG_SYNC):
                continue
            b = best.get(d.eng)
            if b is None or b.gi < d.gi:
                best[d.eng] = d
        for d in best.values():
            d.signal = True
            dl.append(d)
        o.deps = dl
        for r in reads:
            r.rs.append(o)
        for w in writes:
            w.lw = o
            w.rs = []
        self.ops.append(o)
        self.last[eng] = o
        if ndma:
            self.dmas_since_barrier.append(o)
        return o

    def barrier(self):
        toks = []
        for e in self.ENGS:
            if self.last[e] is not None:
                r = Res("bar_" + e)
                self.op(e, lambda eng: eng.drain(), writes=[r])
                toks.append(r)
        dm = list(self.dmas_since_barrier)
        self.dmas_since_barrier = []
        for e in self.ENGS:
            o = self.op(e, lambda eng: eng.drain(), reads=toks)
            for d in dm:
                o.deps.append(d)

    def finalize(self, stack):
        nc = self.nc
        engsem = {e: stack.enter_context(nc.semaphore("s_" + e)) for e in self.ENGS}
        dmasems = {
            e: [stack.enter_context(nc.semaphore("d_%s_%d" % (e, i))) for i in range(N_DMA_SEMS)]
            for e in ("sp", "act", "pool")
        }
        prev = {e: [None] * N_DMA_SEMS for e in dmasems}
        sval = {e: [0] * N_DMA_SEMS for e in dmasems}
        dcnt = {e: 0 for e in dmasems}
        cnt = {e: 0 for e in self.ENGS}
        clock = {e: {} for e in self.ENGS}
        lists = {e: [] for e in self.ENGS}
        for o in self.ops:
            ck = clock[o.eng]
            deps = list(o.deps)
            if o.ndma:
                slot = dcnt[o.eng] % N_DMA_SEMS
                dcnt[o.eng] += 1
                p = prev[o.eng][slot]
                if p is not None:
                    deps.append(p)
                o.sem = dmasems[o.eng][slot]
                sval[o.eng][slot] += 16 * o.ndma
                o.val = sval[o.eng][slot]
                prev[o.eng][slot] = o
            waits = {}
            for d in deps:
                key = id(d.sem)
                if ck.get(key, 0) >= d.val:
                    continue
                if key not in waits or waits[key][1] < d.val:
                    waits[key] = (d.sem, d.val)
            for d in deps:
                if d.clock:
                    for k, v in d.clock.items():
                        if ck.get(k, 0) < v:
                            ck[k] = v
            for key, (s, v) in waits.items():
                if ck.get(key, 0) < v:
                    ck[key] = v
            wl = list(waits.values())
            if o.ndma == 0 and o.signal:
                cnt[o.eng] += 1
                o.sem = engsem[o.eng]
                o.val = cnt[o.eng]
            if o.signal:
                c = dict(ck)
                if o.ndma == 0:
                    c[id(o.sem)] = o.val
                o.clock = c
            lists[o.eng].append((wl, o))
        self.lists = lists

    def emit(self):
        nc = self.nc
        lists = self.lists

        def run(eng, items):
            for wl, o in items:
                for s, v in wl:
                    eng.wait_ge(s, v)
                if o.ndma:
                    o.fn(eng, o.sem)
                else:
                    ins = o.fn(eng)
                    if o.signal:
                        ins.then_inc(o.sem, 1)

        with nc.Block() as block:
            @block.tensor
            def _(e):
                run(e, lists["pe"])

            @block.scalar
            def _(e):
                run(e, lists["act"])

            @block.vector
            def _(e):
                run(e, lists["dve"])

            @block.gpsimd
            def _(e):
                run(e, lists["pool"])

            @block.sync
            def _(e):
                run(e, lists["sp"])


def _t5_buckets(rel):
    nb = 16
    max_exact = nb // 2
    n = np.abs(rel)
    large = max_exact + (np.log(np.maximum(n, 1) / max_exact) / np.log(128 / max_exact) * (nb - max_exact)).astype(np.int64)
    large = np.minimum(large, nb - 1)
    return np.where(rel > 0, nb, 0) + np.where(n < max_exact, n, large)


def _static_tables():
    t = {}
    t["c_ident"] = np.eye(128, dtype=np.float32).astype(ml_dtypes.bfloat16)
    t["c_j"] = np.ascontiguousarray(np.eye(128, dtype=np.float32)[::-1])
    delta = np.arange(512) - 255
    bk = _t5_buckets(delta)
    oh = np.zeros((32, 512), np.float32)
    oh[bk, np.arange(512)] = 1.0
    t["c_oh"] = oh
    k = np.arange(128)[:, None, None]
    ri = np.arange(3)[None, :, None]
    q = np.arange(128)[None, None, :]
    dd = k + 128 * (ri - 1) - q
    t["c_maska"] = (np.abs(dd) <= 128).astype(np.float32).astype(ml_dtypes.bfloat16)
    kc = np.arange(64)[:, None]
    qc = np.arange(64)[None, :]
    cs = np.clip(qc - 8, 0, 48)
    cv = ((kc >= cs) & (kc < cs + 16)).astype(np.float32)
    t["c_colv"] = np.concatenate([cv, cv], axis=0).astype(ml_dtypes.bfloat16)
    tok = np.arange(S)
    row = (tok // 64).astype(np.float32)
    col = (tok % 64).astype(np.float32)
    inv = (10000.0 ** (-np.arange(16) * 2.0 / 32)).astype(np.float32)
    ar = row[:, None] * inv[None, :]
    ac = col[:, None] * inv[None, :]
    cc = np.concatenate([np.cos(ar), np.cos(ar), np.cos(ac), np.cos(ac)], axis=1)
    ss = np.concatenate([-np.sin(ar), np.sin(ar), -np.sin(ac), np.sin(ac)], axis=1)
    t["c_ropec"] = cc.astype(np.float32)
    t["c_ropes"] = ss.astype(np.float32)
    t["c_zero"] = np.zeros((8, 1152), np.float32)
    return t


WEIGHT_SPECS = [
    ("t5_rel_bias", (32, 12)), ("norm_mix", (L, D)), ("w_in", (L, D, 4096)), ("qk_norm_a", (L, 2, 64)),
    ("sink_a", (L, 12)), ("qk_norm_b", (L, 2, 64)), ("rpb_b", (L, 8, 15, 31)), ("qk_norm_c", (L, 2, 64)),
    ("out_norm", (L, D)), ("w_out", (L, D, D)), ("norm_mem", (L, D)), ("norm_mem_kv", (L, D)),
    ("w_mem_q", (L, D, 512)), ("w_mem_kv", (L, D, 1024)), ("qk_norm_mem", (L, 2, 128)), ("w_mem_o", (L, 512, D)),
    ("norm_ffn", (L, D)), ("peer_w_q", (L, D, D)), ("peer_keys", (L, 8, 2, 128, 128)),
    ("peer_u", (L, 16384, D)), ("peer_v", (L, 16384, D)),
]


class KB:
    def __init__(self, nc, nseq, layers, phases, dbg, lw=L):
        self.nc = nc
        self.lw = lw
        self.P = Prog(nc)
        self.nseq = nseq
        self.layers = layers
        self.phases = phases
        self.dbg = dbg
        self.st = ExitStack()
        st = self.st
        self.I = {}
        self.I["x"] = nc.dram_tensor("x", [nseq, S, D], F32, kind="ExternalInput").ap()
        self.I["mem"] = nc.dram_tensor("mem", [nseq, NMEM, D], F32, kind="ExternalInput").ap()
        for name, shp in WEIGHT_SPECS:
            if name != "t5_rel_bias":
                shp = (lw,) + tuple(shp[1:])
            if name in ("peer_u", "peer_v") and "E" not in phases:
                shp = (lw, 128, D)
            self.I[name] = nc.dram_tensor(name, list(shp), F32, kind="ExternalInput").ap()
        for name, arr in _static_tables().items():
            dt = BF16 if arr.dtype == ml_dtypes.bfloat16 else F32
            self.I[name] = nc.dram_tensor(name, list(arr.shape), dt, kind="ExternalInput").ap()
        self.out = nc.dram_tensor("out", [nseq, S, D], F32, kind="ExternalOutput").ap()
        kind = "ExternalOutput" if dbg else "Internal"
        self.Sx = {}

        def scr(name, shp, dt):
            self.Sx[name] = nc.dram_tensor(name, list(shp), dt, kind=kind).ap()

        scr("qt_a", [12, 64, S], BF16)
        scr("kt_a", [4, 64, S], BF16)
        scr("v_a", [S, 4, 65], BF16)
        scr("qt_b", [8, 64, S], BF16)
        scr("kt_b", [8, 64, S], BF16)
        scr("v_b", [S, 8, 65], BF16)
        scr("qt_c", [12, 64, S], BF16)
        scr("kt_c", [4, 64, S], BF16)
        scr("v_c", [S, 4, 65], BF16)
        scr("mixed", [S, D], BF16)
        if dbg:
            scr("dbg_sm", [128, 64], F32)
            scr("dbg_s0", [128, 1024], F32)
            scr("dbg_s1", [128, 1024], F32)
            scr("dbg_ge", [128, 512], BF16)
            scr("dbg_wt", [128, 512], BF16)
            scr("dbg_y", [128, 2048], F32)
        scr("tv", [12, 512], F32)
        self.ut_s = nc.dram_tensor("ut_s", [128, 128, KC * 128], BF16, kind="Internal").ap()
        self.v_s = nc.dram_tensor("v_s", [16384, D], BF16, kind="Internal").ap()
        scr("wb", [8, 1152], F32)
        self.arena = st.enter_context(nc.sbuf_tensor("arena", [128, ARENA], BF16))
        self.ps = [st.enter_context(nc.psum_tensor("ps%d" % i, [128, 512], F32)) for i in range(8)]
        self.rps = [Res("ps%d" % i) for i in range(8)]
        self.top = 0
        self.dma_rr = 0

    def alloc(self, n, dt=BF16):
        nb = n if dt == BF16 else 2 * n
        self.top = (self.top + 31) // 32 * 32
        a = self.top
        self.top += nb
        assert self.top <= ARENA, "arena overflow %d" % self.top
        ap = self.arena[:, a:a + nb]
        if dt == F32:
            ap = ap.bitcast(F32)
        return ap

    def mm(self, out, lhsT, rhs, start, stop, rd, wr):
        return self.P.op("pe", lambda e: e.matmul(out, lhsT=lhsT, rhs=rhs, start=start, stop=stop), rd, wr)

    def tr(self, out, in_, rd, wr):
        ident = self.ident
        return self.P.op("pe", lambda e: e.transpose(out=out, in_=in_, identity=ident), list(rd) + [self.rconst], wr)

    def act(self, out, in_, func, rd, wr, scale=None, accum=None):
        kw = {}
        if scale is not None:
            kw["scale"] = scale
        if accum is not None:
            kw["accum_out"] = accum
        return self.P.op("act", lambda e: e.activation(out=out, in_=in_, func=func, **kw), rd, wr)

    def tt(self, eng, out, in0, in1, op, rd, wr):
        return self.P.op(eng, lambda e: e.tensor_tensor(out=out, in0=in0, in1=in1, op=op), rd, wr)

    def ts(self, eng, out, in0, s1, s2, op0, op1, rd, wr):
        if op1 is None:
            return self.P.op(eng, lambda e: e.tensor_scalar(out=out, in0=in0, scalar1=s1, scalar2=None, op0=op0), rd, wr)
        return self.P.op(eng, lambda e: e.tensor_scalar(out=out, in0=in0, scalar1=s1, scalar2=s2, op0=op0, op1=op1), rd, wr)

    def stt(self, out, in0, scalar, in1, op0, op1, rd, wr):
        return self.P.op("dve", lambda e: e.scalar_tensor_tensor(out=out, in0=in0, scalar=scalar, in1=in1, op0=op0, op1=op1), rd, wr)

    def cp(self, eng, out, in_, rd, wr):
        if eng == "act":
            return self.act(out, in_, AF.Copy, rd, wr)
        return self.P.op(eng, lambda e: e.tensor_copy(out=out, in_=in_), rd, wr)

    def recip(self, out, in_, rd, wr):
        return self.P.op("dve", lambda e: e.reciprocal(out=out, in_=in_), rd, wr)

    def memset(self, eng, ap, val, wr):
        return self.P.op(eng, lambda e: e.memset(ap, val), [], wr)

    def dma(self, out, in_, rd, wr, eng=None, slow=False):
        if eng is None:
            eng = "sp"
        if slow:
            fn = lambda e, s: e.dma_start(out=out, in_=in_, allow_slow_non_contiguous=True).then_inc(s, 16)
        else:
            fn = lambda e, s: e.dma_start(out=out, in_=in_).then_inc(s, 16)
        return self.P.op(eng, fn, rd, wr, ndma=1)

    def rstd_from_ssq(self, st, n, rd_wr):
        self.ts("dve", st[:, 1:2], st[:, 0:1], 1.0 / n, EPS, ALU.mult, ALU.add, [rd_wr], [rd_wr])
        self.act(st[:, 2:3], st[:, 1:2], AF.Sqrt, [rd_wr], [rd_wr])
        self.recip(st[:, 3:4], st[:, 2:3], [rd_wr], [rd_wr])

    def setup_consts(self):
        I = self.I
        self.rconst = Res("const")
        rc = self.rconst
        self.ident = self.alloc(128)
        self.jf = self.alloc(128, F32)
        self.maska = self.alloc(3 * 128)
        self.colv = self.alloc(64)
        self.ropec = self.alloc(NT * 64, F32).rearrange("p (t e) -> p t e", t=NT)
        self.ropes = self.alloc(NT * 64, F32).rearrange("p (t e) -> p t e", t=NT)
        self.eba = self.alloc(3 * 12 * 128).rearrange("p (r h q) -> p r h q", r=3, h=12)
        self.ebb = self.alloc(8 * 14 * 64).rearrange("p (h r q) -> p h r q", h=8, r=14)
        self.gq = {}
        self.dma(self.ident, I["c_ident"], [], [rc])
        self.dma(self.jf, I["c_j"], [], [rc])
        self.dma(self.maska, I["c_maska"].rearrange("k r q -> k (r q)"), [], [rc])
        self.dma(self.colv, I["c_colv"], [], [rc])
        self.dma(self.ropec, I["c_ropec"].rearrange("(t p) e -> p t e", p=128), [], [rc])
        self.dma(self.ropes, I["c_ropes"].rearrange("(t p) e -> p t e", p=128), [], [rc])
        LW = self.lw
        self.gains = self.alloc(LW * (6 * 64 + 2 * 128), F32)
        self.esink = self.alloc(LW * 12, F32)
        self.gcol = self.alloc(LW * 5 * KC, F32).rearrange("p (l g k) -> p l g k", l=LW, g=5)
        self.g = {}
        off = 0
        for l in range(LW):
            for nm, n2 in (("qk_norm_a", 64), ("qk_norm_b", 64), ("qk_norm_c", 64), ("qk_norm_mem", 128)):
                for i in range(2):
                    dst = self.gains[:, off:off + n2]
                    src = I[nm][l, i:i + 1, :]
                    src = bass.AP(src.tensor, src.offset, [[0, 128], [1, n2]])
                    self.dma(dst, src, [], [rc])
                    self.g[(nm, l, i)] = dst
                    off += n2
            src = I["sink_a"][l:l + 1, :]
            src = bass.AP(src.tensor, src.offset, [[0, 128], [1, 12]])
            self.dma(self.esink[:, l * 12:(l + 1) * 12], src, [], [rc])
            for gi, nm in enumerate(("norm_mix", "out_norm", "norm_mem", "norm_mem_kv", "norm_ffn")):
                src = I[nm][l]
                src = bass.AP(src.tensor, src.offset, [[1, 128], [128, KC]])
                self.dma(self.gcol[:, l, gi, :], src, [], [rc], slow=True)
        self.act(self.esink, self.esink, AF.Exp, [rc], [rc])
        self.build_eba()
        self.const_top = self.top

    def build_eba(self):
        I = self.I
        rc = self.rconst
        m = self.top
        relb = self.alloc(12, F32)
        oh = self.alloc(512, F32)
        tvs = self.alloc(512, F32)
        ht = self.alloc(3 * 128, F32).rearrange("p (r k) -> p r k", r=3)
        tmp = self.alloc(128)
        r1, r2, r3, r4 = Res("relb"), Res("tvs"), Res("tvd"), Res("ht")
        self.dma(relb[0:32, :], I["t5_rel_bias"], [], [r1])
        self.dma(oh[0:32, :], I["c_oh"], [], [r1])
        self.mm(self.ps[0][0:12, :], relb[0:32, :], oh[0:32, :], True, True, [r1], [self.rps[0]])
        self.act(tvs[0:12, :], self.ps[0][0:12, :], AF.Copy, [self.rps[0]], [r2])
        self.dma(self.Sx["tv"], tvs[0:12, :], [r2], [r3], eng="pool")
        tvt = self.Sx["tv"].tensor
        rtmp = Res("tmp")
        for h in range(12):
            src = bass.AP(tvt, h * 512, [[1, 128], [128, 3], [1, 128]])
            self.dma(ht, src, [r3], [r4])
            for r in range(3):
                pb = 1 + (h * 3 + r) % 2
                self.mm(self.ps[pb][:, 0:128], ht[:, r, :], self.jf, True, True, [r4, rc], [self.rps[pb]])
                self.act(tmp, self.ps[pb][:, 0:128], AF.Exp, [self.rps[pb]], [rtmp])
                self.tt("dve", self.eba[:, r, h, :], tmp, self.maska[:, r * 128:(r + 1) * 128], ALU.mult, [rtmp, rc], [rc])
        self.P.barrier()
        self.top = m

    def build_ebb(self, l):
        I = self.I
        rc = self.rconst
        m = self.top
        htb = self.alloc(14 * 128, F32).rearrange("p (r k) -> p r k", r=14)
        tmp = self.alloc(7 * 64)
        r1, r2, r3 = Res("wbz"), Res("htb"), Res("tmpb")
        wbt = self.Sx["wb"].tensor
        self.dma(self.Sx["wb"], I["c_zero"], [], [r1])
        for h in range(8):
            dst = bass.AP(wbt, h * 1152 + 48, [[64, 15], [1, 31]])
            self.dma(dst, I["rpb_b"][l, h], [r1], [r1])
        j64 = self.jf[0:64, 64:128]
        for h in range(8):
            src = bass.AP(wbt, h * 1152, [[1, 64], [64, 14], [1, 128]])
            self.dma(htb[0:64], src, [r1], [r2])
            for half in range(2):
                pb = 1 + half
                for r in range(7):
                    self.mm(self.ps[pb][:, r * 64:(r + 1) * 64], htb[0:64, half * 7 + r, :], j64, True, True,
                            [r2, rc], [self.rps[pb]])
                self.act(tmp, self.ps[pb][:, 0:448], AF.Exp, [self.rps[pb]], [r3])
                colb = bass.AP(self.colv.tensor, self.colv.offset, [list(self.colv.ap[0]), [0, 7], [1, 64]])
                self.tt("dve", self.ebb[:, h, half * 7:(half + 1) * 7, :], tmp.rearrange("p (r q) -> p r q", r=7), colb,
                        ALU.mult, [r3, rc], [rc])
        self.P.barrier()
        self.top = m

    def norm_transpose(self, src_rows, ntiles, dstT, rdst, rsrc=None, pbase=0, gcol=None):
        m = self.top
        xt = [self.alloc(D, F32) for _ in range(2)]
        xn = [self.alloc(D) for _ in range(2)]
        junk = self.alloc(D)
        stt_ = [self.alloc(8, F32) for _ in range(2)]
        rx = [Res("xt%d" % i) for i in range(2)]
        rn = [Res("xn%d" % i) for i in range(2)]
        rs = [Res("st%d" % i) for i in range(2)]
        rj = Res("junk")
        for t in range(ntiles):
            b = t % 2
            self.dma(xt[b], src_rows(t), [rsrc[t]] if rsrc else [], [rx[b]])
            self.act(junk, xt[b], AF.Square, [rx[b]], [rj, rs[b]], accum=stt_[b][:, 0:1])
            self.rstd_from_ssq(stt_[b], D, rs[b])
            self.act(xn[b], xt[b], AF.Copy, [rx[b], rs[b]], [rn[b]], scale=stt_[b][:, 3:4])
            for g in range(4):
                pb = pbase + g % 2
                pv = self.ps[pb][:, 0:256].bitcast(BF16)
                for j in range(4):
                    kc = g * 4 + j
                    self.tr(pv[:, j * 128:(j + 1) * 128], xn[b][:, kc * 128:(kc + 1) * 128], [rn[b]], [self.rps[pb]])
                eng = "dve" if g % 2 == 0 else "act"
                if gcol is None:
                    self.cp(eng, dstT[:, g * 4:(g + 1) * 4, t * 128:(t + 1) * 128], pv.rearrange("p (j q) -> p j q", j=4),
                            [self.rps[pb]], [rdst[t]])
                else:
                    for j in range(4):
                        kc = g * 4 + j
                        if (kc % 2) == 0:
                            self.act(dstT[:, kc, t * 128:(t + 1) * 128], pv[:, j * 128:(j + 1) * 128], AF.Copy, [self.rps[pb], self.rconst],
                                     [rdst[t]], scale=gcol[:, kc:kc + 1])
                        else:
                            self.ts("dve", dstT[:, kc, t * 128:(t + 1) * 128], pv[:, j * 128:(j + 1) * 128], gcol[:, kc:kc + 1], None,
                                    ALU.mult, None, [self.rps[pb], self.rconst], [rdst[t]])
        self.P.barrier()
        self.top = m

    def linear(self, actT, ract, ntiles, w_dram, kcn, ncols, nblk, gcol, epilogue, pbase=2):
        m = self.top
        wst = [self.alloc(kcn * nblk, F32).rearrange("p (k n) -> p k n", k=kcn) for _ in range(2)]
        wbf = [self.alloc(kcn * nblk).rearrange("p (k n) -> p k n", k=kcn) for _ in range(2)]
        rst = [Res("wst%d" % i) for i in range(2)]
        rwb = [[Res("wbf%d_%d" % (i, k)) for k in range(kcn)] for i in range(2)]
        wv = w_dram.rearrange("(k p) n -> p k n", p=128)
        engs = ["act", "dve", "pool"]
        for nb in range(ncols // nblk):
            b = nb % 2
            self.dma(wst[b], wv[:, :, nb * nblk:(nb + 1) * nblk], [], [rst[b]])
            for k in range(kcn):
                eng = engs[k % 3]
                if gcol is None:
                    self.cp(eng, wbf[b][:, k, :], wst[b][:, k, :], [rst[b]], [rwb[b][k]])
                elif eng == "act":
                    self.act(wbf[b][:, k, :], wst[b][:, k, :], AF.Copy, [rst[b], self.rconst], [rwb[b][k]], scale=gcol[:, k:k + 1])
                else:
                    self.ts(eng, wbf[b][:, k, :], wst[b][:, k, :], gcol[:, k:k + 1], None, ALU.mult, None,
                            [rst[b], self.rconst], [rwb[b][k]])
            for t in range(ntiles):
                pb = pbase + t % 2
                ps = self.ps[pb][:, 0:nblk]
                for k in range(kcn):
                    self.mm(ps, actT[:, k, t * 128:(t + 1) * 128], wbf[b][:, k, :], k == 0, k == kcn - 1,
                            [ract[t], rwb[b][k]], [self.rps[pb]])
                epilogue(nb, t, ps, self.rps[pb])
        self.P.barrier()
        self.top = m

    def headnorm(self, ps, rps, nh, dh, gain, sc):
        sq, ss, qn, qg = sc["sq"], sc["ss"], sc["qn"], sc["qg"]
        r = sc["r"]
        n = nh * dh
        self.act(sq[:, 0:n], ps, AF.Square, [rps], [r["sq"]])
        self.P.op("dve", lambda e: e.tensor_reduce(out=ss[:, 0:nh], in_=sq[:, 0:n].rearrange("p (h d) -> p h d", h=nh), axis=AX.X, op=ALU.add),
                  [r["sq"]], [r["ss"]])
        self.ts("dve", ss[:, 8:8 + nh], ss[:, 0:nh], 1.0 / dh, EPS, ALU.mult, ALU.add, [r["ss"]], [r["ss"]])
        self.act(ss[:, 16:16 + nh], ss[:, 8:8 + nh], AF.Sqrt, [r["ss"]], [r["ss"]])
        self.recip(ss[:, 24:24 + nh], ss[:, 16:16 + nh], [r["ss"]], [r["ss"]])
        rsb = ss[:, 24:24 + nh].unsqueeze(2).to_broadcast([128, nh, dh])
        self.tt("dve", qn[:, 0:n].rearrange("p (h d) -> p h d", h=nh), ps.rearrange("p (h d) -> p h d", h=nh), rsb, ALU.mult,
                [rps, r["ss"]], [r["qn"]])
        gb = gain.unsqueeze(1).to_broadcast([128, nh, dh])
        return gb

    def phase_A(self, l, xsrc):
        I, Sx = self.I, self.Sx
        m = self.top
        xnT = self.alloc(KC * S).rearrange("p (k t) -> p k t", k=KC)
        rxn = [Res("xnT%d" % t) for t in range(NT)]
        self.norm_transpose(lambda t: xsrc[t * 128:(t + 1) * 128, :], NT, xnT, rxn)
        sc = {
            "sq": self.alloc(256, F32), "ss": self.alloc(32, F32), "qn": self.alloc(256, F32), "qg": self.alloc(256, F32),
            "t1": self.alloc(256, F32), "t2": self.alloc(256, F32),
            "r": {k: Res(k) for k in ("sq", "ss", "qn", "qg", "t1", "t2")},
        }
        qb = [self.alloc(256) for _ in range(2)]
        rqb = [Res("qb%d" % i) for i in range(2)]
        hT = [self.alloc(4 * S).rearrange("p (h t) -> p h t", h=4) for _ in range(2)]
        rhT = [Res("hT%d" % i) for i in range(2)]
        vst = [self.alloc(NT * 4 * 65).rearrange("p (t h e) -> p t h e", t=NT, h=4) for _ in range(1)]
        rvs = [Res("vst%d" % i) for i in range(1)]
        for i in range(1):
            self.memset("pool", vst[i][:, :, :, 64:65], 1.0, [rvs[i]])
        blocks = [("q", "qt_a", 0, ("qk_norm_a", 0), False), ("q", "qt_a", 4, ("qk_norm_a", 0), False), ("q", "qt_a", 8, ("qk_norm_a", 0), False),
                  ("q", "kt_a", 0, ("qk_norm_a", 1), False), ("v", "v_a", 0, None, False),
                  ("q", "qt_b", 0, ("qk_norm_b", 0), False), ("q", "qt_b", 4, ("qk_norm_b", 0), False),
                  ("q", "kt_b", 0, ("qk_norm_b", 1), False), ("q", "kt_b", 4, ("qk_norm_b", 1), False),
                  ("v", "v_b", 0, None, False), ("v", "v_b", 4, None, False),
                  ("q", "qt_c", 0, ("qk_norm_c", 0), True), ("q", "qt_c", 4, ("qk_norm_c", 0), True), ("q", "qt_c", 8, ("qk_norm_c", 0), True),
                  ("q", "kt_c", 0, ("qk_norm_c", 1), True), ("v", "v_c", 0, None, False)]
        cnt = {"q": 0, "v": 0, "e": 0}

        def epi(nb, t, ps, rps):
            kind, dst, h0, gk, rope = blocks[nb]
            if kind == "v":
                vb = 0
                cnt["v"] += 1
                self.cp("act", vst[vb][:, t, :, 0:64], ps.rearrange("p (h d) -> p h d", h=4), [rps], [rvs[vb]])
                if t == NT - 1:
                    dv = Sx[dst].rearrange("(t p) h e -> p t h e", p=128)[:, :, h0:h0 + 4, :]
                    self.dma(dv, vst[vb], [rvs[vb]], [], eng="pool")
                return
            hb = (cnt["q"] // NT) % 2
            cnt["q"] += 1
            e = cnt["e"] % 2
            cnt["e"] += 1
            gain = self.g[(gk[0], l, gk[1])]
            gb = self.headnorm(ps, rps, 4, 64, gain, sc)
            r = sc["r"]
            qn3 = sc["qn"].rearrange("p (h d) -> p h d", h=4)
            if not rope:
                self.tt("dve", qb[e].rearrange("p (h d) -> p h d", h=4), qn3, gb, ALU.mult, [r["qn"], self.rconst], [rqb[e]])
            else:
                qg = sc["qg"]
                self.tt("dve", qg.rearrange("p (h d) -> p h d", h=4), qn3, gb, ALU.mult, [r["qn"], self.rconst], [r["qg"]])
                cc = self.ropec[:, t, :].unsqueeze(1).to_broadcast([128, 4, 64])
                self.tt("dve", sc["t1"].rearrange("p (h d) -> p h d", h=4), qg.rearrange("p (h d) -> p h d", h=4), cc, ALU.mult,
                        [r["qg"], self.rconst], [r["t1"]])
                qg5 = qg.rearrange("p (a x f) -> p a x f", x=2, f=16)
                t25 = sc["t2"].rearrange("p (a x f) -> p a x f", x=2, f=16)
                ss5 = self.ropes[:, t, :].rearrange("p (a x f) -> p a x f", x=2, f=16)
                for x in range(2):
                    src = qg5[:, :, 1 - x, :].rearrange("p (h a) f -> p h a f", h=4)
                    dst_ = t25[:, :, x, :].rearrange("p (h a) f -> p h a f", h=4)
                    sb = ss5[:, :, x, :].unsqueeze(1).to_broadcast([128, 4, 2, 16])
                    self.tt("dve", dst_, src, sb, ALU.mult, [r["qg"], self.rconst], [r["t2"]])
                self.tt("dve", qb[e], sc["t1"], sc["t2"], ALU.add, [r["t1"], r["t2"]], [rqb[e]])
            pb = 4 + e
            pv = self.ps[pb][:, 0:256].bitcast(BF16)
            for j in range(4):
                self.tr(pv[0:64, j * 128:(j + 1) * 128], qb[e][:, j * 64:(j + 1) * 64], [rqb[e]], [self.rps[pb]])
            self.cp("act", hT[hb][0:64, :, t * 128:(t + 1) * 128], pv[0:64, :].rearrange("p (j q) -> p j q", j=4),
                    [self.rps[pb]], [rhT[hb]])
            if t == NT - 1:
                dv = Sx[dst][h0:h0 + 4].rearrange("h d t -> d h t")
                self.dma(dv, hT[hb][0:64], [rhT[hb]], [], eng="pool")

        self.linear(xnT, rxn, NT, I["w_in"][l], KC, 4096, 256, self.gcol[:, l, 0, :], epi)
        self.top = m
        self.P.barrier()

    def outnorm_store(self, o_f32, n, parts, dst, sc):
        r = sc["r"]
        self.act(sc["junk"][0:parts, 0:n], o_f32, AF.Square, [r["o"]], [r["junk"], r["st"]], accum=sc["st"][0:parts, 0:1])
        self.rstd_from_ssq(sc["st"][0:parts], n, r["st"])
        self.act(sc["ob"][0:parts, 0:n], o_f32, AF.Copy, [r["o"], r["st"]], [r["ob"]], scale=sc["st"][0:parts, 3:4])
        self.dma(dst, sc["ob"][0:parts, 0:n], [r["ob"]], [], eng="pool")

    def mixer_AC(self, l, which):
        I, Sx = self.I, self.Sx
        m = self.top
        qn_, kn_, vn_ = ("qt_a", "kt_a", "v_a") if which == "a" else ("qt_c", "kt_c", "v_c")
        QT = self.alloc(12 * S).rearrange("p (h t) -> p h t", h=12)
        KT = self.alloc(4 * S).rearrange("p (h t) -> p h t", h=4)
        V = self.alloc(NT * 4 * 65).rearrange("p (t h e) -> p t h e", t=NT, h=4)
        rq, rk, rv = Res("QT"), Res("KT"), Res("V")
        self.dma(QT[0:64], Sx[qn_].rearrange("h d t -> d h t"), [], [rq])
        self.dma(KT[0:64], Sx[kn_].rearrange("h d t -> d h t"), [], [rk])
        self.dma(V, Sx[vn_].rearrange("(t p) h e -> p t h e", p=128), [], [rv])
        NR = 2
        Pe = [self.alloc(384) for _ in range(NR)]
        Pm = [self.alloc(384) for _ in range(NR)]
        rpe = [Res("pe%d" % i) for i in range(NR)]
        rpm = [Res("pm%d" % i) for i in range(NR)]
        scs = []
        for i in range(2):
            scs.append({"den": self.alloc(16, F32), "rden": self.alloc(16, F32), "o": self.alloc(768, F32), "junk": self.alloc(768),
                        "st": self.alloc(8, F32), "ob": self.alloc(768),
                        "r": {k: Res(k + str(i)) for k in ("den", "o", "junk", "st", "ob")}})
        rot = 0
        col0 = 0 if which == "a" else 1280
        for qb in range(NT):
            ob_ = qb % 2
            pso = [self.ps[2 + 3 * ob_ + g] for g in range(3)]
            rpo = [self.rps[2 + 3 * ob_ + g] for g in range(3)]
            kts = [kt for kt in ((qb - 1, qb, qb + 1) if which == "a" else range(NT)) if 0 <= kt < NT]
            for hk in range(4):
                for idx, kt in enumerate(kts):
                    s_ = rot % NR
                    rot += 1
                    ps = self.ps[s_][:, 0:384]
                    self.mm(ps, KT[0:64, hk, kt * 128:(kt + 1) * 128], QT[0:64, 3 * hk:3 * hk + 3, qb * 128:(qb + 1) * 128],
                            True, True, [rk, rq], [self.rps[s_]])
                    self.act(Pe[s_], ps, AF.Exp, [self.rps[s_]], [rpe[s_]], scale=0.125)
                    if which == "a":
                        ri = kt - qb + 1
                        self.tt("dve", Pm[s_].rearrange("p (g q) -> p g q", g=3), Pe[s_].rearrange("p (g q) -> p g q", g=3),
                                self.eba[:, ri, 3 * hk:3 * hk + 3, :], ALU.mult, [rpe[s_], self.rconst], [rpm[s_]])
                        pp, rpp = Pm[s_], rpm[s_]
                    else:
                        pp, rpp = Pe[s_], rpe[s_]
                    for g in range(3):
                        self.mm(pso[g][:, hk * 65:hk * 65 + 65], pp[:, g * 128:(g + 1) * 128], V[:, kt, hk, :],
                                idx == 0, idx == len(kts) - 1, [rpp, rv], [rpo[g]])
            sc = scs[ob_]
            r = sc["r"]
            den3 = sc["den"][:, 0:12].rearrange("p (k g) -> p k g", g=3)
            rden3 = sc["rden"][:, 0:12].rearrange("p (k g) -> p k g", g=3)
            es3 = self.esink[:, l * 12:(l + 1) * 12].rearrange("p (k g) -> p k g", g=3)
            o4 = sc["o"].rearrange("p (k g d) -> p k g d", k=4, g=3)
            for g in range(3):
                o3 = pso[g][:, 0:260].rearrange("p (k e) -> p k e", k=4)
                if which == "a":
                    self.tt("dve", den3[:, :, g], o3[:, :, 64], es3[:, :, g], ALU.add, [rpo[g], self.rconst], [r["den"]])
                else:
                    self.cp("dve", den3[:, :, g], o3[:, :, 64], [rpo[g]], [r["den"]])
            self.recip(sc["rden"][:, 0:12], sc["den"][:, 0:12], [r["den"]], [r["den"]])
            for g in range(3):
                o3 = pso[g][:, 0:260].rearrange("p (k e) -> p k e", k=4)
                rb = rden3[:, :, g].unsqueeze(2).to_broadcast([128, 4, 64])
                self.tt("dve", o4[:, :, g, :], o3[:, :, 0:64], rb, ALU.mult, [rpo[g], r["den"]], [r["o"]])
            self.outnorm_store(sc["o"], 768, 128, Sx["mixed"][qb * 128:(qb + 1) * 128, col0:col0 + 768], sc)
        self.top = m
        self.P.barrier()

    def mixer_B(self, l):
        I, Sx = self.I, self.Sx
        m = self.top
        QT = self.alloc(8 * S).rearrange("p (h t) -> p h t", h=8)
        KT = self.alloc(8 * S).rearrange("p (h t) -> p h t", h=8)
        rq, rk = Res("QTb"), Res("KTb")
        self.dma(QT[0:64], Sx["qt_b"].rearrange("h d t -> d h t"), [], [rq])
        self.dma(KT[0:64], Sx["kt_b"].rearrange("h d t -> d h t"), [], [rk])
        Vr = [self.alloc(4 * 8 * 65).rearrange("p (i h e) -> p i h e", i=4, h=8) for _ in range(2)]
        rvr = [Res("vr%d" % i) for i in range(2)]
        NR = 3
        Pe = [self.alloc(512) for _ in range(NR)]
        Pm = [self.alloc(512) for _ in range(NR)]
        rpe = [Res("peb%d" % i) for i in range(NR)]
        rpm = [Res("pmb%d" % i) for i in range(NR)]
        scs = []
        for i in range(2):
            scs.append({"rden": self.alloc(16, F32), "o": self.alloc(512, F32), "junk": self.alloc(512), "st": self.alloc(8, F32),
                        "ob": self.alloc(512), "r": {k: Res(k + "b" + str(i)) for k in ("den", "o", "junk", "st", "ob")}})
        vbt = Sx["v_b"].tensor
        rot = 0
        for r_ in range(32):
            r0 = min(max(r_ - 4, 0), 24)
            s_ = r_ - r0
            vb = r_ % 2
            src = bass.AP(vbt, 64 * r0 * 520, [[520, 128], [128 * 520, 4], [1, 520]])
            self.dma(Vr[vb].rearrange("p i h e -> p i (h e)"), src, [], [rvr[vb]])
            ob_ = r_ % 2
            pso = [self.ps[3 + 2 * ob_], self.ps[4 + 2 * ob_]]
            rpo = [self.rps[3 + 2 * ob_], self.rps[4 + 2 * ob_]]
            for hp in range(4):
                sl = rot % NR
                rot += 1
                ps = self.ps[sl][:, 0:512].rearrange("p (a i q) -> p a i q", a=2, i=4)
                for hh in range(2):
                    h = 2 * hp + hh
                    for i in range(4):
                        k0 = 64 * r0 + 128 * i
                        self.mm(ps[:, hh, i, :], KT[0:64, h, k0:k0 + 128], QT[0:64, h, 64 * r_:64 * r_ + 64], True, True,
                                [rk, rq], [self.rps[sl]])
                self.act(Pe[sl], self.ps[sl][:, 0:512], AF.Exp, [self.rps[sl]], [rpe[sl]], scale=0.125)
                eb = self.ebb[:, 2 * hp:2 * hp + 2, 7 - s_:7 - s_ + 7:2, :]
                self.tt("dve", Pm[sl].rearrange("p (a i q) -> p a i q", a=2, i=4), Pe[sl].rearrange("p (a i q) -> p a i q", a=2, i=4), eb,
                        ALU.mult, [rpe[sl], self.rconst], [rpm[sl]])
                pm4 = Pm[sl].rearrange("p (a i q) -> p a i q", a=2, i=4)
                for hh in range(2):
                    h = 2 * hp + hh
                    for i in range(4):
                        self.mm(pso[h // 4][0:64, (h % 4) * 65:(h % 4) * 65 + 65], pm4[:, hh, i, :], Vr[vb][:, i, h, :], i == 0, i == 3,
                                [rpm[sl], rvr[vb]], [rpo[h // 4]])
            sc = scs[ob_]
            r = sc["r"]
            for b in range(2):
                o3 = pso[b][0:64, 0:260].rearrange("p (h e) -> p h e", h=4)
                self.recip(sc["rden"][0:64, 4 * b:4 * b + 4], o3[:, :, 64], [rpo[b]], [r["den"]])
            for b in range(2):
                o3 = pso[b][0:64, 0:260].rearrange("p (h e) -> p h e", h=4)
                rb = sc["rden"][0:64, 4 * b:4 * b + 4].unsqueeze(2).to_broadcast([64, 4, 64])
                self.tt("dve", sc["o"][0:64, 256 * b:256 * b + 256].rearrange("p (h d) -> p h d", h=4), o3[:, :, 0:64], rb, ALU.mult,
                        [rpo[b], r["den"]], [r["o"]])
            self.outnorm_store(sc["o"][0:64], 512, 64, Sx["mixed"][64 * r_:64 * r_ + 64, 768:1280], sc)
        self.top = m
        self.P.barrier()

    def residual_epilogue(self, xsrc, xdst, nblk, rrow):
        m_ = {}
        xt = [self.alloc(nblk, F32) for _ in range(3)]
        rxt = [Res("rxt%d" % i) for i in range(3)]
        cnt = [0]

        def epi(nb, t, ps, rps):
            b = cnt[0] % 3
            cnt[0] += 1
            self.dma(xt[b], xsrc[t * 128:(t + 1) * 128, nb * nblk:(nb + 1) * nblk], [rrow[t]] if rrow else [], [rxt[b]])
            self.tt("dve", xt[b], ps, xt[b], ALU.add, [rps, rxt[b]], [rxt[b]])
            self.dma(xdst[t * 128:(t + 1) * 128, nb * nblk:(nb + 1) * nblk], xt[b], [rxt[b]], [rrow[t]] if rrow else [], eng="pool")

        return epi

    def phase_C(self, l, xsrc, xdst, inplace):
        I, Sx = self.I, self.Sx
        m = self.top
        mixT = self.alloc(KC * S).rearrange("p (k t) -> p k t", k=KC)
        rmx = [Res("mixT%d" % t) for t in range(NT)]
        mt = [self.alloc(D) for _ in range(2)]
        rmt = [Res("mt%d" % i) for i in range(2)]
        for t in range(NT):
            b = t % 2
            self.dma(mt[b], Sx["mixed"][t * 128:(t + 1) * 128, :], [], [rmt[b]])
            for g in range(4):
                pb = g % 2
                pv = self.ps[pb][:, 0:256].bitcast(BF16)
                for j in range(4):
                    kc = g * 4 + j
                    self.tr(pv[:, j * 128:(j + 1) * 128], mt[b][:, kc * 128:(kc + 1) * 128], [rmt[b]], [self.rps[pb]])
                self.cp("dve" if g % 2 == 0 else "act", mixT[:, g * 4:(g + 1) * 4, t * 128:(t + 1) * 128],
                        pv.rearrange("p (j q) -> p j q", j=4), [self.rps[pb]], [rmx[t]])
        rrow = [Res("row%d" % t) for t in range(NT)] if inplace else None
        epi = self.residual_epilogue(xsrc, xdst, 256, rrow)
        self.linear(mixT, rmx, NT, I["w_out"][l], KC, D, 256, self.gcol[:, l, 1, :], epi)
        self.top = m
        self.P.barrier()

    def phase_D(self, l, s, xio):
        I, Sx = self.I, self.Sx
        m = self.top
        memT = self.alloc(KC * NMEM).rearrange("p (k t) -> p k t", k=KC)
        rmemT = [Res("memT%d" % t) for t in range(2)]
        msrc = I["mem"][s]
        self.norm_transpose(lambda t: msrc[t * 128:(t + 1) * 128, :], 2, memT, rmemT)
        KmT = self.alloc(4 * NMEM).rearrange("p (h t) -> p h t", h=4)
        Vm = self.alloc(2 * 4 * 129).rearrange("p (t h e) -> p t h e", t=2, h=4)
        rkm, rvm = Res("KmT"), Res("Vm")
        self.memset("pool", Vm[:, :, :, 128:129], 1.0, [rvm])
        sc = {"sq": self.alloc(512, F32), "ss": self.alloc(32, F32), "qn": self.alloc(512, F32), "qg": None,
              "r": {k: Res(k + "d") for k in ("sq", "ss", "qn")}}
        qb = [self.alloc(512) for _ in range(2)]
        rqb = [Res("qbd%d" % i) for i in range(2)]
        cnt = [0]

        def epi_kv(nb, t, ps, rps):
            if nb == 1:
                self.cp("act", Vm[:, t, :, 0:128], ps.rearrange("p (h d) -> p h d", h=4), [rps], [rvm])
                return
            e = cnt[0] % 2
            cnt[0] += 1
            gb = self.headnorm(ps, rps, 4, 128, self.g[("qk_norm_mem", l, 1)], sc)
            self.tt("dve", qb[e].rearrange("p (h d) -> p h d", h=4), sc["qn"].rearrange("p (h d) -> p h d", h=4), gb, ALU.mult,
                    [sc["r"]["qn"], self.rconst], [rqb[e]])
            pb = 4 + e
            pv = self.ps[pb][:, 0:256].bitcast(BF16)
            for j in range(4):
                self.tr(pv[:, j * 128:(j + 1) * 128], qb[e][:, j * 128:(j + 1) * 128], [rqb[e]], [self.rps[pb]])
            self.cp("act", KmT[:, :, t * 128:(t + 1) * 128], pv.rearrange("p (j q) -> p j q", j=4), [self.rps[pb]], [rkm])

        self.linear(memT, rmemT, 2, I["w_mem_kv"][l], KC, 1024, 512, self.gcol[:, l, 3, :], epi_kv)
        QmT = self.alloc(4 * S).rearrange("p (h t) -> p h t", h=4)
        rqm = [Res("QmT%d" % t) for t in range(NT)]
        mD2 = self.top
        xnT = self.alloc(KC * S).rearrange("p (k t) -> p k t", k=KC)
        rxn = [Res("xnTd%d" % t) for t in range(NT)]
        rrow = [Res("rowd%d" % t) for t in range(NT)]
        self.norm_transpose(lambda t: xio[t * 128:(t + 1) * 128, :], NT, xnT, rxn, rsrc=rrow)

        def epi_q(nb, t, ps, rps):
            e = cnt[0] % 2
            cnt[0] += 1
            gb = self.headnorm(ps, rps, 2, 128, self.g[("qk_norm_mem", l, 0)], sc)
            self.tt("dve", qb[e][:, 0:256].rearrange("p (h d) -> p h d", h=2), sc["qn"][:, 0:256].rearrange("p (h d) -> p h d", h=2), gb, ALU.mult,
                    [sc["r"]["qn"], self.rconst], [rqb[e]])
            pb = 4 + e
            pv = self.ps[pb][:, 0:256].bitcast(BF16)
            for j in range(2):
                self.tr(pv[:, j * 128:(j + 1) * 128], qb[e][:, j * 128:(j + 1) * 128], [rqb[e]], [self.rps[pb]])
            self.cp("act", QmT[:, 2 * nb:2 * nb + 2, t * 128:(t + 1) * 128], pv[:, 0:256].rearrange("p (j q) -> p j q", j=2), [self.rps[pb]], [rqm[t]])

        self.linear(xnT, rxn, NT, I["w_mem_q"][l], KC, 512, 256, self.gcol[:, l, 2, :], epi_q)
        self.top = mD2
        wo_st = self.alloc(4 * 512, F32).rearrange("p (k n) -> p k n", k=4)
        wo = self.alloc(4 * D).rearrange("p (k n) -> p k n", k=4)
        rwst, rwo = Res("wost"), Res("wo")
        wov = I["w_mem_o"][l].rearrange("(k p) n -> p k n", p=128)
        for nb in range(4):
            self.dma(wo_st, wov[:, :, nb * 512:(nb + 1) * 512], [], [rwst])
            self.cp("pool" if nb % 2 else "act", wo[:, :, nb * 512:(nb + 1) * 512], wo_st, [rwst], [rwo])
        Pe = [self.alloc(1024) for _ in range(2)]
        rpe = [Res("ped%d" % i) for i in range(2)]
        rden = [self.alloc(8, F32) for _ in range(2)]
        om = [self.alloc(512) for _ in range(2)]
        omT = [self.alloc(512).rearrange("p (h t) -> p h t", h=4) for _ in range(2)]
        xt = [self.alloc(D, F32) for _ in range(2)]
        rrd = [Res("rdend%d" % i) for i in range(2)]
        rom = [Res("om%d" % i) for i in range(2)]
        romT = [Res("omT%d" % i) for i in range(2)]
        rxt = [Res("xtd%d" % i) for i in range(2)]
        scale = 128.0 ** -0.5
        for t in range(NT):
            b = t % 2
            self.dma(xt[b], xio[t * 128:(t + 1) * 128, :], [rrow[t]], [rxt[b]])
            for mt_ in range(2):
                for h in range(4):
                    self.mm(self.ps[mt_][:, h * 128:(h + 1) * 128], KmT[:, h, mt_ * 128:(mt_ + 1) * 128], QmT[:, h, t * 128:(t + 1) * 128],
                            True, True, [rkm, rqm[t]], [self.rps[mt_]])
                self.act(Pe[b][:, mt_ * 512:(mt_ + 1) * 512], self.ps[mt_][:, 0:512], AF.Exp, [self.rps[mt_]], [rpe[b]], scale=scale)
            for h in range(4):
                pb, off = (2, h * 129) if h < 3 else (3, 0)
                for mt_ in range(2):
                    self.mm(self.ps[pb][:, off:off + 129], Pe[b][:, mt_ * 512 + h * 128:mt_ * 512 + (h + 1) * 128], Vm[:, mt_, h, :],
                            mt_ == 0, mt_ == 1, [rpe[b], rvm], [self.rps[pb]])
            o3 = self.ps[2][:, 0:387].rearrange("p (h e) -> p h e", h=3)
            self.recip(rden[b][:, 0:3], o3[:, :, 128], [self.rps[2]], [rrd[b]])
            self.recip(rden[b][:, 3:4], self.ps[3][:, 128:129], [self.rps[3]], [rrd[b]])
            self.tt("dve", om[b][:, 0:384].rearrange("p (h d) -> p h d", h=3), o3[:, :, 0:128],
                    rden[b][:, 0:3].unsqueeze(2).to_broadcast([128, 3, 128]), ALU.mult, [self.rps[2], rrd[b]], [rom[b]])
            self.ts("dve", om[b][:, 384:512], self.ps[3][:, 0:128], rden[b][:, 3:4], None, ALU.mult, None, [self.rps[3], rrd[b]], [rom[b]])
            pv = self.ps[4][:, 0:256].bitcast(BF16)
            for j in range(4):
                self.tr(pv[:, j * 128:(j + 1) * 128], om[b][:, j * 128:(j + 1) * 128], [rom[b]], [self.rps[4]])
            self.cp("act", omT[b], pv.rearrange("p (j q) -> p j q", j=4), [self.rps[4]], [romT[b]])
            for nb in range(4):
                pb = 5 + nb % 2
                for k in range(4):
                    self.mm(self.ps[pb][:, 0:512], omT[b][:, k, :], wo[:, k, nb * 512:(nb + 1) * 512], k == 0, k == 3,
                            [romT[b], rwo], [self.rps[pb]])
                self.tt("dve", xt[b][:, nb * 512:(nb + 1) * 512], self.ps[pb][:, 0:512], xt[b][:, nb * 512:(nb + 1) * 512], ALU.add,
                        [self.rps[pb], rxt[b]], [rxt[b]])
            self.dma(xio[t * 128:(t + 1) * 128, :], xt[b], [rxt[b]], [rrow[t]], eng="pool")
        self.top = m
        self.P.barrier()

    def phase_P(self, l):
        I = self.I
        m = self.top
        ust = [self.alloc(D, F32) for _ in range(2)]
        ubf = [self.alloc(D) for _ in range(2)]
        uTb = [self.alloc(KC * 128).rearrange("p (k e) -> p k e", k=KC) for _ in range(2)]
        vst = [self.alloc(D, F32) for _ in range(2)]
        vb = [self.alloc(D) for _ in range(2)]
        r = {k: [Res(k + str(i)) for i in range(2)] for k in ("ust", "ubf", "uTb", "vst", "vb")}
        uv = I["peer_u"][l]
        vv = I["peer_v"][l]
        for c in range(128):
            b = c % 2
            self.dma(ust[b], uv[c * 128:(c + 1) * 128, :], [], [r["ust"][b]])
            self.cp("pool", ubf[b], ust[b], [r["ust"][b]], [r["ubf"][b]])
            for g in range(4):
                pb = g % 2
                pv = self.ps[pb][:, 0:256].bitcast(BF16)
                for j in range(4):
                    k = g * 4 + j
                    self.tr(pv[:, j * 128:(j + 1) * 128], ubf[b][:, k * 128:(k + 1) * 128], [r["ubf"][b]], [self.rps[pb]])
                self.cp("act" if g % 2 else "dve", uTb[b][:, g * 4:(g + 1) * 4, :], pv.rearrange("p (j q) -> p j q", j=4),
                        [self.rps[pb]], [r["uTb"][b]])
            self.dma(self.ut_s[c], uTb[b].rearrange("p k e -> p (k e)"), [r["uTb"][b]], [], eng="pool")
            self.dma(vst[b], vv[c * 128:(c + 1) * 128, :], [], [r["vst"][b]])
            self.cp("act" if c % 2 else "dve", vb[b], vst[b], [r["vst"][b]], [r["vb"][b]])
            self.dma(self.v_s[c * 128:(c + 1) * 128, :], vb[b], [r["vb"][b]], [], eng="pool")
        self.P.barrier()
        self.top = m

    def phase_E(self, l, xio):
        I, Sx = self.I, self.Sx
        m = self.top
        TP = 512
        NTP = 4
        keysT = self.alloc(16 * 128).rearrange("p (a n) -> p a n", a=16)
        rkt = Res("keysT")
        m1 = self.top
        kst = self.alloc(16 * 128, F32).rearrange("p (a n) -> p a n", a=16)
        kbf = self.alloc(16 * 128).rearrange("p (a n) -> p a n", a=16)
        rks, rkb = Res("kst"), Res("kbf")
        self.dma(kst, I["peer_keys"][l].rearrange("h c n d -> n (h c) d"), [], [rks])
        self.cp("dve", kbf, kst, [rks], [rkb])
        for g in range(4):
            pv = self.ps[g % 2][:, 0:256].bitcast(BF16)
            for j in range(4):
                self.tr(pv[:, j * 128:(j + 1) * 128], kbf[:, g * 4 + j, :], [rkb], [self.rps[g % 2]])
            self.cp("act", keysT[:, g * 4:(g + 1) * 4, :], pv.rearrange("p (j q) -> p j q", j=4), [self.rps[g % 2]], [rkt])
        self.P.barrier()
        gcol = self.gcol[:, l, 4, :]
        for p_ in range(S // TP):
            self.top = m1
            rows = lambda t: xio[p_ * TP + t * 128:p_ * TP + (t + 1) * 128, :]
            hnT = self.alloc(KC * TP).rearrange("p (k t) -> p k t", k=KC)
            rhn = [Res("hnT%d" % t) for t in range(NTP)]
            s1b = self.alloc(NTP * 8 * 128, F32).rearrange("p (t h n) -> p t h n", t=NTP, h=8)
            phi = self.alloc(NTP * 128 * 8, F32).rearrange("p (t c h) -> p t c h", t=NTP, c=128)
            Dm = self.alloc(NTP * 8 * 128).rearrange("p (t h q) -> p t h q", t=NTP, h=8)
            ytok = self.alloc(NTP * D, F32).rearrange("p (t d) -> p t d", t=NTP)
            rs1 = [Res("s1b%d" % t) for t in range(NTP)]
            rphi = [Res("phi%d" % t) for t in range(NTP)]
            rdm = [Res("Dm%d" % t) for t in range(NTP)]
            ryt = [[Res("yt%d_%d" % (t, d)) for d in range(4)] for t in range(NTP)]
            m2 = self.top
            self.norm_transpose(rows, NTP, hnT, rhn, gcol=gcol)
            qT = self.alloc(16 * TP).rearrange("p (a t) -> p a t", a=16)
            rqT = [Res("qT%d" % a) for a in range(16)]
            wst = [self.alloc(KC * 128, F32).rearrange("p (k n) -> p k n", k=KC) for _ in range(2)]
            wbf = [self.alloc(KC * 128).rearrange("p (k n) -> p k n", k=KC) for _ in range(2)]
            rst = [Res("pwst%d" % i) for i in range(2)]
            rwb = [[Res("pwbf%d_%d" % (i, k)) for k in range(KC)] for i in range(2)]
            wv = I["peer_w_q"][l].rearrange("(k p) n -> p k n", p=128)
            engs = ["act", "dve", "pool"]
            for nb in range(16):
                b = nb % 2
                self.dma(wst[b], wv[:, :, nb * 128:(nb + 1) * 128], [], [rst[b]])
                self.P.op("act" if nb % 2 else "pool", (lambda e, o_=wbf[b], i_=wst[b]: e.activation(out=o_, in_=i_, func=AF.Copy)) if nb % 2 else
                          (lambda e, o_=wbf[b], i_=wst[b]: e.tensor_copy(out=o_, in_=i_)), [rst[b]], rwb[b])
                for ch in range(1):
                    a = nb
                    pb = 2 + a % 2
                    for k in range(KC):
                        self.mm(self.ps[pb][:, 0:TP], wbf[b][:, k, ch * 128:(ch + 1) * 128], hnT[:, k, :], k == 0, k == KC - 1,
                                [rwb[b][k]] + rhn, [self.rps[pb]])
                    self.cp("act" if a % 2 else "dve", qT[:, a, :], self.ps[pb][:, 0:TP], [self.rps[pb]], [rqT[a]])
            s0t = self.alloc(8 * 128, F32).rearrange("p (h n) -> p h n", h=8)
            top = self.alloc(16 * 16, F32).rearrange("p (a k) -> p a k", a=16)
            wk = self.alloc(256, F32)
            cand = self.alloc(8 * 256, F32).rearrange("p (h a b) -> p h a b", h=8, a=16)
            best = self.alloc(8 * 16, F32).rearrange("p (h k) -> p h k", h=8)
            eb_ = self.alloc(8 * 16, F32).rearrange("p (h k) -> p h k", h=8)
            sm = self.alloc(64, F32)
            rs0, rtop, rwk, rcand, rbest, rsm = Res("s0t"), Res("top"), Res("wk"), Res("cand"), Res("best"), Res("sm")
            for t in range(NTP):
                for g in range(4):
                    pb = 4 + g % 2
                    for j in range(4):
                        a = g * 4 + j
                        self.mm(self.ps[pb][:, j * 128:(j + 1) * 128], qT[:, a, t * 128:(t + 1) * 128], keysT[:, a, :], True, True,
                                [rqT[a], rkt], [self.rps[pb]])
                    p4 = self.ps[pb][:, 0:512].rearrange("p (h c n) -> p h c n", h=2, c=2)
                    self.cp("act", s0t[:, 2 * g:2 * g + 2, :], p4[:, :, 0, :], [self.rps[pb]], [rs0])
                    self.cp("act", s1b[:, t, 2 * g:2 * g + 2, :], p4[:, :, 1, :], [self.rps[pb]], [rs1[t]])
                for a in range(16):
                    src = s0t[:, a // 2, :] if a % 2 == 0 else s1b[:, t, a // 2, :]
                    rsrc_ = rs0 if a % 2 == 0 else rs1[t]
                    self.P.op("dve", lambda e, src=src, a=a: e.max(out=top[:, a, 0:8], in_=src), [rsrc_], [rtop])
                    self.P.op("dve", lambda e, src=src, a=a: e.match_replace(out=wk[:, 0:128], in_to_replace=top[:, a, 0:8], in_values=src, imm_value=-1e30),
                              [rsrc_, rtop], [rwk])
                    self.P.op("dve", lambda e, a=a: e.max(out=top[:, a, 8:16], in_=wk[:, 0:128]), [rwk], [rtop])
                top4 = top.rearrange("p (h c) k -> p h c k", c=2)
                in0 = top4[:, :, 0, :].unsqueeze(3).to_broadcast([128, 8, 16, 16])
                in1 = top4[:, :, 1, :].unsqueeze(2).to_broadcast([128, 8, 16, 16])
                self.tt("dve", cand, in0, in1, ALU.add, [rtop], [rcand])
                for h in range(8):
                    cf = cand[:, h].rearrange("p a b -> p (a b)")
                    self.P.op("dve", lambda e, cf=cf, h=h: e.max(out=best[:, h, 0:8], in_=cf), [rcand], [rbest])
                    self.P.op("dve", lambda e, cf=cf, h=h: e.match_replace(out=wk, in_to_replace=best[:, h, 0:8], in_values=cf, imm_value=-1e30),
                              [rcand, rbest], [rwk])
                    self.P.op("dve", lambda e, h=h: e.max(out=best[:, h, 8:16], in_=wk), [rwk], [rbest])
                self.cp("dve", sm[:, 0:8], best[:, :, 0], [rbest], [rsm])
                self.ts("dve", sm[:, 8:16], best[:, :, 15], -1e-5, None, ALU.add, None, [rbest], [rsm])
                self.tt("dve", eb_, best, sm[:, 0:8].unsqueeze(2).to_broadcast([128, 8, 16]), ALU.subtract, [rbest, rsm], [rbest])
                self.act(eb_, eb_, AF.Exp, [rbest], [rbest])
                self.P.op("dve", lambda e: e.tensor_reduce(out=sm[:, 16:24], in_=eb_, axis=AX.X, op=ALU.add), [rbest], [rsm])
                self.recip(sm[:, 24:32], sm[:, 16:24], [rsm], [rsm])
                self.tt("dve", sm[:, 32:40], sm[:, 8:16], sm[:, 0:8], ALU.subtract, [rsm], [rsm])
                self.act(sm[:, 32:40], sm[:, 32:40], AF.Exp, [rsm], [rsm])
                self.tt("dve", sm[:, 40:48], sm[:, 32:40], sm[:, 24:32], ALU.mult, [rsm], [rsm])
                self.tt("dve", phi[:, t].rearrange("p c h -> p h c"), s0t, sm[:, 8:16].unsqueeze(2).to_broadcast([128, 8, 128]), ALU.subtract,
                        [rs0, rsm], [rphi[t]])
                for h in range(8):
                    eng = "pool" if h % 2 else "dve"
                    self.ts(eng, Dm[:, t, h, :], self.ident, sm[:, 40 + h:41 + h], None, ALU.mult, None, [rsm, self.rconst], [rdm[t]])
                if self.dbg and p_ == 0 and t == 0:
                    self.dma(Sx["dbg_sm"], sm, [rsm], [], eng="pool")
                    self.dma(Sx["dbg_s0"], s0t.rearrange("p h n -> p (h n)"), [rs0], [], eng="pool")
                    self.dma(Sx["dbg_s1"], s1b[:, 0].rearrange("p h n -> p (h n)"), [rs1[0]], [], eng="pool")
            self.P.barrier()
            self.top = m2
            uT = [self.alloc(KC * 128).rearrange("p (k e) -> p k e", k=KC) for _ in range(2)]
            vbf = [self.alloc(2 * D).rearrange("p (c d) -> p c d", c=2) for _ in range(2)]
            wT = [self.alloc(2 * TP).rearrange("p (c t) -> p c t", c=2) for _ in range(2)]
            zz = [self.alloc(2048, F32) for _ in range(2)]
            E_ = [self.alloc(2048) for _ in range(2)]
            Gm = [self.alloc(2048) for _ in range(2)]
            ge = [self.alloc(TP) for _ in range(2)]
            ruT = [Res("uT%d" % i) for i in range(2)]
            rvbf = [[Res("vbf%d_%d" % (i, c)) for c in range(2)] for i in range(2)]
            rwT = [[Res("wT%d_%d" % (i, c)) for c in range(2)] for i in range(2)]
            rzz = [Res("zz%d" % i) for i in range(2)]
            rE = [Res("E%d" % i) for i in range(2)]
            rGm = [Res("Gm%d" % i) for i in range(2)]
            rge = [Res("ge%d" % i) for i in range(2)]
            zi = 0

            def y_stage(cpm):
                ygm = cpm % 2
                for t in range(NTP):
                    for d4 in range(4):
                        pb2 = 4 + (t * 4 + d4) % 4
                        for cc in range(2):
                            self.mm(self.ps[pb2][:, 0:512], wT[ygm][:, cc, t * 128:(t + 1) * 128], vbf[ygm][:, cc, d4 * 512:(d4 + 1) * 512],
                                    cc == 0, cc == 1, [rwT[ygm][cc], rvbf[ygm][cc]], [self.rps[pb2]])
                        dst = ytok[:, t, d4 * 512:(d4 + 1) * 512]
                        if cpm == 0:
                            self.cp("act", dst, self.ps[pb2][:, 0:512], [self.rps[pb2]], [ryt[t][d4]])
                        else:
                            self.tt("dve", dst, self.ps[pb2][:, 0:512], dst, ALU.add, [self.rps[pb2], ryt[t][d4]], [ryt[t][d4]])

            def p1(cp_, t, z):
                in0 = s1b[:, t].unsqueeze(1).to_broadcast([128, 2, 8, 128])
                in1 = phi[:, t, 2 * cp_:2 * cp_ + 2, :].unsqueeze(3).to_broadcast([128, 2, 8, 128])
                self.tt("dve", zz[z].rearrange("p (c h n) -> p c h n", c=2, h=8), in0, in1, ALU.add, [rs1[t], rphi[t]], [rzz[z]])

            for cp_ in range(64):
                yg = cp_ % 2
                if cp_ > 0:
                    y_stage(cp_ - 1)
                for cc in range(2):
                    c = 2 * cp_ + cc
                    self.dma(uT[cc].rearrange("p k e -> p (k e)"), self.ut_s[c], [], [ruT[cc]])
                    self.dma(vbf[yg][:, cc, :], self.v_s[c * 128:(c + 1) * 128, :], [], [rvbf[yg][cc]])
                    for k in range(KC):
                        self.mm(self.ps[cc][:, 0:TP], uT[cc][:, k, :], hnT[:, k, :], k == 0, k == KC - 1, [ruT[cc]] + rhn, [self.rps[cc]])
                    self.act(ge[cc], self.ps[cc][:, 0:TP], AF.Gelu, [self.rps[cc]], [rge[cc]])
                p1(cp_, 0, zi % 2)
                for t in range(NTP):
                    z = zi % 2
                    zi += 1
                    if t + 1 < NTP:
                        p1(cp_, t + 1, zi % 2)
                    self.act(E_[z], zz[z], AF.Exp, [rzz[z]], [rE[z]])
                    self.stt(Gm[z], zz[z], 0.0, E_[z], ALU.is_ge, ALU.mult, [rzz[z], rE[z]], [rGm[z]])
                    for cc in range(2):
                        gb_ = 2 + cc
                        for h in range(8):
                            o_ = (cc * 8 + h) * 128
                            self.mm(self.ps[gb_][:, t * 128:(t + 1) * 128], Gm[z][:, o_:o_ + 128], Dm[:, t, h, :], h == 0, h == 7,
                                    [rGm[z], rdm[t]], [self.rps[gb_]])
                for cc in range(2):
                    self.tt("dve", wT[yg][:, cc, :], ge[cc], self.ps[2 + cc][:, 0:TP], ALU.mult, [rge[cc], self.rps[2 + cc]], [rwT[yg][cc]])
                if self.dbg and p_ == 0 and cp_ == 0:
                    self.dma(Sx["dbg_ge"], ge[0], [rge[0]], [], eng="pool")
                    self.dma(Sx["dbg_wt"], wT[yg][:, 0, :], [rwT[yg][0]], [], eng="pool")
            y_stage(63)
            if self.dbg and p_ == 0:
                self.dma(Sx["dbg_y"], ytok[:, 0, :], ryt[0], [], eng="pool")
            xt = zz[0]
            rxt = rzz[0]
            for t in range(NTP):
                self.dma(xt, rows(t), [], [rxt])
                self.tt("pool", xt, xt, ytok[:, t, :], ALU.add, [rxt] + ryt[t], [rxt])
                self.dma(rows(t), xt, [rxt], [], eng="pool")
            self.P.barrier()
        self.top = m
        self.P.barrier()

    def build(self):
        self.setup_consts()
        ph = self.phases
        for l in self.layers:
            self.build_ebb(l)
            if "E" in ph:
                self.phase_P(l)
            for s in range(self.nseq):
                xsrc = self.I["x"][s] if l == self.layers[0] else self.out[s]
                xo = self.out[s]
                if "A" in ph:
                    self.phase_A(l, xsrc)
                if "B" in ph:
                    self.mixer_AC(l, "a")
                    self.mixer_B(l)
                    self.mixer_AC(l, "c")
                if "C" in ph:
                    self.phase_C(l, xsrc, xo, l != self.layers[0])
                if "D" in ph:
                    self.phase_D(l, s, xo)
                if "E" in ph:
                    self.phase_E(l, xo)
        self.P.barrier()
        self.P.finalize(self.st)
        self.P.emit()
        self.st.close()
        return self.nc


def build_program(nseq=2, layers=(0, 1), phases="ABCDE", dbg=False, lw=L):
    nc = bass.Bass("TRN2", target_bir_lowering=False)
    kb = KB(nc, nseq, list(layers), phases, dbg, lw)
    kb.build()
    return nc, kb


def make_in_maps(inputs, nseq=2, ncores=8, small_peer=False):
    tabs = _static_tables()
    maps = []
    w = {name: np.ascontiguousarray(np.asarray(inputs[name], dtype=np.float32)) for name, _ in WEIGHT_SPECS}
    if small_peer:
        w["peer_u"] = np.ascontiguousarray(w["peer_u"][:, :128])
        w["peer_v"] = np.ascontiguousarray(w["peer_v"][:, :128])
    x = np.asarray(inputs["x"], dtype=np.float32)
    mem = np.asarray(inputs["mem"], dtype=np.float32)
    for c in range(ncores):
        m = dict(w)
        m.update(tabs)
        m["x"] = np.ascontiguousarray(x[2 * c:2 * c + nseq])
        m["mem"] = np.ascontiguousarray(mem[2 * c:2 * c + nseq])
        maps.append(m)
    return maps


def kernel(**inputs):
    nc, _ = build_program(nseq=2, layers=(0, 1), lw=L)
    maps = make_in_maps(inputs)
    res = run_bass_kernel_spmd(nc, maps, core_ids=list(range(8)))
    return np.concatenate([np.asarray(r["out"], dtype=np.float32) for r in res.results], axis=0)
```

```python
import math
from contextlib import ExitStack

import ml_dtypes
import numpy as np

import concourse.bass as bass
import concourse.mybir as mybir
from concourse.bass_utils import run_bass_kernel_spmd

F32 = mybir.dt.float32
BF16 = mybir.dt.bfloat16
ALU = mybir.AluOpType
AF = mybir.ActivationFunctionType
AX = mybir.AxisListType

D = 2048
S = 2048
NT = 16
KC = 16
L = 2
NMEM = 256
EPS = 1e-6
N_DMA_SEMS = 24
SAME_ENG_SYNC = True
ARENA = 105000


class Res:
    __slots__ = ("name", "lw", "rs")

    def __init__(self, name=""):
        self.name = name
        self.lw = None
        self.rs = []


class Op:
    __slots__ = ("eng", "fn", "deps", "signal", "sem", "val", "ndma", "clock", "gi")


class Prog:
    ENGS = ["pe", "act", "dve", "pool", "sp"]

    def __init__(self, nc):
        self.nc = nc
        self.ops = []
        self.last = {e: None for e in self.ENGS}
        self.dmas_since_barrier = []

    def op(self, eng, fn, reads=(), writes=(), ndma=0):
        o = Op()
        o.eng = eng
        o.fn = fn
        o.ndma = ndma
        o.signal = ndma > 0
        o.sem = None
        o.val = None
        o.clock = None
        o.gi = len(self.ops)
        deps = {}
        for r in reads:
            if r.lw is not None:
                deps[r.lw.gi] = r.lw
        for w in writes:
            if w.lw is not None:
                deps[w.lw.gi] = w.lw
            for x in w.rs:
                deps[x.gi] = x
        best = {}
        dl = []
        for d in deps.values():
            if d.ndma:
                dl.append(d)
                continue
            if d.eng == o.eng and (o.eng == "pe" or not SAME_ENG_SYNC):
                continue
            b = best.get(d.eng)
            if b is None or b.gi < d.gi:
                best[d.eng] = d
        for d in best.values():
            d.signal = True
            dl.append(d)
        o.deps = dl
        for r in reads:
            r.rs.append(o)
        for w in writes:
            w.lw = o
            w.rs = []
        self.ops.append(o)
        self.last[eng] = o
        if ndma:
            self.dmas_since_barrier.append(o)
        return o

    def barrier(self):
        toks = []
        for e in self.ENGS:
            if self.last[e] is not None:
                r = Res("bar_" + e)
                self.op(e, lambda eng: eng.drain(), writes=[r])
                toks.append(r)
        dm = list(self.dmas_since_barrier)
        self.dmas_since_barrier = []
        for e in self.ENGS:
            o = self.op(e, lambda eng: eng.drain(), reads=toks)
            for d in dm:
                o.deps.append(d)

    def finalize(self, stack):
        nc = self.nc
        engsem = {e: stack.enter_context(nc.semaphore("s_" + e)) for e in self.ENGS}
        dmasems = {
            e: [stack.enter_context(nc.semaphore("d_%s_%d" % (e, i))) for i in range(N_DMA_SEMS)]
            for e in ("sp", "act", "pool")
        }
        prev = {e: [None] * N_DMA_SEMS for e in dmasems}
        sval = {e: [0] * N_DMA_SEMS for e in dmasems}
        dcnt = {e: 0 for e in dmasems}
        cnt = {e: 0 for e in self.ENGS}
        clock = {e: {} for e in self.ENGS}
        lists = {e: [] for e in self.ENGS}
        for o in self.ops:
            ck = clock[o.eng]
            deps = list(o.deps)
            if o.ndma:
                slot = dcnt[o.eng] % N_DMA_SEMS
                dcnt[o.eng] += 1
                p = prev[o.eng][slot]
                if p is not None:
                    deps.append(p)
                o.sem = dmasems[o.eng][slot]
                sval[o.eng][slot] += 16 * o.ndma
                o.val = sval[o.eng][slot]
                prev[o.eng][slot] = o
            waits = {}
            for d in deps:
                key = id(d.sem)
                if ck.get(key, 0) >= d.val:
                    continue
                if key not in waits or waits[key][1] < d.val:
                    waits[key] = (d.sem, d.val)
            for d in deps:
                if d.clock:
                    for k, v in d.clock.items():
                        if ck.get(k, 0) < v:
                            ck[k] = v
            for key, (s, v) in waits.items():
                if ck.get(key, 0) < v:
                    ck[key] = v
            wl = list(waits.values())
            if o.ndma == 0 and o.signal:
                cnt[o.eng] += 1
                o.sem = engsem[o.eng]
                o.val = cnt[o.eng]
            if o.signal:
                c = dict(ck)
                if o.ndma == 0:
                    c[id(o.sem)] = o.val
                o.clock = c
            lists[o.eng].append((wl, o))
        self.lists = lists

    def emit(self):
        nc = self.nc
        lists = self.lists

        def run(eng, items):
            for wl, o in items:
                for s, v in wl:
                    eng.wait_ge(s, v)
                if o.ndma:
                    o.fn(eng, o.sem)
                else:
                    ins = o.fn(eng)
                    if o.signal:
                        ins.then_inc(o.sem, 1)

        with nc.Block() as block:
            @block.tensor
            def _(e):
                run(e, lists["pe"])

            @block.scalar
            def _(e):
                run(e, lists["act"])

            @block.vector
            def _(e):
                run(e, lists["dve"])

            @block.gpsimd
            def _(e):
                run(e, lists["pool"])

            @block.sync
            def _(e):
                run(e, lists["sp"])


def _t5_buckets(rel):
    nb = 16
    max_exact = nb // 2
    n = np.abs(rel)
    large = max_exact + (np.log(np.maximum(n, 1) / max_exact) / np.log(128 / max_exact) * (nb - max_exact)).astype(np.int64)
    large = np.minimum(large, nb - 1)
    return np.where(rel > 0, nb, 0) + np.where(n < max_exact, n, large)


def _static_tables():
    t = {}
    t["c_ident"] = np.eye(128, dtype=np.float32).astype(ml_dtypes.bfloat16)
    t["c_j"] = np.ascontiguousarray(np.eye(128, dtype=np.float32)[::-1])
    delta = np.arange(512) - 255
    bk = _t5_buckets(delta)
    oh = np.zeros((32, 512), np.float32)
    oh[bk, np.arange(512)] = 1.0
    t["c_oh"] = oh
    k = np.arange(128)[:, None, None]
    ri = np.arange(3)[None, :, None]
    q = np.arange(128)[None, None, :]
    dd = k + 128 * (ri - 1) - q
    t["c_maska"] = (np.abs(dd) <= 128).astype(np.float32).astype(ml_dtypes.bfloat16)
    kc = np.arange(64)[:, None]
    qc = np.arange(64)[None, :]
    cs = np.clip(qc - 8, 0, 48)
    cv = ((kc >= cs) & (kc < cs + 16)).astype(np.float32)
    t["c_colv"] = np.concatenate([cv, cv], axis=0).astype(ml_dtypes.bfloat16)
    tok = np.arange(S)
    row = (tok // 64).astype(np.float32)
    col = (tok % 64).astype(np.float32)
    inv = (10000.0 ** (-np.arange(16) * 2.0 / 32)).astype(np.float32)
    ar = row[:, None] * inv[None, :]
    ac = col[:, None] * inv[None, :]
    cc = np.concatenate([np.cos(ar), np.cos(ar), np.cos(ac), np.cos(ac)], axis=1)
    ss = np.concatenate([-np.sin(ar), np.sin(ar), -np.sin(ac), np.sin(ac)], axis=1)
    t["c_ropec"] = cc.astype(np.float32)
    t["c_ropes"] = ss.astype(np.float32)
    t["c_zero"] = np.zeros((8, 1152), np.float32)
    return t


WEIGHT_SPECS = [
    ("t5_rel_bias", (32, 12)), ("norm_mix", (L, D)), ("w_in", (L, D, 4096)), ("qk_norm_a", (L, 2, 64)),
    ("sink_a", (L, 12)), ("qk_norm_b", (L, 2, 64)), ("rpb_b", (L, 8, 15, 31)), ("qk_norm_c", (L, 2, 64)),
    ("out_norm", (L, D)), ("w_out", (L, D, D)), ("norm_mem", (L, D)), ("norm_mem_kv", (L, D)),
    ("w_mem_q", (L, D, 512)), ("w_mem_kv", (L, D, 1024)), ("qk_norm_mem", (L, 2, 128)), ("w_mem_o", (L, 512, D)),
    ("norm_ffn", (L, D)), ("peer_w_q", (L, D, D)), ("peer_keys", (L, 8, 2, 128, 128)),
    ("peer_u", (L, 16384, D)), ("peer_v", (L, 16384, D)),
]


class KB:
    def __init__(self, nc, nseq, layers, phases, dbg, lw=L):
        self.nc = nc
        self.lw = lw
        self.P = Prog(nc)
        self.nseq = nseq
        self.layers = layers
        self.phases = phases
        self.dbg = dbg
        self.st = ExitStack()
        st = self.st
        self.I = {}
        self.I["x"] = nc.dram_tensor("x", [nseq, S, D], F32, kind="ExternalInput").ap()
        self.I["mem"] = nc.dram_tensor("mem", [nseq, NMEM, D], F32, kind="ExternalInput").ap()
        for name, shp in WEIGHT_SPECS:
            if name != "t5_rel_bias":
                shp = (lw,) + tuple(shp[1:])
            if name in ("peer_u", "peer_v") and "E" not in phases:
                shp = (lw, 128, D)
            self.I[name] = nc.dram_tensor(name, list(shp), F32, kind="ExternalInput").ap()
        for name, arr in _static_tables().items():
            dt = BF16 if arr.dtype == ml_dtypes.bfloat16 else F32
            self.I[name] = nc.dram_tensor(name, list(arr.shape), dt, kind="ExternalInput").ap()
        self.out = nc.dram_tensor("out", [nseq, S, D], F32, kind="ExternalOutput").ap()
        kind = "ExternalOutput" if dbg else "Internal"
        self.Sx = {}

        def scr(name, shp, dt):
            self.Sx[name] = nc.dram_tensor(name, list(shp), dt, kind=kind).ap()

        scr("qt_a", [12, 64, S], BF16)
        scr("kt_a", [4, 64, S], BF16)
        scr("v_a", [S, 4, 65], BF16)
        scr("qt_b", [8, 64, S], BF16)
        scr("kt_b", [8, 64, S], BF16)
        scr("v_b", [S, 8, 65], BF16)
        scr("qt_c", [12, 64, S], BF16)
        scr("kt_c", [4, 64, S], BF16)
        scr("v_c", [S, 4, 65], BF16)
        scr("mixed", [S, D], BF16)
        if dbg:
            scr("dbg_sm", [128, 64], F32)
            scr("dbg_s0", [128, 1024], F32)
            scr("dbg_s1", [128, 1024], F32)
            scr("dbg_ge", [128, 512], BF16)
            scr("dbg_wt", [128, 512], BF16)
            scr("dbg_y", [128, 2048], F32)
        scr("tv", [12, 512], F32)
        self.ut_s = nc.dram_tensor("ut_s", [128, 128, KC * 128], BF16, kind="Internal").ap()
        self.v_s = nc.dram_tensor("v_s", [16384, D], BF16, kind="Internal").ap()
        scr("wb", [8, 1152], F32)
        self.arena = st.enter_context(nc.sbuf_tensor("arena", [128, ARENA], BF16))
        self.ps = [st.enter_context(nc.psum_tensor("ps%d" % i, [128, 512], F32)) for i in range(8)]
        self.rps = [Res("ps%d" % i) for i in range(8)]
        self.top = 0
        self.dma_rr = 0

    def alloc(self, n, dt=BF16):
        nb = n if dt == BF16 else 2 * n
        self.top = (self.top + 31) // 32 * 32
        a = self.top
        self.top += nb
        assert self.top <= ARENA, "arena overflow %d" % self.top
        ap = self.arena[:, a:a + nb]
        if dt == F32:
            ap = ap.bitcast(F32)
        return ap

    def mm(self, out, lhsT, rhs, start, stop, rd, wr):
        return self.P.op("pe", lambda e: e.matmul(out, lhsT=lhsT, rhs=rhs, start=start, stop=stop), rd, wr)

    def tr(self, out, in_, rd, wr):
        ident = self.ident
        return self.P.op("pe", lambda e: e.transpose(out=out, in_=in_, identity=ident), list(rd) + [self.rconst], wr)

    def act(self, out, in_, func, rd, wr, scale=None, accum=None):
        kw = {}
        if scale is not None:
            kw["scale"] = scale
        if accum is not None:
            kw["accum_out"] = accum
        return self.P.op("act", lambda e: e.activation(out=out, in_=in_, func=func, **kw), rd, wr)

    def tt(self, eng, out, in0, in1, op, rd, wr):
        return self.P.op(eng, lambda e: e.tensor_tensor(out=out, in0=in0, in1=in1, op=op), rd, wr)

    def ts(self, eng, out, in0, s1, s2, op0, op1, rd, wr):
        if op1 is None:
            return self.P.op(eng, lambda e: e.tensor_scalar(out=out, in0=in0, scalar1=s1, scalar2=None, op0=op0), rd, wr)
        return self.P.op(eng, lambda e: e.tensor_scalar(out=out, in0=in0, scalar1=s1, scalar2=s2, op0=op0, op1=op1), rd, wr)

    def stt(self, out, in0, scalar, in1, op0, op1, rd, wr):
        return self.P.op("dve", lambda e: e.scalar_tensor_tensor(out=out, in0=in0, scalar=scalar, in1=in1, op0=op0, op1=op1), rd, wr)

    def cp(self, eng, out, in_, rd, wr):
        if eng == "act":
            return self.act(out, in_, AF.Copy, rd, wr)
        return self.P.op(eng, lambda e: e.tensor_copy(out=out, in_=in_), rd, wr)

    def recip(self, out, in_, rd, wr):
        return self.P.op("dve", lambda e: e.reciprocal(out=out, in_=in_), rd, wr)

    def memset(self, eng, ap, val, wr):
        return self.P.op(eng, lambda e: e.memset(ap, val), [], wr)

    def dma(self, out, in_, rd, wr, eng=None, slow=False):
        if eng is None:
            eng = "sp"
        if slow:
            fn = lambda e, s: e.dma_start(out=out, in_=in_, allow_slow_non_contiguous=True).then_inc(s, 16)
        else:
            fn = lambda e, s: e.dma_start(out=out, in_=in_).then_inc(s, 16)
        return self.P.op(eng, fn, rd, wr, ndma=1)

    def rstd_from_ssq(self, st, n, rd_wr):
        self.ts("dve", st[:, 1:2], st[:, 0:1], 1.0 / n, EPS, ALU.mult, ALU.add, [rd_wr], [rd_wr])
        self.act(st[:, 2:3], st[:, 1:2], AF.Sqrt, [rd_wr], [rd_wr])
        self.recip(st[:, 3:4], st[:, 2:3], [rd_wr], [rd_wr])

    def setup_consts(self):
        I = self.I
        self.rconst = Res("const")
        rc = self.rconst
        self.ident = self.alloc(128)
        self.jf = self.alloc(128, F32)
        self.maska = self.alloc(3 * 128)
        self.colv = self.alloc(64)
        self.ropec = self.alloc(NT * 64, F32).rearrange("p (t e) -> p t e", t=NT)
        self.ropes = self.alloc(NT * 64, F32).rearrange("p (t e) -> p t e", t=NT)
        self.eba = self.alloc(3 * 12 * 128).rearrange("p (r h q) -> p r h q", r=3, h=12)
        self.ebb = self.alloc(8 * 14 * 64).rearrange("p (h r q) -> p h r q", h=8, r=14)
        self.gq = {}
        self.dma(self.ident, I["c_ident"], [], [rc])
        self.dma(self.jf, I["c_j"], [], [rc])
        self.dma(self.maska, I["c_maska"].rearrange("k r q -> k (r q)"), [], [rc])
        self.dma(self.colv, I["c_colv"], [], [rc])
        self.dma(self.ropec, I["c_ropec"].rearrange("(t p) e -> p t e", p=128), [], [rc])
        self.dma(self.ropes, I["c_ropes"].rearrange("(t p) e -> p t e", p=128), [], [rc])
        LW = self.lw
        self.gains = self.alloc(LW * (6 * 64 + 2 * 128), F32)
        self.esink = self.alloc(LW * 12, F32)
        self.gcol = self.alloc(LW * 5 * KC, F32).rearrange("p (l g k) -> p l g k", l=LW, g=5)
        self.g = {}
        off = 0
        for l in range(LW):
            for nm, n2 in (("qk_norm_a", 64), ("qk_norm_b", 64), ("qk_norm_c", 64), ("qk_norm_mem", 128)):
                for i in range(2):
                    dst = self.gains[:, off:off + n2]
                    src = I[nm][l, i:i + 1, :]
                    src = bass.AP(src.tensor, src.offset, [[0, 128], [1, n2]])
                    self.dma(dst, src, [], [rc])
                    self.g[(nm, l, i)] = dst
                    off += n2
            src = I["sink_a"][l:l + 1, :]
            src = bass.AP(src.tensor, src.offset, [[0, 128], [1, 12]])
            self.dma(self.esink[:, l * 12:(l + 1) * 12], src, [], [rc])
            for gi, nm in enumerate(("norm_mix", "out_norm", "norm_mem", "norm_mem_kv", "norm_ffn")):
                src = I[nm][l]
                src = bass.AP(src.tensor, src.offset, [[1, 128], [128, KC]])
                self.dma(self.gcol[:, l, gi, :], src, [], [rc], slow=True)
        self.act(self.esink, self.esink, AF.Exp, [rc], [rc])
        self.build_eba()
        self.const_top = self.top

    def build_eba(self):
        I = self.I
        rc = self.rconst
        m = self.top
        relb = self.alloc(12, F32)
        oh = self.alloc(512, F32)
        tvs = self.alloc(512, F32)
        ht = self.alloc(3 * 128, F32).rearrange("p (r k) -> p r k", r=3)
        tmp = self.alloc(128)
        r1, r2, r3, r4 = Res("relb"), Res("tvs"), Res("tvd"), Res("ht")
        self.dma(relb[0:32, :], I["t5_rel_bias"], [], [r1])
        self.dma(oh[0:32, :], I["c_oh"], [], [r1])
        self.mm(self.ps[0][0:12, :], relb[0:32, :], oh[0:32, :], True, True, [r1], [self.rps[0]])
        self.act(tvs[0:12, :], self.ps[0][0:12, :], AF.Copy, [self.rps[0]], [r2])
        self.dma(self.Sx["tv"], tvs[0:12, :], [r2], [r3], eng="pool")
        tvt = self.Sx["tv"].tensor
        rtmp = Res("tmp")
        for h in range(12):
            src = bass.AP(tvt, h * 512, [[1, 128], [128, 3], [1, 128]])
            self.dma(ht, src, [r3], [r4])
            for r in range(3):
                pb = 1 + (h * 3 + r) % 2
                self.mm(self.ps[pb][:, 0:128], ht[:, r, :], self.jf, True, True, [r4, rc], [self.rps[pb]])
                self.act(tmp, self.ps[pb][:, 0:128], AF.Exp, [self.rps[pb]], [rtmp])
                self.tt("dve", self.eba[:, r, h, :], tmp, self.maska[:, r * 128:(r + 1) * 128], ALU.mult, [rtmp, rc], [rc])
        self.P.barrier()
        self.top = m

    def build_ebb(self, l):
        I = self.I
        rc = self.rconst
        m = self.top
        htb = self.alloc(14 * 128, F32).rearrange("p (r k) -> p r k", r=14)
        tmp = self.alloc(7 * 64)
        r1, r2, r3 = Res("wbz"), Res("htb"), Res("tmpb")
        wbt = self.Sx["wb"].tensor
        self.dma(self.Sx["wb"], I["c_zero"], [], [r1])
        for h in range(8):
            dst = bass.AP(wbt, h * 1152 + 48, [[64, 15], [1, 31]])
            self.dma(dst, I["rpb_b"][l, h], [r1], [r1])
        j64 = self.jf[0:64, 64:128]
        for h in range(8):
            src = bass.AP(wbt, h * 1152, [[1, 64], [64, 14], [1, 128]])
            self.dma(htb[0:64], src, [r1], [r2])
            for half in range(2):
                pb = 1 + half
                for r in range(7):
                    self.mm(self.ps[pb][:, r * 64:(r + 1) * 64], htb[0:64, half * 7 + r, :], j64, True, True,
                            [r2, rc], [self.rps[pb]])
                self.act(tmp, self.ps[pb][:, 0:448], AF.Exp, [self.rps[pb]], [r3])
                colb = bass.AP(self.colv.tensor, self.colv.offset, [list(self.colv.ap[0]), [0, 7], [1, 64]])
                self.tt("dve", self.ebb[:, h, half * 7:(half + 1) * 7, :], tmp.rearrange("p (r q) -> p r q", r=7), colb,
                        ALU.mult, [r3, rc], [rc])
        self.P.barrier()
        self.top = m

    def norm_transpose(self, src_rows, ntiles, dstT, rdst, rsrc=None, pbase=0, gcol=None):
        m = self.top
        xt = [self.alloc(D, F32) for _ in range(2)]
        xn = [self.alloc(D) for _ in range(2)]
        junk = self.alloc(D)
        stt_ = [self.alloc(8, F32) for _ in range(2)]
        rx = [Res("xt%d" % i) for i in range(2)]
        rn = [Res("xn%d" % i) for i in range(2)]
        rs = [Res("st%d" % i) for i in range(2)]
        rj = Res("junk")
        for t in range(ntiles):
            b = t % 2
            self.dma(xt[b], src_rows(t), [rsrc[t]] if rsrc else [], [rx[b]])
            self.act(junk, xt[b], AF.Square, [rx[b]], [rj, rs[b]], accum=stt_[b][:, 0:1])
            self.rstd_from_ssq(stt_[b], D, rs[b])
            self.act(xn[b], xt[b], AF.Copy, [rx[b], rs[b]], [rn[b]], scale=stt_[b][:, 3:4])
            for g in range(4):
                pb = pbase + g % 2
                pv = self.ps[pb][:, 0:256].bitcast(BF16)
                for j in range(4):
                    kc = g * 4 + j
                    self.tr(pv[:, j * 128:(j + 1) * 128], xn[b][:, kc * 128:(kc + 1) * 128], [rn[b]], [self.rps[pb]])
                eng = "dve" if g % 2 == 0 else "act"
                if gcol is None:
                    self.cp(eng, dstT[:, g * 4:(g + 1) * 4, t * 128:(t + 1) * 128], pv.rearrange("p (j q) -> p j q", j=4),
                            [self.rps[pb]], [rdst[t]])
                else:
                    for j in range(4):
                        kc = g * 4 + j
                        if (kc % 2) == 0:
                            self.act(dstT[:, kc, t * 128:(t + 1) * 128], pv[:, j * 128:(j + 1) * 128], AF.Copy, [self.rps[pb], self.rconst],
                                     [rdst[t]], scale=gcol[:, kc:kc + 1])
                        else:
                            self.ts("dve", dstT[:, kc, t * 128:(t + 1) * 128], pv[:, j * 128:(j + 1) * 128], gcol[:, kc:kc + 1], None,
                                    ALU.mult, None, [self.rps[pb], self.rconst], [rdst[t]])
        self.P.barrier()
        self.top = m

    def linear(self, actT, ract, ntiles, w_dram, kcn, ncols, nblk, gcol, epilogue, pbase=2):
        m = self.top
        wst = [self.alloc(kcn * nblk, F32).rearrange("p (k n) -> p k n", k=kcn) for _ in range(2)]
        wbf = [self.alloc(kcn * nblk).rearrange("p (k n) -> p k n", k=kcn) for _ in range(2)]
        rst = [Res("wst%d" % i) for i in range(2)]
        rwb = [[Res("wbf%d_%d" % (i, k)) for k in range(kcn)] for i in range(2)]
        wv = w_dram.rearrange("(k p) n -> p k n", p=128)
        engs = ["act", "dve", "pool"]
        for nb in range(ncols // nblk):
            b = nb % 2
            self.dma(wst[b], wv[:, :, nb * nblk:(nb + 1) * nblk], [], [rst[b]])
            for k in range(kcn):
                eng = engs[k % 3]
                if gcol is None:
                    self.cp(eng, wbf[b][:, k, :], wst[b][:, k, :], [rst[b]], [rwb[b][k]])
                elif eng == "act":
                    self.act(wbf[b][:, k, :], wst[b][:, k, :], AF.Copy, [rst[b], self.rconst], [rwb[b][k]], scale=gcol[:, k:k + 1])
                else:
                    self.ts(eng, wbf[b][:, k, :], wst[b][:, k, :], gcol[:, k:k + 1], None, ALU.mult, None,
                            [rst[b], self.rconst], [rwb[b][k]])
            for t in range(ntiles):
                pb = pbase + t % 2
                ps = self.ps[pb][:, 0:nblk]
                for k in range(kcn):
                    self.mm(ps, actT[:, k, t * 128:(t + 1) * 128], wbf[b][:, k, :], k == 0, k == kcn - 1,
                            [ract[t], rwb[b][k]], [self.rps[pb]])
                epilogue(nb, t, ps, self.rps[pb])
        self.P.barrier()
        self.top = m

    def headnorm(self, ps, rps, nh, dh, gain, sc):
        sq, ss, qn, qg = sc["sq"], sc["ss"], sc["qn"], sc["qg"]
        r = sc["r"]
        n = nh * dh
        self.act(sq[:, 0:n], ps, AF.Square, [rps], [r["sq"]])
        self.P.op("dve", lambda e: e.tensor_reduce(out=ss[:, 0:nh], in_=sq[:, 0:n].rearrange("p (h d) -> p h d", h=nh), axis=AX.X, op=ALU.add),
                  [r["sq"]], [r["ss"]])
        self.ts("dve", ss[:, 8:8 + nh], ss[:, 0:nh], 1.0 / dh, EPS, ALU.mult, ALU.add, [r["ss"]], [r["ss"]])
        self.act(ss[:, 16:16 + nh], ss[:, 8:8 + nh], AF.Sqrt, [r["ss"]], [r["ss"]])
        self.recip(ss[:, 24:24 + nh], ss[:, 16:16 + nh], [r["ss"]], [r["ss"]])
        rsb = ss[:, 24:24 + nh].unsqueeze(2).to_broadcast([128, nh, dh])
        self.tt("dve", qn[:, 0:n].rearrange("p (h d) -> p h d", h=nh), ps.rearrange("p (h d) -> p h d", h=nh), rsb, ALU.mult,
                [rps, r["ss"]], [r["qn"]])
        gb = gain.unsqueeze(1).to_broadcast([128, nh, dh])
        return gb

    def phase_A(self, l, xsrc):
        I, Sx = self.I, self.Sx
        m = self.top
        xnT = self.alloc(KC * S).rearrange("p (k t) -> p k t", k=KC)
        rxn = [Res("xnT%d" % t) for t in range(NT)]
        self.norm_transpose(lambda t: xsrc[t * 128:(t + 1) * 128, :], NT, xnT, rxn)
        scs_ = [{
            "sq": self.alloc(256, F32), "ss": self.alloc(32, F32), "qn": self.alloc(256, F32), "qg": self.alloc(256, F32),
            "t1": self.alloc(256, F32), "t2": self.alloc(256, F32),
            "r": {k: Res(k + str(i_)) for k in ("sq", "ss", "qn", "qg", "t1", "t2")},
        } for i_ in range(2)]
        qb = [self.alloc(256) for _ in range(2)]
        rqb = [Res("qb%d" % i) for i in range(2)]
        hT = [self.alloc(4 * S).rearrange("p (h t) -> p h t", h=4) for _ in range(2)]
        rhT = [Res("hT%d" % i) for i in range(2)]
        vst = [self.alloc(NT * 4 * 65).rearrange("p (t h e) -> p t h e", t=NT, h=4) for _ in range(1)]
        rvs = [Res("vst%d" % i) for i in range(1)]
        for i in range(1):
            self.memset("pool", vst[i][:, :, :, 64:65], 1.0, [rvs[i]])
        blocks = [("q", "qt_a", 0, ("qk_norm_a", 0), False), ("q", "qt_a", 4, ("qk_norm_a", 0), False), ("q", "qt_a", 8, ("qk_norm_a", 0), False),
                  ("q", "kt_a", 0, ("qk_norm_a", 1), False), ("v", "v_a", 0, None, False),
                  ("q", "qt_b", 0, ("qk_norm_b", 0), False), ("q", "qt_b", 4, ("qk_norm_b", 0), False),
                  ("q", "kt_b", 0, ("qk_norm_b", 1), False), ("q", "kt_b", 4, ("qk_norm_b", 1), False),
                  ("v", "v_b", 0, None, False), ("v", "v_b", 4, None, False),
                  ("q", "qt_c", 0, ("qk_norm_c", 0), True), ("q", "qt_c", 4, ("qk_norm_c", 0), True), ("q", "qt_c", 8, ("qk_norm_c", 0), True),
                  ("q", "kt_c", 0, ("qk_norm_c", 1), True), ("v", "v_c", 0, None, False)]
        cnt = {"q": 0, "v": 0, "e": 0}

        def epi(nb, t, ps, rps):
            kind, dst, h0, gk, rope = blocks[nb]
            if kind == "v":
                vb = 0
                cnt["v"] += 1
                self.cp("act", vst[vb][:, t, :, 0:64], ps.rearrange("p (h d) -> p h d", h=4), [rps], [rvs[vb]])
                if t == NT - 1:
                    dv = Sx[dst].rearrange("(t p) h e -> p t h e", p=128)[:, :, h0:h0 + 4, :]
                    self.dma(dv, vst[vb], [rvs[vb]], [], eng="pool")
                return
            hb = (cnt["q"] // NT) % 2
            cnt["q"] += 1
            e = cnt["e"] % 2
            cnt["e"] += 1
            sc = scs_[e]
            gain = self.g[(gk[0], l, gk[1])]
            gb = self.headnorm(ps, rps, 4, 64, gain, sc)
            r = sc["r"]
            qn3 = sc["qn"].rearrange("p (h d) -> p h d", h=4)
            if not rope:
                self.tt("dve", qb[e].rearrange("p (h d) -> p h d", h=4), qn3, gb, ALU.mult, [r["qn"], self.rconst], [rqb[e]])
            else:
                qg = sc["qg"]
                self.tt("dve", qg.rearrange("p (h d) -> p h d", h=4), qn3, gb, ALU.mult, [r["qn"], self.rconst], [r["qg"]])
                cc = self.ropec[:, t, :].unsqueeze(1).to_broadcast([128, 4, 64])
                self.tt("dve", sc["t1"].rearrange("p (h d) -> p h d", h=4), qg.rearrange("p (h d) -> p h d", h=4), cc, ALU.mult,
                        [r["qg"], self.rconst], [r["t1"]])
                qg5 = qg.rearrange("p (a x f) -> p a x f", x=2, f=16)
                t25 = sc["t2"].rearrange("p (a x f) -> p a x f", x=2, f=16)
                ss5 = self.ropes[:, t, :].rearrange("p (a x f) -> p a x f", x=2, f=16)
                for x in range(2):
                    src = qg5[:, :, 1 - x, :].rearrange("p (h a) f -> p h a f", h=4)
                    dst_ = t25[:, :, x, :].rearrange("p (h a) f -> p h a f", h=4)
                    sb = ss5[:, :, x, :].unsqueeze(1).to_broadcast([128, 4, 2, 16])
                    self.tt("dve", dst_, src, sb, ALU.mult, [r["qg"], self.rconst], [r["t2"]])
                self.tt("dve", qb[e], sc["t1"], sc["t2"], ALU.add, [r["t1"], r["t2"]], [rqb[e]])
            pb = 4 + e
            pv = self.ps[pb][:, 0:256].bitcast(BF16)
            for j in range(4):
                self.tr(pv[0:64, j * 128:(j + 1) * 128], qb[e][:, j * 64:(j + 1) * 64], [rqb[e]], [self.rps[pb]])
            self.cp("act", hT[hb][0:64, :, t * 128:(t + 1) * 128], pv[0:64, :].rearrange("p (j q) -> p j q", j=4),
                    [self.rps[pb]], [rhT[hb]])
            if t == NT - 1:
                dv = Sx[dst][h0:h0 + 4].rearrange("h d t -> d h t")
                self.dma(dv, hT[hb][0:64], [rhT[hb]], [], eng="pool")

        self.linear(xnT, rxn, NT, I["w_in"][l], KC, 4096, 256, self.gcol[:, l, 0, :], epi)
        self.top = m
        self.P.barrier()

    def outnorm_store(self, o_f32, n, parts, dst, sc):
        r = sc["r"]
        self.act(sc["junk"][0:parts, 0:n], o_f32, AF.Square, [r["o"]], [r["junk"], r["st"]], accum=sc["st"][0:parts, 0:1])
        self.rstd_from_ssq(sc["st"][0:parts], n, r["st"])
        self.act(sc["ob"][0:parts, 0:n], o_f32, AF.Copy, [r["o"], r["st"]], [r["ob"]], scale=sc["st"][0:parts, 3:4])
        self.dma(dst, sc["ob"][0:parts, 0:n], [r["ob"]], [], eng="pool")

    def mixer_AC(self, l, which):
        I, Sx = self.I, self.Sx
        m = self.top
        qn_, kn_, vn_ = ("qt_a", "kt_a", "v_a") if which == "a" else ("qt_c", "kt_c", "v_c")
        QT = self.alloc(12 * S).rearrange("p (h t) -> p h t", h=12)
        KT = self.alloc(4 * S).rearrange("p (h t) -> p h t", h=4)
        V = self.alloc(NT * 4 * 65).rearrange("p (t h e) -> p t h e", t=NT, h=4)
        rq, rk, rv = Res("QT"), Res("KT"), Res("V")
        self.dma(QT[0:64], Sx[qn_].rearrange("h d t -> d h t"), [], [rq])
        self.dma(KT[0:64], Sx[kn_].rearrange("h d t -> d h t"), [], [rk])
        self.dma(V, Sx[vn_].rearrange("(t p) h e -> p t h e", p=128), [], [rv])
        NR = 2
        Pe = [self.alloc(384) for _ in range(NR)]
        Pm = [self.alloc(384) for _ in range(NR)]
        rpe = [Res("pe%d" % i) for i in range(NR)]
        rpm = [Res("pm%d" % i) for i in range(NR)]
        scs = []
        for i in range(2):
            scs.append({"den": self.alloc(16, F32), "rden": self.alloc(16, F32), "o": self.alloc(768, F32), "junk": self.alloc(768),
                        "st": self.alloc(8, F32), "ob": self.alloc(768),
                        "r": {k: Res(k + str(i)) for k in ("den", "o", "junk", "st", "ob")}})
        rot = 0
        col0 = 0 if which == "a" else 1280
        for qb in range(NT):
            ob_ = qb % 2
            pso = [self.ps[2 + 3 * ob_ + g] for g in range(3)]
            rpo = [self.rps[2 + 3 * ob_ + g] for g in range(3)]
            kts = [kt for kt in ((qb - 1, qb, qb + 1) if which == "a" else range(NT)) if 0 <= kt < NT]
            for hk in range(4):
                for idx, kt in enumerate(kts):
                    s_ = rot % NR
                    rot += 1
                    ps = self.ps[s_][:, 0:384]
                    self.mm(ps, KT[0:64, hk, kt * 128:(kt + 1) * 128], QT[0:64, 3 * hk:3 * hk + 3, qb * 128:(qb + 1) * 128],
                            True, True, [rk, rq], [self.rps[s_]])
                    self.act(Pe[s_], ps, AF.Exp, [self.rps[s_]], [rpe[s_]], scale=0.125)
                    if which == "a":
                        ri = kt - qb + 1
                        self.tt("dve", Pm[s_].rearrange("p (g q) -> p g q", g=3), Pe[s_].rearrange("p (g q) -> p g q", g=3),
                                self.eba[:, ri, 3 * hk:3 * hk + 3, :], ALU.mult, [rpe[s_], self.rconst], [rpm[s_]])
                        pp, rpp = Pm[s_], rpm[s_]
                    else:
                        pp, rpp = Pe[s_], rpe[s_]
                    for g in range(3):
                        self.mm(pso[g][:, hk * 65:hk * 65 + 65], pp[:, g * 128:(g + 1) * 128], V[:, kt, hk, :],
                                idx == 0, idx == len(kts) - 1, [rpp, rv], [rpo[g]])
            sc = scs[ob_]
            r = sc["r"]
            den3 = sc["den"][:, 0:12].rearrange("p (k g) -> p k g", g=3)
            rden3 = sc["rden"][:, 0:12].rearrange("p (k g) -> p k g", g=3)
            es3 = self.esink[:, l * 12:(l + 1) * 12].rearrange("p (k g) -> p k g", g=3)
            o4 = sc["o"].rearrange("p (k g d) -> p k g d", k=4, g=3)
            for g in range(3):
                o3 = pso[g][:, 0:260].rearrange("p (k e) -> p k e", k=4)
                if which == "a":
                    self.tt("dve", den3[:, :, g], o3[:, :, 64], es3[:, :, g], ALU.add, [rpo[g], self.rconst], [r["den"]])
                else:
                    self.cp("dve", den3[:, :, g], o3[:, :, 64], [rpo[g]], [r["den"]])
            self.recip(sc["rden"][:, 0:12], sc["den"][:, 0:12], [r["den"]], [r["den"]])
            for g in range(3):
                o3 = pso[g][:, 0:260].rearrange("p (k e) -> p k e", k=4)
                rb = rden3[:, :, g].unsqueeze(2).to_broadcast([128, 4, 64])
                self.tt("dve", o4[:, :, g, :], o3[:, :, 0:64], rb, ALU.mult, [rpo[g], r["den"]], [r["o"]])
            self.outnorm_store(sc["o"], 768, 128, Sx["mixed"][qb * 128:(qb + 1) * 128, col0:col0 + 768], sc)
        self.top = m
        self.P.barrier()

    def mixer_B(self, l):
        I, Sx = self.I, self.Sx
        m = self.top
        QT = self.alloc(8 * S).rearrange("p (h t) -> p h t", h=8)
        KT = self.alloc(8 * S).rearrange("p (h t) -> p h t", h=8)
        rq, rk = Res("QTb"), Res("KTb")
        self.dma(QT[0:64], Sx["qt_b"].rearrange("h d t -> d h t"), [], [rq])
        self.dma(KT[0:64], Sx["kt_b"].rearrange("h d t -> d h t"), [], [rk])
        Vr = [self.alloc(4 * 8 * 65).rearrange("p (i h e) -> p i h e", i=4, h=8) for _ in range(2)]
        rvr = [Res("vr%d" % i) for i in range(2)]
        NR = 3
        Pe = [self.alloc(512) for _ in range(NR)]
        Pm = [self.alloc(512) for _ in range(NR)]
        rpe = [Res("peb%d" % i) for i in range(NR)]
        rpm = [Res("pmb%d" % i) for i in range(NR)]
        scs = []
        for i in range(2):
            scs.append({"rden": self.alloc(16, F32), "o": self.alloc(512, F32), "junk": self.alloc(512), "st": self.alloc(8, F32),
                        "ob": self.alloc(512), "r": {k: Res(k + "b" + str(i)) for k in ("den", "o", "junk", "st", "ob")}})
        vbt = Sx["v_b"].tensor
        rot = 0
        for r_ in range(32):
            r0 = min(max(r_ - 4, 0), 24)
            s_ = r_ - r0
            vb = r_ % 2
            src = bass.AP(vbt, 64 * r0 * 520, [[520, 128], [128 * 520, 4], [1, 520]])
            self.dma(Vr[vb].rearrange("p i h e -> p i (h e)"), src, [], [rvr[vb]])
            ob_ = r_ % 2
            pso = [self.ps[3 + 2 * ob_], self.ps[4 + 2 * ob_]]
            rpo = [self.rps[3 + 2 * ob_], self.rps[4 + 2 * ob_]]
            for hp in range(4):
                sl = rot % NR
                rot += 1
                ps = self.ps[sl][:, 0:512].rearrange("p (a i q) -> p a i q", a=2, i=4)
                for hh in range(2):
                    h = 2 * hp + hh
                    for i in range(4):
                        k0 = 64 * r0 + 128 * i
                        self.mm(ps[:, hh, i, :], KT[0:64, h, k0:k0 + 128], QT[0:64, h, 64 * r_:64 * r_ + 64], True, True,
                                [rk, rq], [self.rps[sl]])
                self.act(Pe[sl], self.ps[sl][:, 0:512], AF.Exp, [self.rps[sl]], [rpe[sl]], scale=0.125)
                eb = self.ebb[:, 2 * hp:2 * hp + 2, 7 - s_:7 - s_ + 7:2, :]
                self.tt("dve", Pm[sl].rearrange("p (a i q) -> p a i q", a=2, i=4), Pe[sl].rearrange("p (a i q) -> p a i q", a=2, i=4), eb,
                        ALU.mult, [rpe[sl], self.rconst], [rpm[sl]])
                pm4 = Pm[sl].rearrange("p (a i q) -> p a i q", a=2, i=4)
                for hh in range(2):
                    h = 2 * hp + hh
                    for i in range(4):
                        self.mm(pso[h // 4][0:64, (h % 4) * 65:(h % 4) * 65 + 65], pm4[:, hh, i, :], Vr[vb][:, i, h, :], i == 0, i == 3,
                                [rpm[sl], rvr[vb]], [rpo[h // 4]])
            sc = scs[ob_]
            r = sc["r"]
            for b in range(2):
                o3 = pso[b][0:64, 0:260].rearrange("p (h e) -> p h e", h=4)
                self.recip(sc["rden"][0:64, 4 * b:4 * b + 4], o3[:, :, 64], [rpo[b]], [r["den"]])
            for b in range(2):
                o3 = pso[b][0:64, 0:260].rearrange("p (h e) -> p h e", h=4)
                rb = sc["rden"][0:64, 4 * b:4 * b + 4].unsqueeze(2).to_broadcast([64, 4, 64])
                self.tt("dve", sc["o"][0:64, 256 * b:256 * b + 256].rearrange("p (h d) -> p h d", h=4), o3[:, :, 0:64], rb, ALU.mult,
                        [rpo[b], r["den"]], [r["o"]])
            self.outnorm_store(sc["o"][0:64], 512, 64, Sx["mixed"][64 * r_:64 * r_ + 64, 768:1280], sc)
        self.top = m
        self.P.barrier()

    def residual_epilogue(self, xsrc, xdst, nblk, rrow):
        m_ = {}
        xt = [self.alloc(nblk, F32) for _ in range(3)]
        rxt = [Res("rxt%d" % i) for i in range(3)]
        cnt = [0]

        def epi(nb, t, ps, rps):
            b = cnt[0] % 3
            cnt[0] += 1
            self.dma(xt[b], xsrc[t * 128:(t + 1) * 128, nb * nblk:(nb + 1) * nblk], [rrow[t]] if rrow else [], [rxt[b]])
            self.tt("dve", xt[b], ps, xt[b], ALU.add, [rps, rxt[b]], [rxt[b]])
            self.dma(xdst[t * 128:(t + 1) * 128, nb * nblk:(nb + 1) * nblk], xt[b], [rxt[b]], [rrow[t]] if rrow else [], eng="pool")

        return epi

    def phase_C(self, l, xsrc, xdst, inplace):
        I, Sx = self.I, self.Sx
        m = self.top
        mixT = self.alloc(KC * S).rearrange("p (k t) -> p k t", k=KC)
        rmx = [Res("mixT%d" % t) for t in range(NT)]
        mt = [self.alloc(D) for _ in range(2)]
        rmt = [Res("mt%d" % i) for i in range(2)]
        for t in range(NT):
            b = t % 2
            self.dma(mt[b], Sx["mixed"][t * 128:(t + 1) * 128, :], [], [rmt[b]])
            for g in range(4):
                pb = g % 2
                pv = self.ps[pb][:, 0:256].bitcast(BF16)
                for j in range(4):
                    kc = g * 4 + j
                    self.tr(pv[:, j * 128:(j + 1) * 128], mt[b][:, kc * 128:(kc + 1) * 128], [rmt[b]], [self.rps[pb]])
                self.cp("dve" if g % 2 == 0 else "act", mixT[:, g * 4:(g + 1) * 4, t * 128:(t + 1) * 128],
                        pv.rearrange("p (j q) -> p j q", j=4), [self.rps[pb]], [rmx[t]])
        rrow = [Res("row%d" % t) for t in range(NT)] if inplace else None
        epi = self.residual_epilogue(xsrc, xdst, 256, rrow)
        self.linear(mixT, rmx, NT, I["w_out"][l], KC, D, 256, self.gcol[:, l, 1, :], epi)
        self.top = m
        self.P.barrier()

    def phase_D(self, l, s, xio):
        I, Sx = self.I, self.Sx
        m = self.top
        memT = self.alloc(KC * NMEM).rearrange("p (k t) -> p k t", k=KC)
        rmemT = [Res("memT%d" % t) for t in range(2)]
        msrc = I["mem"][s]
        self.norm_transpose(lambda t: msrc[t * 128:(t + 1) * 128, :], 2, memT, rmemT)
        KmT = self.alloc(4 * NMEM).rearrange("p (h t) -> p h t", h=4)
        Vm = self.alloc(2 * 4 * 129).rearrange("p (t h e) -> p t h e", t=2, h=4)
        rkm, rvm = Res("KmT"), Res("Vm")
        self.memset("pool", Vm[:, :, :, 128:129], 1.0, [rvm])
        sc = {"sq": self.alloc(512, F32), "ss": self.alloc(32, F32), "qn": self.alloc(512, F32), "qg": None,
              "r": {k: Res(k + "d") for k in ("sq", "ss", "qn")}}
        qb = [self.alloc(512) for _ in range(2)]
        rqb = [Res("qbd%d" % i) for i in range(2)]
        cnt = [0]

        def epi_kv(nb, t, ps, rps):
            if nb == 1:
                self.cp("act", Vm[:, t, :, 0:128], ps.rearrange("p (h d) -> p h d", h=4), [rps], [rvm])
                return
            e = cnt[0] % 2
            cnt[0] += 1
            gb = self.headnorm(ps, rps, 4, 128, self.g[("qk_norm_mem", l, 1)], sc)
            self.tt("dve", qb[e].rearrange("p (h d) -> p h d", h=4), sc["qn"].rearrange("p (h d) -> p h d", h=4), gb, ALU.mult,
                    [sc["r"]["qn"], self.rconst], [rqb[e]])
            pb = 4 + e
            pv = self.ps[pb][:, 0:256].bitcast(BF16)
            for j in range(4):
                self.tr(pv[:, j * 128:(j + 1) * 128], qb[e][:, j * 128:(j + 1) * 128], [rqb[e]], [self.rps[pb]])
            self.cp("act", KmT[:, :, t * 128:(t + 1) * 128], pv.rearrange("p (j q) -> p j q", j=4), [self.rps[pb]], [rkm])

        self.linear(memT, rmemT, 2, I["w_mem_kv"][l], KC, 1024, 512, self.gcol[:, l, 3, :], epi_kv)
        QmT = self.alloc(4 * S).rearrange("p (h t) -> p h t", h=4)
        rqm = [Res("QmT%d" % t) for t in range(NT)]
        mD2 = self.top
        xnT = self.alloc(KC * S).rearrange("p (k t) -> p k t", k=KC)
        rxn = [Res("xnTd%d" % t) for t in range(NT)]
        rrow = [Res("rowd%d" % t) for t in range(NT)]
        self.norm_transpose(lambda t: xio[t * 128:(t + 1) * 128, :], NT, xnT, rxn, rsrc=rrow)

        def epi_q(nb, t, ps, rps):
            e = cnt[0] % 2
            cnt[0] += 1
            gb = self.headnorm(ps, rps, 2, 128, self.g[("qk_norm_mem", l, 0)], sc)
            self.tt("dve", qb[e][:, 0:256].rearrange("p (h d) -> p h d", h=2), sc["qn"][:, 0:256].rearrange("p (h d) -> p h d", h=2), gb, ALU.mult,
                    [sc["r"]["qn"], self.rconst], [rqb[e]])
            pb = 4 + e
            pv = self.ps[pb][:, 0:256].bitcast(BF16)
            for j in range(2):
                self.tr(pv[:, j * 128:(j + 1) * 128], qb[e][:, j * 128:(j + 1) * 128], [rqb[e]], [self.rps[pb]])
            self.cp("act", QmT[:, 2 * nb:2 * nb + 2, t * 128:(t + 1) * 128], pv[:, 0:256].rearrange("p (j q) -> p j q", j=2), [self.rps[pb]], [rqm[t]])

        self.linear(xnT, rxn, NT, I["w_mem_q"][l], KC, 512, 256, self.gcol[:, l, 2, :], epi_q)
        self.top = mD2
        wo_st = self.alloc(4 * 512, F32).rearrange("p (k n) -> p k n", k=4)
        wo = self.alloc(4 * D).rearrange("p (k n) -> p k n", k=4)
        rwst, rwo = Res("wost"), Res("wo")
        wov = I["w_mem_o"][l].rearrange("(k p) n -> p k n", p=128)
        for nb in range(4):
            self.dma(wo_st, wov[:, :, nb * 512:(nb + 1) * 512], [], [rwst])
            self.cp("pool" if nb % 2 else "act", wo[:, :, nb * 512:(nb + 1) * 512], wo_st, [rwst], [rwo])
        Pe = [self.alloc(1024) for _ in range(2)]
        rpe = [Res("ped%d" % i) for i in range(2)]
        rden = [self.alloc(8, F32) for _ in range(2)]
        om = [self.alloc(512) for _ in range(2)]
        omT = [self.alloc(512).rearrange("p (h t) -> p h t", h=4) for _ in range(2)]
        xt = [self.alloc(D, F32) for _ in range(2)]
        rrd = [Res("rdend%d" % i) for i in range(2)]
        rom = [Res("om%d" % i) for i in range(2)]
        romT = [Res("omT%d" % i) for i in range(2)]
        rxt = [Res("xtd%d" % i) for i in range(2)]
        scale = 128.0 ** -0.5
        for t in range(NT):
            b = t % 2
            self.dma(xt[b], xio[t * 128:(t + 1) * 128, :], [rrow[t]], [rxt[b]])
            for mt_ in range(2):
                for h in range(4):
                    self.mm(self.ps[mt_][:, h * 128:(h + 1) * 128], KmT[:, h, mt_ * 128:(mt_ + 1) * 128], QmT[:, h, t * 128:(t + 1) * 128],
                            True, True, [rkm, rqm[t]], [self.rps[mt_]])
                self.act(Pe[b][:, mt_ * 512:(mt_ + 1) * 512], self.ps[mt_][:, 0:512], AF.Exp, [self.rps[mt_]], [rpe[b]], scale=scale)
            for h in range(4):
                pb, off = (2, h * 129) if h < 3 else (3, 0)
                for mt_ in range(2):
                    self.mm(self.ps[pb][:, off:off + 129], Pe[b][:, mt_ * 512 + h * 128:mt_ * 512 + (h + 1) * 128], Vm[:, mt_, h, :],
                            mt_ == 0, mt_ == 1, [rpe[b], rvm], [self.rps[pb]])
            o3 = self.ps[2][:, 0:387].rearrange("p (h e) -> p h e", h=3)
            self.recip(rden[b][:, 0:3], o3[:, :, 128], [self.rps[2]], [rrd[b]])
            self.recip(rden[b][:, 3:4], self.ps[3][:, 128:129], [self.rps[3]], [rrd[b]])
            self.tt("dve", om[b][:, 0:384].rearrange("p (h d) -> p h d", h=3), o3[:, :, 0:128],
                    rden[b][:, 0:3].unsqueeze(2).to_broadcast([128, 3, 128]), ALU.mult, [self.rps[2], rrd[b]], [rom[b]])
            self.ts("dve", om[b][:, 384:512], self.ps[3][:, 0:128], rden[b][:, 3:4], None, ALU.mult, None, [self.rps[3], rrd[b]], [rom[b]])
            pv = self.ps[4][:, 0:256].bitcast(BF16)
            for j in range(4):
                self.tr(pv[:, j * 128:(j + 1) * 128], om[b][:, j * 128:(j + 1) * 128], [rom[b]], [self.rps[4]])
            self.cp("act", omT[b], pv.rearrange("p (j q) -> p j q", j=4), [self.rps[4]], [romT[b]])
            for nb in range(4):
                pb = 5 + nb % 2
                for k in range(4):
                    self.mm(self.ps[pb][:, 0:512], omT[b][:, k, :], wo[:, k, nb * 512:(nb + 1) * 512], k == 0, k == 3,
                            [romT[b], rwo], [self.rps[pb]])
                self.tt("dve", xt[b][:, nb * 512:(nb + 1) * 512], self.ps[pb][:, 0:512], xt[b][:, nb * 512:(nb + 1) * 512], ALU.add,
                        [self.rps[pb], rxt[b]], [rxt[b]])
            self.dma(xio[t * 128:(t + 1) * 128, :], xt[b], [rxt[b]], [rrow[t]], eng="pool")
        self.top = m
        self.P.barrier()

    def phase_P(self, l):
        I = self.I
        m = self.top
        ust = [self.alloc(D, F32) for _ in range(2)]
        ubf = [self.alloc(D) for _ in range(2)]
        uTb = [self.alloc(KC * 128).rearrange("p (k e) -> p k e", k=KC) for _ in range(2)]
        vst = [self.alloc(D, F32) for _ in range(2)]
        vb = [self.alloc(D) for _ in range(2)]
        r = {k: [Res(k + str(i)) for i in range(2)] for k in ("ust", "ubf", "uTb", "vst", "vb")}
        uv = I["peer_u"][l]
        vv = I["peer_v"][l]
        for c in range(128):
            b = c % 2
            self.dma(ust[b], uv[c * 128:(c + 1) * 128, :], [], [r["ust"][b]])
            self.cp("pool", ubf[b], ust[b], [r["ust"][b]], [r["ubf"][b]])
            for g in range(4):
                pb = g % 2
                pv = self.ps[pb][:, 0:256].bitcast(BF16)
                for j in range(4):
                    k = g * 4 + j
                    self.tr(pv[:, j * 128:(j + 1) * 128], ubf[b][:, k * 128:(k + 1) * 128], [r["ubf"][b]], [self.rps[pb]])
                self.cp("act" if g % 2 else "dve", uTb[b][:, g * 4:(g + 1) * 4, :], pv.rearrange("p (j q) -> p j q", j=4),
                        [self.rps[pb]], [r["uTb"][b]])
            self.dma(self.ut_s[c], uTb[b].rearrange("p k e -> p (k e)"), [r["uTb"][b]], [], eng="pool")
            self.dma(vst[b], vv[c * 128:(c + 1) * 128, :], [], [r["vst"][b]])
            self.cp("act" if c % 2 else "dve", vb[b], vst[b], [r["vst"][b]], [r["vb"][b]])
            self.dma(self.v_s[c * 128:(c + 1) * 128, :], vb[b], [r["vb"][b]], [], eng="pool")
        self.P.barrier()
        self.top = m

    def phase_E(self, l, xio):
        I, Sx = self.I, self.Sx
        m = self.top
        TP = 512
        NTP = 4
        keysT = self.alloc(16 * 128).rearrange("p (a n) -> p a n", a=16)
        rkt = Res("keysT")
        m1 = self.top
        kst = self.alloc(16 * 128, F32).rearrange("p (a n) -> p a n", a=16)
        kbf = self.alloc(16 * 128).rearrange("p (a n) -> p a n", a=16)
        rks, rkb = Res("kst"), Res("kbf")
        self.dma(kst, I["peer_keys"][l].rearrange("h c n d -> n (h c) d"), [], [rks])
        self.cp("dve", kbf, kst, [rks], [rkb])
        for g in range(4):
            pv = self.ps[g % 2][:, 0:256].bitcast(BF16)
            for j in range(4):
                self.tr(pv[:, j * 128:(j + 1) * 128], kbf[:, g * 4 + j, :], [rkb], [self.rps[g % 2]])
            self.cp("act", keysT[:, g * 4:(g + 1) * 4, :], pv.rearrange("p (j q) -> p j q", j=4), [self.rps[g % 2]], [rkt])
        self.P.barrier()
        gcol = self.gcol[:, l, 4, :]
        for p_ in range(S // TP):
            self.top = m1
            rows = lambda t: xio[p_ * TP + t * 128:p_ * TP + (t + 1) * 128, :]
            hnT = self.alloc(KC * TP).rearrange("p (k t) -> p k t", k=KC)
            rhn = [Res("hnT%d" % t) for t in range(NTP)]
            s1b = self.alloc(NTP * 8 * 128, F32).rearrange("p (t h n) -> p t h n", t=NTP, h=8)
            phi = self.alloc(NTP * 128 * 8, F32).rearrange("p (t c h) -> p t c h", t=NTP, c=128)
            Dm = self.alloc(NTP * 8 * 128).rearrange("p (t h q) -> p t h q", t=NTP, h=8)
            ytok = self.alloc(NTP * D, F32).rearrange("p (t d) -> p t d", t=NTP)
            rs1 = [Res("s1b%d" % t) for t in range(NTP)]
            rphi = [Res("phi%d" % t) for t in range(NTP)]
            rdm = [Res("Dm%d" % t) for t in range(NTP)]
            ryt = [[Res("yt%d_%d" % (t, d)) for d in range(4)] for t in range(NTP)]
            m2 = self.top
            self.norm_transpose(rows, NTP, hnT, rhn, gcol=gcol)
            qT = self.alloc(16 * TP).rearrange("p (a t) -> p a t", a=16)
            rqT = [Res("qT%d" % a) for a in range(16)]
            wst = [self.alloc(KC * 128, F32).rearrange("p (k n) -> p k n", k=KC) for _ in range(2)]
            wbf = [self.alloc(KC * 128).rearrange("p (k n) -> p k n", k=KC) for _ in range(2)]
            rst = [Res("pwst%d" % i) for i in range(2)]
            rwb = [[Res("pwbf%d_%d" % (i, k)) for k in range(KC)] for i in range(2)]
            wv = I["peer_w_q"][l].rearrange("(k p) n -> p k n", p=128)
            engs = ["act", "dve", "pool"]
            for nb in range(16):
                b = nb % 2
                self.dma(wst[b], wv[:, :, nb * 128:(nb + 1) * 128], [], [rst[b]])
                self.P.op("act" if nb % 2 else "pool", (lambda e, o_=wbf[b], i_=wst[b]: e.activation(out=o_, in_=i_, func=AF.Copy)) if nb % 2 else
                          (lambda e, o_=wbf[b], i_=wst[b]: e.tensor_copy(out=o_, in_=i_)), [rst[b]], rwb[b])
                for ch in range(1):
                    a = nb
                    pb = 2 + a % 2
                    for k in range(KC):
                        self.mm(self.ps[pb][:, 0:TP], wbf[b][:, k, ch * 128:(ch + 1) * 128], hnT[:, k, :], k == 0, k == KC - 1,
                                [rwb[b][k]] + rhn, [self.rps[pb]])
                    self.cp("act" if a % 2 else "dve", qT[:, a, :], self.ps[pb][:, 0:TP], [self.rps[pb]], [rqT[a]])
            s0t = self.alloc(8 * 128, F32).rearrange("p (h n) -> p h n", h=8)
            top = self.alloc(16 * 16, F32).rearrange("p (a k) -> p a k", a=16)
            wk = self.alloc(256, F32)
            cand = self.alloc(8 * 256, F32).rearrange("p (h a b) -> p h a b", h=8, a=16)
            best = self.alloc(8 * 16, F32).rearrange("p (h k) -> p h k", h=8)
            eb_ = self.alloc(8 * 16, F32).rearrange("p (h k) -> p h k", h=8)
            sm = self.alloc(64, F32)
            rs0, rtop, rwk, rcand, rbest, rsm = Res("s0t"), Res("top"), Res("wk"), Res("cand"), Res("best"), Res("sm")
            for t in range(NTP):
                for g in range(4):
                    pb = 4 + g % 2
                    for j in range(4):
                        a = g * 4 + j
                        self.mm(self.ps[pb][:, j * 128:(j + 1) * 128], qT[:, a, t * 128:(t + 1) * 128], keysT[:, a, :], True, True,
                                [rqT[a], rkt], [self.rps[pb]])
                    p4 = self.ps[pb][:, 0:512].rearrange("p (h c n) -> p h c n", h=2, c=2)
                    self.cp("act", s0t[:, 2 * g:2 * g + 2, :], p4[:, :, 0, :], [self.rps[pb]], [rs0])
                    self.cp("act", s1b[:, t, 2 * g:2 * g + 2, :], p4[:, :, 1, :], [self.rps[pb]], [rs1[t]])
                for a in range(16):
                    src = s0t[:, a // 2, :] if a % 2 == 0 else s1b[:, t, a // 2, :]
                    rsrc_ = rs0 if a % 2 == 0 else rs1[t]
                    self.P.op("dve", lambda e, src=src, a=a: e.max(out=top[:, a, 0:8], in_=src), [rsrc_], [rtop])
                    self.P.op("dve", lambda e, src=src, a=a: e.match_replace(out=wk[:, 0:128], in_to_replace=top[:, a, 0:8], in_values=src, imm_value=-1e30),
                              [rsrc_, rtop], [rwk])
                    self.P.op("dve", lambda e, a=a: e.max(out=top[:, a, 8:16], in_=wk[:, 0:128]), [rwk], [rtop])
                top4 = top.rearrange("p (h c) k -> p h c k", c=2)
                in0 = top4[:, :, 0, :].unsqueeze(3).to_broadcast([128, 8, 16, 16])
                in1 = top4[:, :, 1, :].unsqueeze(2).to_broadcast([128, 8, 16, 16])
                self.tt("dve", cand, in0, in1, ALU.add, [rtop], [rcand])
                for h in range(8):
                    cf = cand[:, h].rearrange("p a b -> p (a b)")
                    self.P.op("dve", lambda e, cf=cf, h=h: e.max(out=best[:, h, 0:8], in_=cf), [rcand], [rbest])
                    self.P.op("dve", lambda e, cf=cf, h=h: e.match_replace(out=wk, in_to_replace=best[:, h, 0:8], in_values=cf, imm_value=-1e30),
                              [rcand, rbest], [rwk])
                    self.P.op("dve", lambda e, h=h: e.max(out=best[:, h, 8:16], in_=wk), [rwk], [rbest])
                self.cp("dve", sm[:, 0:8], best[:, :, 0], [rbest], [rsm])
                self.ts("dve", sm[:, 8:16], best[:, :, 15], -1e-5, None, ALU.add, None, [rbest], [rsm])
                self.tt("dve", eb_, best, sm[:, 0:8].unsqueeze(2).to_broadcast([128, 8, 16]), ALU.subtract, [rbest, rsm], [rbest])
                self.act(eb_, eb_, AF.Exp, [rbest], [rbest])
                self.P.op("dve", lambda e: e.tensor_reduce(out=sm[:, 16:24], in_=eb_, axis=AX.X, op=ALU.add), [rbest], [rsm])
                self.recip(sm[:, 24:32], sm[:, 16:24], [rsm], [rsm])
                self.tt("dve", sm[:, 32:40], sm[:, 8:16], sm[:, 0:8], ALU.subtract, [rsm], [rsm])
                self.act(sm[:, 32:40], sm[:, 32:40], AF.Exp, [rsm], [rsm])
                self.tt("dve", sm[:, 40:48], sm[:, 32:40], sm[:, 24:32], ALU.mult, [rsm], [rsm])
                self.tt("dve", phi[:, t].rearrange("p c h -> p h c"), s0t, sm[:, 8:16].unsqueeze(2).to_broadcast([128, 8, 128]), ALU.subtract,
                        [rs0, rsm], [rphi[t]])
                for h in range(8):
                    eng = "pool" if h % 2 else "dve"
                    self.ts(eng, Dm[:, t, h, :], self.ident, sm[:, 40 + h:41 + h], None, ALU.mult, None, [rsm, self.rconst], [rdm[t]])
                if self.dbg and p_ == 0 and t == 0:
                    self.dma(Sx["dbg_sm"], sm, [rsm], [], eng="pool")
                    self.dma(Sx["dbg_s0"], s0t.rearrange("p h n -> p (h n)"), [rs0], [], eng="pool")
                    self.dma(Sx["dbg_s1"], s1b[:, 0].rearrange("p h n -> p (h n)"), [rs1[0]], [], eng="pool")
            self.P.barrier()
            self.top = m2
            uT = [self.alloc(KC * 128).rearrange("p (k e) -> p k e", k=KC) for _ in range(2)]
            vbf = [self.alloc(2 * D).rearrange("p (c d) -> p c d", c=2) for _ in range(3)]
            wT = [self.alloc(2 * TP).rearrange("p (c t) -> p c t", c=2) for _ in range(3)]
            zz = [self.alloc(2048, F32) for _ in range(2)]
            E_ = [self.alloc(2048) for _ in range(2)]
            Gm = [self.alloc(2048) for _ in range(2)]
            ge = [self.alloc(TP) for _ in range(2)]
            ruT = [Res("uT%d" % i) for i in range(2)]
            rvbf = [[Res("vbf%d_%d" % (i, c)) for c in range(2)] for i in range(3)]
            rwT = [[Res("wT%d_%d" % (i, c)) for c in range(2)] for i in range(3)]
            rzz = [Res("zz%d" % i) for i in range(2)]
            rE = [Res("E%d" % i) for i in range(2)]
            rGm = [Res("Gm%d" % i) for i in range(2)]
            rge = [Res("ge%d" % i) for i in range(2)]
            zi = 0

            def y_stage(pa):
                chunks = [(pa % 3, 0), (pa % 3, 1), ((pa + 1) % 3, 0), ((pa + 1) % 3, 1)]
                for t in range(NTP):
                    for d4 in range(4):
                        pb2 = 4 + (t * 4 + d4) % 4
                        for i_, (sl_, cc) in enumerate(chunks):
                            self.mm(self.ps[pb2][:, 0:512], wT[sl_][:, cc, t * 128:(t + 1) * 128], vbf[sl_][:, cc, d4 * 512:(d4 + 1) * 512],
                                    i_ == 0, i_ == 3, [rwT[sl_][cc], rvbf[sl_][cc]], [self.rps[pb2]])
                        dst = ytok[:, t, d4 * 512:(d4 + 1) * 512]
                        if pa == 0:
                            self.cp("act", dst, self.ps[pb2][:, 0:512], [self.rps[pb2]], [ryt[t][d4]])
                        else:
                            self.tt("dve", dst, self.ps[pb2][:, 0:512], dst, ALU.add, [self.rps[pb2], ryt[t][d4]], [ryt[t][d4]])

            def p1(cp_, t, z):
                in0 = s1b[:, t].unsqueeze(1).to_broadcast([128, 2, 8, 128])
                in1 = phi[:, t, 2 * cp_:2 * cp_ + 2, :].unsqueeze(3).to_broadcast([128, 2, 8, 128])
                self.tt("dve", zz[z].rearrange("p (c h n) -> p c h n", c=2, h=8), in0, in1, ALU.add, [rs1[t], rphi[t]], [rzz[z]])

            for cp_ in range(64):
                yg = cp_ % 3
                if cp_ >= 2 and cp_ % 2 == 0:
                    y_stage(cp_ - 2)
                for cc in range(2):
                    c = 2 * cp_ + cc
                    self.dma(uT[cc].rearrange("p k e -> p (k e)"), self.ut_s[c], [], [ruT[cc]])
                    self.dma(vbf[yg][:, cc, :], self.v_s[c * 128:(c + 1) * 128, :], [], [rvbf[yg][cc]])
                    for k in range(KC):
                        self.mm(self.ps[cc][:, 0:TP], uT[cc][:, k, :], hnT[:, k, :], k == 0, k == KC - 1, [ruT[cc]] + rhn, [self.rps[cc]])
                    self.act(ge[cc], self.ps[cc][:, 0:TP], AF.Gelu, [self.rps[cc]], [rge[cc]])
                p1(cp_, 0, zi % 2)
                for t in range(NTP):
                    z = zi % 2
                    zi += 1
                    if t + 1 < NTP:
                        p1(cp_, t + 1, zi % 2)
                    self.act(E_[z], zz[z], AF.Exp, [rzz[z]], [rE[z]])
                    self.stt(Gm[z], zz[z], 0.0, E_[z], ALU.is_ge, ALU.mult, [rzz[z], rE[z]], [rGm[z]])
                    for cc in range(2):
                        gb_ = 2 + cc
                        for h in range(8):
                            o_ = (cc * 8 + h) * 128
                            self.mm(self.ps[gb_][:, t * 128:(t + 1) * 128], Gm[z][:, o_:o_ + 128], Dm[:, t, h, :], h == 0, h == 7,
                                    [rGm[z], rdm[t]], [self.rps[gb_]])
                for cc in range(2):
                    self.tt("dve", wT[yg][:, cc, :], ge[cc], self.ps[2 + cc][:, 0:TP], ALU.mult, [rge[cc], self.rps[2 + cc]], [rwT[yg][cc]])
                if self.dbg and p_ == 0 and cp_ == 0:
                    self.dma(Sx["dbg_ge"], ge[0], [rge[0]], [], eng="pool")
                    self.dma(Sx["dbg_wt"], wT[yg][:, 0, :], [rwT[yg][0]], [], eng="pool")
            y_stage(62)
            if self.dbg and p_ == 0:
                self.dma(Sx["dbg_y"], ytok[:, 0, :], ryt[0], [], eng="pool")
            xt = zz[0]
            rxt = rzz[0]
            for t in range(NTP):
                self.dma(xt, rows(t), [], [rxt])
                self.tt("pool", xt, xt, ytok[:, t, :], ALU.add, [rxt] + ryt[t], [rxt])
                self.dma(rows(t), xt, [rxt], [], eng="pool")
            self.P.barrier()
        self.top = m
        self.P.barrier()

    def build(self):
        self.setup_consts()
        ph = self.phases
        for l in self.layers:
            self.build_ebb(l)
            if "E" in ph:
                self.phase_P(l)
            for s in range(self.nseq):
                xsrc = self.I["x"][s] if l == self.layers[0] else self.out[s]
                xo = self.out[s]
                if "A" in ph:
                    self.phase_A(l, xsrc)
                if "B" in ph:
                    self.mixer_AC(l, "a")
                    self.mixer_B(l)
                    self.mixer_AC(l, "c")
                if "C" in ph:
                    self.phase_C(l, xsrc, xo, l != self.layers[0])
                if "D" in ph:
                    self.phase_D(l, s, xo)
                if "E" in ph:
                    self.phase_E(l, xo)
        self.P.barrier()
        self.P.finalize(self.st)
        self.P.emit()
        self.st.close()
        return self.nc


def build_program(nseq=2, layers=(0, 1), phases="ABCDE", dbg=False, lw=L):
    nc = bass.Bass("TRN2", target_bir_lowering=False)
    kb = KB(nc, nseq, list(layers), phases, dbg, lw)
    kb.build()
    return nc, kb


def make_in_maps(inputs, nseq=2, ncores=8, small_peer=False):
    tabs = _static_tables()
    maps = []
    w = {name: np.ascontiguousarray(np.asarray(inputs[name], dtype=np.float32)) for name, _ in WEIGHT_SPECS}
    if small_peer:
        w["peer_u"] = np.ascontiguousarray(w["peer_u"][:, :128])
        w["peer_v"] = np.ascontiguousarray(w["peer_v"][:, :128])
    x = np.asarray(inputs["x"], dtype=np.float32)
    mem = np.asarray(inputs["mem"], dtype=np.float32)
    for c in range(ncores):
        m = dict(w)
        m.update(tabs)
        m["x"] = np.ascontiguousarray(x[2 * c:2 * c + nseq])
        m["mem"] = np.ascontiguousarray(mem[2 * c:2 * c + nseq])
        maps.append(m)
    return maps


def kernel(**inputs):
    nc, _ = build_program(nseq=2, layers=(0, 1), lw=L)
    maps = make_in_maps(inputs)
    res = run_bass_kernel_spmd(nc, maps, core_ids=list(range(8)))
    return np.concatenate([np.asarray(r["out"], dtype=np.float32) for r in res.results], axis=0)
```
